# Optimizing a Trainium2 kernel written in Bass

```python
import jax
import jax.numpy as jnp
from jax import lax
import numpy as np

D_MODEL = 2048
BATCH = 8
SEQ = 4096
DEPTH = 2

EPS = 1e-6
A_WIDTH = D_MODEL // 2
A_BLOCKS = 8
A_BLOCK = A_WIDTH // A_BLOCKS
A_CONV = 4
LRU_C = 8.0
B_WIDTH = D_MODEL // 2
B_HEAD = 64
B_HEADS = B_WIDTH // B_HEAD
B_DECAY_RANK = 64
B_AAA_RANK = 64
B_GATE_RANK = 160
B_PROJ = 3 * B_WIDTH + B_DECAY_RANK + B_AAA_RANK + B_GATE_RANK
B_SPLITS = (B_WIDTH, 2 * B_WIDTH, 3 * B_WIDTH, 3 * B_WIDTH + B_DECAY_RANK, 3 * B_WIDTH + B_DECAY_RANK + B_AAA_RANK)
B_LN_EPS = 64e-5
EVEN_IN = 2 * A_WIDTH + B_PROJ
C_WIDTH = 2 * D_MODEL
C_HEADS = 8
C_HEAD = C_WIDTH // C_HEADS
C_QKV_BLOCK = 4
C_CONV = 4
C_CHUNK = 64
D_FF = 5632
FFN_CONV = 3
N_EVEN = (DEPTH + 1) // 2
N_ODD = DEPTH // 2

kernel_name = 'hybrid_rglru_rwkv7_mlstm_convffn'


def rms_norm(x, g):
    x32 = x.astype(jnp.float32)
    y = x32 * lax.rsqrt(jnp.mean(x32 * x32, axis=-1, keepdims=True) + EPS)
    return (y * g).astype(x.dtype)


def head_layer_norm(x, eps):
    x32 = x.astype(jnp.float32)
    xc = x32 - jnp.mean(x32, axis=-1, keepdims=True)
    return xc * lax.rsqrt(jnp.mean(xc * xc, axis=-1, keepdims=True) + eps)


def causal_dwconv(x, w, b):
    k_width, t_len = w.shape[0], x.shape[1]
    xp = jnp.pad(x, ((0, 0), (k_width - 1, 0), (0, 0)))
    out = b + xp[:, 0:t_len] * w[0]
    for j in range(1, k_width):
        out = out + xp[:, j:j + t_len] * w[j]
    return out


def token_shift(x):
    return jnp.pad(x[:, :-1], ((0, 0), (1, 0), (0, 0)))


def block_diag(x, w):
    g, bs = w.shape[0], w.shape[1]
    y = jnp.einsum('btgi,gij->btgj', x.reshape(x.shape[0], x.shape[1], g, bs), w)
    return y.reshape(x.shape)


def rg_lru(x, w_r, b_r, w_i, b_i, lam):
    x32 = x.astype(jnp.float32)
    r = jax.nn.sigmoid(block_diag(x32, w_r) + b_r)
    i = jax.nn.sigmoid(block_diag(x32, w_i) + b_i)
    log_a = -LRU_C * r * jax.nn.softplus(-lam)
    a = jnp.exp(log_a)
    u = jnp.sqrt(-jnp.expm1(2.0 * log_a)) * (i * x32)

    def combine(lhs, rhs):
        a_l, u_l = lhs
        a_r, u_r = rhs
        return a_r * a_l, a_r * u_l + u_r

    _, h = lax.associative_scan(combine, (a, u), axis=1)
    return h.astype(x.dtype)


def rwkv7_time_mix(p, mu, w0, w_up, a0, a_up, g_up, k_k, k_a, r_k, ln_w, ln_b):
    bsz, t_len, _ = p.shape
    p = p + (token_shift(p) - p) * mu
    r, k, v, xw, xa, xg = jnp.split(p, B_SPLITS, axis=-1)
    log_w = -jnp.exp(-jax.nn.softplus(-(w0 + jnp.tanh(xw) @ w_up)) - 0.5)
    a = jax.nn.sigmoid(a0 + xa @ a_up)
    g = jax.nn.sigmoid(xg) @ g_up

    def heads(t):
        return t.astype(jnp.float32).reshape(bsz, t_len, B_HEADS, B_HEAD)

    kk = heads(k * k_k)
    kk = kk * lax.rsqrt(jnp.maximum(jnp.sum(kk * kk, axis=-1, keepdims=True), 1e-12))
    k = heads(k * (1.0 + (a - 1.0) * k_a))
    r, v, a, w = heads(r), heads(v), heads(a), jnp.exp(heads(log_w))

    def step(s, inp):
        r_t, w_t, k_t, v_t, a_t, b_t = inp
        sa = jnp.einsum('bhvk,bhk->bhv', s, a_t)
        s = s * w_t[:, :, None, :] + sa[..., None] * b_t[:, :, None, :] + v_t[..., None] * k_t[:, :, None, :]
        return s, jnp.einsum('bhvk,bhk->bhv', s, r_t)

    xs = tuple(jnp.moveaxis(t, 1, 0) for t in (r, w, k, v, -kk, kk * a))
    s0 = jnp.zeros((bsz, B_HEADS, B_HEAD, B_HEAD), jnp.float32)
    _, y = lax.scan(step, s0, xs)
    y = jnp.moveaxis(y, 0, 1)
    y = head_layer_norm(y, B_LN_EPS) * ln_w.reshape(B_HEADS, B_HEAD) + ln_b.reshape(B_HEADS, B_HEAD)
    y = y + jnp.sum(r * k * r_k, axis=-1, keepdims=True) * v
    return (y.reshape(bsz, t_len, B_WIDTH) * g).astype(p.dtype)


def mlstm_chunkwise(q, k, v, i_pre, f_pre):
    bsz, t_len = q.shape[0], q.shape[1]
    nc = t_len // C_CHUNK

    def chunks(t):
        t = t.reshape((bsz, nc, C_CHUNK) + t.shape[2:])
        return jnp.moveaxis(jnp.moveaxis(t, 1, 0), 2, 3)

    causal = jnp.tril(jnp.ones((C_CHUNK, C_CHUNK), dtype=bool))

    def step(carry, inp):
        c_mat, n_vec, m = carry
        q_c, k_c, v_c, lf, li = inp
        b = jnp.cumsum(lf, axis=-1)
        d_log = jnp.where(causal, b[..., :, None] - b[..., None, :] + li[..., None, :], -jnp.inf)
        inter = b + m[..., None]
        m_t = jnp.maximum(inter, jnp.max(d_log, axis=-1))
        s = jnp.einsum('bhtd,bhsd->bhts', q_c, k_c) * jnp.exp(d_log - m_t[..., None])
        sc = jnp.exp(inter - m_t)
        num = jnp.einsum('bhts,bhsd->bhtd', s, v_c) + sc[..., None] * jnp.einsum('bhvk,bhtk->bhtv', c_mat, q_c)
        den = jnp.sum(s, axis=-1) + sc * jnp.einsum('bhk,bhtk->bht', n_vec, q_c)
        h = num / jnp.maximum(jnp.abs(den), jnp.exp(-m_t))[..., None]
        b_last = b[..., -1]
        g_log = b_last[..., None] - b + li
        m_new = jnp.maximum(b_last + m, jnp.max(g_log, axis=-1))
        e = jnp.exp(g_log - m_new[..., None])
        decay = jnp.exp(b_last + m - m_new)
        c_mat = decay[..., None, None] * c_mat + jnp.einsum('bhsv,bhsk->bhvk', v_c * e[..., None], k_c)
        n_vec = decay[..., None] * n_vec + jnp.einsum('bhs,bhsk->bhk', e, k_c)
        return (c_mat, n_vec, m_new), h

    k = k * (C_HEAD ** -0.5)
    xs = (chunks(q), chunks(k), chunks(v), chunks(jax.nn.log_sigmoid(f_pre)), chunks(i_pre))
    init = (jnp.zeros((bsz, C_HEADS, C_HEAD, C_HEAD), jnp.float32),
            jnp.zeros((bsz, C_HEADS, C_HEAD), jnp.float32),
            jnp.zeros((bsz, C_HEADS), jnp.float32))
    _, h = lax.scan(step, init, xs)
    return jnp.moveaxis(jnp.moveaxis(h, 3, 2), 0, 1).reshape(bsz, t_len, C_HEADS, C_HEAD)


def rglru_rwkv7_layer(x, norm, w_in, a_conv_w, a_conv_b, a_w_r, a_b_r, a_w_i, a_b_i, a_lambda,
                      b_mu, b_w0, b_w_up, b_a0, b_a_up, b_g_up, b_k_k, b_k_a, b_r_k, b_ln_w, b_ln_b, w_out):
    h = rms_norm(x, norm)
    xa, ga, pb = jnp.split(h @ w_in, [A_WIDTH, 2 * A_WIDTH], axis=-1)
    ya = rg_lru(causal_dwconv(xa, a_conv_w, a_conv_b), a_w_r, a_b_r, a_w_i, a_b_i, a_lambda) * jax.nn.gelu(ga)
    yb = rwkv7_time_mix(pb, b_mu, b_w0, b_w_up, b_a0, b_a_up, b_g_up, b_k_k, b_k_a, b_r_k, b_ln_w, b_ln_b)
    return x + jnp.concatenate([ya, yb], axis=-1) @ w_out


def mlstm_layer(x, norm, w_in, conv_w, conv_b, w_q, w_k, w_v, w_if, b_if, ln_w, skip, w_out):
    bsz, t_len, _ = x.shape
    h = rms_norm(x, norm)
    xm, z = jnp.split(h @ w_in, [C_WIDTH], axis=-1)
    xc = jax.nn.silu(causal_dwconv(xm, conv_w, conv_b))
    q, k, v = block_diag(xc, w_q), block_diag(xc, w_k), block_diag(xm, w_v)
    gates = (jnp.einsum('btc,cg->btg', q, w_if[0]) + jnp.einsum('btc,cg->btg', k, w_if[1])
             + jnp.einsum('btc,cg->btg', v, w_if[2]) + b_if)
    i_pre, f_pre = jnp.split(gates.astype(jnp.float32), [C_HEADS], axis=-1)

    def heads(t):
        return t.astype(jnp.float32).reshape(bsz, t_len, C_HEADS, C_HEAD)

    hc = mlstm_chunkwise(heads(q), heads(k), heads(v), i_pre, f_pre)
    hn = head_layer_norm(hc, EPS) * ln_w.reshape(C_HEADS, C_HEAD)
    hs = hn.reshape(bsz, t_len, C_WIDTH).astype(x.dtype) + skip * xc
    return x + (hs * jax.nn.silu(z)) @ w_out


def conv_ffn(x, norm, w_gate, w_up, conv_w, conv_b, w_down):
    h = rms_norm(x, norm)
    u = jax.nn.silu(causal_dwconv(h @ w_gate, conv_w, conv_b)) * (h @ w_up)
    return x + u @ w_down


def setup_inputs(seed: int = 0) -> dict:
    key = jax.random.key(seed)
    ks = list(jax.random.split(key, 48))
    f32 = jnp.float32

    def normal(i, shape, scale):
        return jax.random.normal(ks[i], shape, f32) * scale

    def uniform(i, shape, lo, hi):
        return jax.random.uniform(ks[i], shape, f32, lo, hi)

    def gain(i, shape):
        return 1.0 + normal(i, shape, 0.02)

    ne, no, nl = N_EVEN, N_ODD, DEPTH
    a_decay = uniform(9, (ne, A_WIDTH), 0.9, 0.999)
    b_if = jnp.concatenate([normal(33, (no, C_HEADS), 0.1), uniform(34, (no, C_HEADS), 3.0, 6.0)], axis=-1)
    return {
        'x': normal(0, (BATCH, SEQ, D_MODEL), 1.0),
        'even_norm': gain(1, (ne, D_MODEL)),
        'even_w_in': normal(2, (ne, D_MODEL, EVEN_IN), D_MODEL ** -0.5),
        'a_conv_w': normal(3, (ne, A_CONV, A_WIDTH), A_CONV ** -0.5),
        'a_conv_b': normal(4, (ne, A_WIDTH), 0.01),
        'a_w_r': normal(5, (ne, A_BLOCKS, A_BLOCK, A_BLOCK), A_BLOCK ** -0.5),
        'a_b_r': normal(6, (ne, A_WIDTH), 0.1),
        'a_w_i': normal(7, (ne, A_BLOCKS, A_BLOCK, A_BLOCK), A_BLOCK ** -0.5),
        'a_b_i': normal(8, (ne, A_WIDTH), 0.1),
        'a_lambda': jnp.log(a_decay) - jnp.log1p(-a_decay),
        'b_mu': uniform(10, (ne, B_PROJ), 0.0, 1.0),
        'b_w0': uniform(11, (ne, B_WIDTH), -6.0, 0.0),
        'b_w_up': normal(12, (ne, B_DECAY_RANK, B_WIDTH), 0.5 * B_DECAY_RANK ** -0.5),
        'b_a0': normal(13, (ne, B_WIDTH), 0.1),
        'b_a_up': normal(14, (ne, B_AAA_RANK, B_WIDTH), 0.5 * B_AAA_RANK ** -0.5),
        'b_g_up': normal(15, (ne, B_GATE_RANK, B_WIDTH), B_GATE_RANK ** -0.5),
        'b_k_k': uniform(16, (ne, B_WIDTH), 0.7, 1.0),
        'b_k_a': uniform(17, (ne, B_WIDTH), 0.8, 1.2),
        'b_r_k': normal(18, (ne, B_HEADS, B_HEAD), 0.1),
        'b_ln_w': gain(19, (ne, B_WIDTH)),
        'b_ln_b': normal(20, (ne, B_WIDTH), 0.01),
        'even_w_out': normal(21, (ne, D_MODEL, D_MODEL), D_MODEL ** -0.5),
        'odd_norm': gain(22, (no, D_MODEL)),
        'odd_w_in': normal(23, (no, D_MODEL, 2 * C_WIDTH), D_MODEL ** -0.5),
        'c_conv_w': normal(24, (no, C_CONV, C_WIDTH), C_CONV ** -0.5),
        'c_conv_b': normal(25, (no, C_WIDTH), 0.01),
        'c_w_q': normal(26, (no, C_WIDTH // C_QKV_BLOCK, C_QKV_BLOCK, C_QKV_BLOCK), C_QKV_BLOCK ** -0.5),
        'c_w_k': normal(27, (no, C_WIDTH // C_QKV_BLOCK, C_QKV_BLOCK, C_QKV_BLOCK), C_QKV_BLOCK ** -0.5),
        'c_w_v': normal(28, (no, C_WIDTH // C_QKV_BLOCK, C_QKV_BLOCK, C_QKV_BLOCK), C_QKV_BLOCK ** -0.5),
        'c_w_if': normal(29, (no, 3, C_WIDTH, 2 * C_HEADS), (3 * C_WIDTH) ** -0.5),
        'c_b_if': b_if,
        'c_ln_w': gain(30, (no, C_WIDTH)),
        'c_skip': gain(31, (no, C_WIDTH)),
        'odd_w_out': normal(32, (no, C_WIDTH, D_MODEL), C_WIDTH ** -0.5),
        'ffn_norm': gain(35, (nl, D_MODEL)),
        'ffn_w_gate': normal(36, (nl, D_MODEL, D_FF), D_MODEL ** -0.5),
        'ffn_w_up': normal(37, (nl, D_MODEL, D_FF), D_MODEL ** -0.5),
        'ffn_conv_w': normal(38, (nl, FFN_CONV, D_FF), FFN_CONV ** -0.5),
        'ffn_conv_b': normal(39, (nl, D_FF), 0.01),
        'ffn_w_down': normal(40, (nl, D_FF, D_MODEL), D_FF ** -0.5),
        'final_norm': gain(41, (D_MODEL,)),
    }


def reference(x, even_norm, even_w_in, a_conv_w, a_conv_b, a_w_r, a_b_r, a_w_i, a_b_i, a_lambda,
              b_mu, b_w0, b_w_up, b_a0, b_a_up, b_g_up, b_k_k, b_k_a, b_r_k, b_ln_w, b_ln_b, even_w_out,
              odd_norm, odd_w_in, c_conv_w, c_conv_b, c_w_q, c_w_k, c_w_v, c_w_if, c_b_if, c_ln_w, c_skip, odd_w_out,
              ffn_norm, ffn_w_gate, ffn_w_up, ffn_conv_w, ffn_conv_b, ffn_w_down, final_norm):
    for layer in range(DEPTH):
        if layer % 2 == 0:
            e = layer // 2
            x = rglru_rwkv7_layer(x, even_norm[e], even_w_in[e], a_conv_w[e], a_conv_b[e], a_w_r[e], a_b_r[e],
                                  a_w_i[e], a_b_i[e], a_lambda[e], b_mu[e], b_w0[e], b_w_up[e], b_a0[e], b_a_up[e],
                                  b_g_up[e], b_k_k[e], b_k_a[e], b_r_k[e], b_ln_w[e], b_ln_b[e], even_w_out[e])
        else:
            o = layer // 2
            x = mlstm_layer(x, odd_norm[o], odd_w_in[o], c_conv_w[o], c_conv_b[o], c_w_q[o], c_w_k[o], c_w_v[o],
                            c_w_if[o], c_b_if[o], c_ln_w[o], c_skip[o], odd_w_out[o])
        x = conv_ffn(x, ffn_norm[layer], ffn_w_gate[layer], ffn_w_up[layer], ffn_conv_w[layer],
                     ffn_conv_b[layer], ffn_w_down[layer])
    return rms_norm(x, final_norm)
```

```python
from contextlib import ExitStack
import numpy as np
import concourse.bass as bass
import concourse.mybir as mybir
from concourse.bass_utils import run_bass_kernel_spmd

F32 = mybir.dt.float32
BF16 = mybir.dt.bfloat16
AF = mybir.ActivationFunctionType
ALU = mybir.AluOpType
AX = mybir.AxisListType

ENGS = ('pe', 'act', 'dve', 'pool', 'sp')


class Buf:
    __slots__ = ('name', 'kind', 'lw', 'rd', 'semw', 'semr', 'cw', 'cr')

    def __init__(self, name, kind):
        self.name = name
        self.kind = kind
        self.lw = None
        self.rd = {}
        self.semw = None
        self.semr = None
        self.cw = 0
        self.cr = 0


class V:
    __slots__ = ('ap', 'buf')

    def __init__(self, ap, buf):
        self.ap = ap
        self.buf = buf

    def __getitem__(self, idx):
        return V(self.ap[idx], self.buf)

    def re(self, pattern, **kw):
        return V(self.ap.rearrange(pattern, **kw), self.buf)

    def sub(self, name_unused, idx):
        return V(self.ap[idx], self.buf)


def _ap(x):
    return x.ap if isinstance(x, V) else x


class Prog:
    def __init__(self, nc):
        self.nc = nc
        self.stack = ExitStack()
        self.ins = []
        self.nsem = 0
        self.npsum = 0
        self.last_eng = {}
        self.last_dma = {}
        self.bar = set()
        self.sem_pool = []
        self.sem_active = []

    def dram(self, name, shape, dtype, kind="Internal"):
        t = self.nc.dram_tensor(name, list(shape), dtype, kind=kind)
        return V(t.ap(), Buf(name, 'dram'))

    def sbuf(self, name, shape, dtype, nbuf=None):
        t = self.stack.enter_context(self.nc.sbuf_tensor(name, list(shape), dtype))
        return V(t[:], Buf(name, 'sbuf'))

    def psum(self, name, shape=(128, 512), dtype=F32):
        t = self.stack.enter_context(self.nc.psum_tensor(name, list(shape), dtype))
        return V(t[:], Buf(name, 'psum'))

    def view(self, v, name):
        return V(v.ap, Buf(name, v.buf.kind))

    def _sem(self, name):
        self.nsem += 1
        return self.stack.enter_context(self.nc.semaphore(name))

    def emit(self, eng, fn, reads, writes, dma=None):
        iid = len(self.ins)
        deps = set(self.bar)
        wb = []
        for v in writes:
            b = v.buf
            if b in wb:
                continue
            wb.append(b)
            if b.lw is not None:
                deps.add(b.lw)
            deps.update(b.rd.values())
        rb = []
        for v in reads:
            if not isinstance(v, V):
                continue
            b = v.buf
            if b in wb or b in rb:
                continue
            rb.append(b)
            if b.lw is not None:
                deps.add(b.lw)
        key = eng
        dsem = None
        if dma is not None:
            kind, sb = dma
            if kind == 'w':
                if sb.semw is None:
                    sb.semw, sb.cw = self._take_sem("dw_" + sb.name)
                    self.sem_active.append((sb, 'w'))
                sb.cw += 16
                dsem = (sb.semw, sb.cw)
            else:
                if sb.semr is None:
                    sb.semr, sb.cr = self._take_sem("dr_" + sb.name)
                    self.sem_active.append((sb, 'r'))
                sb.cr += 16
                dsem = (sb.semr, sb.cr)
            key = ('dma', id(dsem[0]))
        for b in wb:
            b.lw = iid
            b.rd = {}
        for b in rb:
            b.rd[key] = iid
        if dsem is None:
            self.last_eng[eng] = iid
        else:
            self.last_dma[id(dsem[0])] = iid
        self.ins.append((eng, fn, deps, dsem))
        return iid

    def _take_sem(self, name):
        if self.sem_pool:
            return self.sem_pool.pop()
        return self._sem(name), 0

    def barrier(self):
        self.bar = set(self.last_eng.values()) | set(self.last_dma.values())
        for (b, kind) in self.sem_active:
            if kind == 'w':
                self.sem_pool.append((b.semw, b.cw))
                b.semw = None
            else:
                self.sem_pool.append((b.semr, b.cr))
                b.semr = None
        self.sem_active = []

    def dma(self, q, out, in_):
        ob, ib = out.buf, in_.buf
        if ob.kind == 'sbuf':
            d = ('w', ob)
        else:
            assert ib.kind == 'sbuf', "dram->dram dma not supported"
            d = ('r', ib)
        o, i = out.ap, in_.ap
        return self.emit(q, lambda e: e.dma_start(out=o, in_=i), [in_], [out], dma=d)

    def mm(self, out, lhsT, rhs, start=True, stop=True, **kw):
        o, l, r = out.ap, lhsT.ap, rhs.ap
        return self.emit('pe', lambda e: e.matmul(o, l, r, start=start, stop=stop, **kw),
                         [lhsT, rhs], [out])

    def transpose(self, out, in_, ident):
        o, i, d = out.ap, in_.ap, ident.ap
        return self.emit('pe', lambda e: e.transpose(o, i, d), [in_, ident], [out])

    def act(self, out, in_, func, bias=None, scale=1.0, accum=None, eng='act'):
        o, i = out.ap, in_.ap
        b, s, a = _ap(bias), _ap(scale), _ap(accum)
        kw = {}
        if b is not None:
            kw['bias'] = b
        if a is not None:
            kw['accum_out'] = a
        w = [out] + ([accum] if accum is not None else [])
        return self.emit(eng, lambda e: e.activation(out=o, in_=i, func=func, scale=s, **kw),
                         [in_, bias, scale], w)

    def tt(self, eng, out, in0, in1, op):
        o, a, b = out.ap, in0.ap, in1.ap
        return self.emit(eng, lambda e: e.tensor_tensor(out=o, in0=a, in1=b, op=op), [in0, in1], [out])

    def ts(self, eng, out, in0, s1, s2, op0, op1=None, accum=None):
        o, a = out.ap, in0.ap
        x1, x2, ac = _ap(s1), _ap(s2), _ap(accum)
        kw = {}
        if op1 is not None:
            kw['op1'] = op1
        if ac is not None:
            kw['accum_out'] = ac
        w = [out] + ([accum] if accum is not None else [])
        return self.emit(eng, lambda e: e.tensor_scalar(out=o, in0=a, scalar1=x1, scalar2=x2, op0=op0, **kw),
                         [in0, s1, s2], w)

    def stt(self, eng, out, in0, scalar, in1, op0, op1):
        o, a, b, s = out.ap, in0.ap, in1.ap, _ap(scalar)
        return self.emit(eng, lambda e: e.scalar_tensor_tensor(out=o, in0=a, scalar=s, in1=b, op0=op0, op1=op1),
                         [in0, in1, scalar], [out])

    def scan(self, out, d0, d1, init, op0, op1):
        o, a, b, i = out.ap, d0.ap, d1.ap, _ap(init)
        return self.emit('dve', lambda e: e.tensor_tensor_scan(out=o, data0=a, data1=b, initial=i, op0=op0, op1=op1),
                         [d0, d1, init], [out])

    def copy(self, eng, out, in_):
        o, i = out.ap, in_.ap
        if eng == 'act':
            return self.emit(eng, lambda e: e.copy(out=o, in_=i), [in_], [out])
        return self.emit(eng, lambda e: e.tensor_copy(out=o, in_=i), [in_], [out])

    def memset(self, eng, out, val):
        o = out.ap
        return self.emit(eng, lambda e: e.memset(o, val), [], [out])

    def reduce(self, out, in_, op, axis=AX.X, eng='dve'):
        o, i = out.ap, in_.ap
        return self.emit(eng, lambda e: e.tensor_reduce(out=o, in_=i, axis=axis, op=op), [in_], [out])

    def recip(self, out, in_):
        o, i = out.ap, in_.ap
        return self.emit('dve', lambda e: e.reciprocal(out=o, in_=i), [in_], [out])

    def affine_select(self, out, in_, pattern, cmp, fill, base, cm):
        o, i = out.ap, in_.ap
        return self.emit('pool', lambda e: e.affine_select(out=o, in_=i, pattern=pattern, compare_op=cmp,
                                                           fill=fill, base=base, channel_multiplier=cm),
                         [in_], [out])

    def finish(self, final_wait=True):
        nc = self.nc
        ins = self.ins
        n = len(ins)
        needed = [False] * n
        for (eng, fn, deps, dsem) in ins:
            for d in deps:
                if ins[d][0] == 'pe' and eng == 'pe' and ins[d][3] is None:
                    continue
                needed[d] = True
        esem = {e: self._sem("c_" + e) for e in ('pe', 'act', 'dve', 'pool')}
        ecnt = {e: 0 for e in esem}
        token = [None] * n
        known = {e: {} for e in ENGS}
        snap = [None] * n
        stream = {e: [] for e in ENGS}
        for iid, (eng, fn, deps, dsem) in enumerate(ins):
            kn = known[eng]
            waits = {}
            for d in deps:
                if ins[d][0] == 'pe' and eng == 'pe' and ins[d][3] is None:
                    continue
                sem, val = token[d]
                sid = id(sem)
                if kn.get(sid, 0) >= val:
                    continue
                if sid not in waits or waits[sid][1] < val:
                    waits[sid] = (sem, val)
            for d in deps:
                s = snap[d]
                if s is not None and token[d] is not None and id(token[d][0]) in waits:
                    for k2, v2 in s.items():
                        if kn.get(k2, 0) < v2:
                            kn[k2] = v2
            wl = []
            for sid, (sem, val) in waits.items():
                if kn.get(sid, 0) >= val:
                    continue
                kn[sid] = val
                wl.append((sem, val))
            inc = None
            if dsem is not None:
                token[iid] = dsem
                inc = (dsem[0], 16)
            elif needed[iid]:
                ecnt[eng] += 1
                token[iid] = (esem[eng], ecnt[eng])
                inc = (esem[eng], 1)
                kn[id(esem[eng])] = max(kn.get(id(esem[eng]), 0), 0)
            if needed[iid] or dsem is not None:
                snap[iid] = dict(kn)
            stream[eng].append((wl, fn, inc))
        finals = []
        seen = set()
        for (eng, fn, deps, dsem) in ins:
            if dsem is not None:
                seen.add(id(dsem[0]))
        allbufs = {}
        for (eng, fn, deps, dsem) in ins:
            if dsem is not None:
                sid = id(dsem[0])
                if sid not in allbufs or allbufs[sid][1] < dsem[1]:
                    allbufs[sid] = dsem
        finals = list(allbufs.values())
        self.stats = {e: len(stream[e]) for e in ENGS}
        self.stats['waits'] = sum(len(w) for e in ENGS for (w, _, _) in stream[e])
        self.stats['sems'] = self.nsem

        with nc.Block() as block:
            def run(e, name):
                for (wl, fn, inc) in stream[name]:
                    for (sem, val) in wl:
                        e.wait_ge(sem, val)
                    r = fn(e)
                    if inc is not None:
                        r.then_inc(inc[0], inc[1])
                if name == 'sp' and final_wait:
                    for (sem, val) in finals:
                        e.wait_ge(sem, val)
                    for en in esem:
                        if ecnt[en] > 0:
                            e.wait_ge(esem[en], ecnt[en])

            @block.sync
            def _(e):
                run(e, 'sp')

            @block.scalar
            def _(e):
                run(e, 'act')

            @block.vector
            def _(e):
                run(e, 'dve')

            @block.gpsimd
            def _(e):
                run(e, 'pool')

            @block.tensor
            def _(e):
                run(e, 'pe')
        self.stack.close()
        return nc


T = 4096
D = 2048
EPS = 1e-6
J_EIN = 43
P_ROWS = 5408
DFF = 5632
KFF = 44
CW = 4096
B_LN_EPS = 64e-5


def _cc(v, pad_to=None):
    v = np.asarray(v, np.float32).reshape(-1)
    if pad_to is not None and v.size < pad_to:
        v = np.concatenate([v, np.zeros(pad_to - v.size, np.float32)])
    return np.ascontiguousarray(v.reshape(-1, 128).T)


def _tile_w(w, J):
    K, M = w.shape
    kc = K // 128
    wp = np.zeros((K, J * 128), np.float32)
    wp[:, :M] = w
    return np.ascontiguousarray(wp.reshape(kc, 128, J, 128).transpose(2, 1, 0, 3).reshape(J, 128, kc * 128))


def pack_consts(inp):
    ent = []

    def add(name, arr):
        ent.append((name, np.asarray(arr, np.float32)))

    add('even_norm', _cc(inp['even_norm'][0]))
    add('ffn_norm0', _cc(inp['ffn_norm'][0]))
    add('ffn_norm1', _cc(inp['ffn_norm'][1]))
    add('odd_norm', _cc(inp['odd_norm'][0]))
    add('final_norm', _cc(inp['final_norm']))
    add('a_conv_w', inp['a_conv_w'][0].reshape(4, 8, 128).transpose(2, 1, 0).reshape(128, 32))
    for nm in ('a_conv_b', 'a_b_r', 'a_b_i', 'a_lambda'):
        add(nm, _cc(inp[nm][0]))
    add('b_mu', _cc(inp['b_mu'][0], 27 * 128))
    for nm in ('b_w0', 'b_a0', 'b_k_k', 'b_k_a', 'b_r_k', 'b_ln_w', 'b_ln_b'):
        add(nm, _cc(inp[nm][0]))
    for l in range(2):
        add('ffn_conv_w%d' % l, inp['ffn_conv_w'][l].reshape(3, KFF, 128).transpose(2, 1, 0).reshape(128, KFF * 3))
        add('ffn_conv_b%d' % l, _cc(inp['ffn_conv_b'][l]))
    add('c_conv_w', inp['c_conv_w'][0].reshape(4, 32, 128).transpose(2, 1, 0).reshape(128, 128))
    for nm in ('c_conv_b', 'c_ln_w', 'c_skip'):
        add(nm, _cc(inp[nm][0]))
    for nm in ('c_w_q', 'c_w_k', 'c_w_v'):
        add(nm, inp[nm][0].reshape(32, 128, 4).transpose(1, 0, 2).reshape(128, 128))
    bif = np.zeros((128, 1), np.float32)
    bif[:16, 0] = inp['c_b_if'][0]
    add('c_b_if', bif)
    offs = {}
    o = 0
    for name, a in ent:
        offs[name] = (o, a.shape[1])
        o += a.shape[1]
    return np.ascontiguousarray(np.concatenate([a for _, a in ent], axis=1)), offs


def const_offsets():
    dummy = {
        'even_norm': np.zeros((1, D)), 'ffn_norm': np.zeros((2, D)), 'odd_norm': np.zeros((1, D)),
        'final_norm': np.zeros(D), 'a_conv_w': np.zeros((1, 4, 1024)),
        'b_mu': np.zeros((1, 3360)), 'ffn_conv_w': np.zeros((2, 3, DFF)), 'ffn_conv_b': np.zeros((2, DFF)),
        'c_conv_w': np.zeros((1, 4, CW)), 'c_b_if': np.zeros((1, 16)),
    }
    for nm in ('a_conv_b', 'a_b_r', 'a_b_i', 'a_lambda', 'b_w0', 'b_a0', 'b_k_k', 'b_k_a', 'b_r_k', 'b_ln_w', 'b_ln_b'):
        dummy[nm] = np.zeros((1, 1024))
    for nm in ('c_conv_b', 'c_ln_w', 'c_skip'):
        dummy[nm] = np.zeros((1, CW))
    for nm in ('c_w_q', 'c_w_k', 'c_w_v'):
        dummy[nm] = np.zeros((1, 1024, 4, 4))
    c, offs = pack_consts(dummy)
    return c.shape[1], offs


class Arena:
    def __init__(self, P, cols):
        self.P = P
        self.t = P.sbuf("arena", [128, cols], F32)
        self.cols = cols
        self.base = 0
        self.off = 0
        self.n = 0

    def reset(self):
        self.off = self.base

    def persist(self):
        self.base = self.off

    def alloc(self, cols, dtype=F32, name=None):
        n32 = cols if dtype == F32 else (cols + 1) // 2
        a = self.off
        self.off += n32
        assert self.off <= self.cols, ("arena overflow", self.off, self.cols)
        ap = self.t.ap[:, a:a + n32]
        if dtype != F32:
            ap = ap.bitcast(dtype)[:, 0:cols]
        self.n += 1
        return V(ap, Buf(name or ("ar%d" % self.n), 'sbuf'))


class Ctx:
    pass


def load_cast(X, dst, src, rows=128):
    cols = dst.ap.shape[1]
    stg = X.A.alloc(cols, F32)
    X.P.dma('sp', stg[0:rows], src)
    X.P.copy('pool', dst[0:rows], stg[0:rows])


def split_groups(n, g):
    g = min(g, n)
    base, rem = divmod(n, g)
    out = []
    a = 0
    for i in range(g):
        b = a + base + (1 if i < rem else 0)
        out.append((a, b))
        a = b
    return out


def gemm(X, src, KC, wsets, J, TB, epi, jlist=None):
    P, A = X.P, X.A
    ns = len(wsets)
    groups = split_groups(KC, 4)
    act = [A.alloc((b - a) * TB, BF16) for (a, b) in groups]
    wb = [[A.alloc(KC * 128, BF16) for _ in range(2)] for _ in range(ns)]
    wst = [[A.alloc(KC * 128, F32) for _ in range(2)] for _ in range(ns)]
    wcnt = 0
    srcv = src.re("(k p) t -> p k t", p=128)
    nsub = TB // 512
    cnt = 0
    jl = list(range(J)) if jlist is None else jlist
    for tb in range(T // TB):
        for gi, (a, b) in enumerate(groups):
            P.dma('sp', act[gi].re("p (k t) -> p k t", t=TB), srcv[:, a:b, tb * TB:(tb + 1) * TB])
        for ji, j in enumerate(jl):
            for s in range(ns):
                P.dma('sp', wst[s][ji % 2], wsets[s][j])
                P.copy('pool' if wcnt % 2 == 0 else 'act', wb[s][ji % 2], wst[s][ji % 2])
                wcnt += 1
            for sub in range(nsub):
                pss = []
                for s in range(ns):
                    ps = X.ps[(cnt % (6 // ns)) * ns + s]
                    pss.append(ps)
                    for gi, (a, b) in enumerate(groups):
                        for k in range(a, b):
                            P.mm(ps, wb[s][ji % 2][:, k * 128:(k + 1) * 128],
                                 act[gi][:, (k - a) * TB + sub * 512:(k - a) * TB + (sub + 1) * 512],
                                 start=(k == 0), stop=(k == KC - 1))
                cnt += 1
                epi(j, tb * TB + sub * 512, pss)


def phase_norm(X, src, gname, dst, out_f32=False):
    P, A = X.P, X.A
    P.barrier()
    A.reset()
    g = X.C(gname)
    odt = F32 if out_f32 else BF16
    xs = [A.alloc(16 * 512, F32) for _ in range(2)]
    sq = [A.alloc(16 * 512, BF16) for _ in range(2)]
    hs = [A.alloc(16 * 512, odt) for _ in range(2)]
    rs = [A.alloc(512, F32) for _ in range(2)]
    sv = src.re("(k p) t -> p k t", p=128)
    dv = dst.re("(k p) t -> p k t", p=128)
    for it in range(T // 512):
        x, q, h, r = xs[it % 2], sq[it % 2], hs[it % 2], rs[it % 2]
        P.dma('sp', x.re("p (k t) -> p k t", t=512), sv[:, :, it * 512:(it + 1) * 512])
        P.act(q, x, AF.Square)
        ps = X.ps[6 + it % 2]
        for k in range(16):
            P.mm(ps, X.ones_bf, q[:, k * 512:(k + 1) * 512], start=(k == 0), stop=(k == 15))
        P.ts('dve', r, ps, EPS, None, ALU.add)
        P.act(r, r, AF.Ln)
        P.act(r, r, AF.Exp, scale=-0.5)
        for k in range(16):
            P.stt('dve', h[:, k * 512:(k + 1) * 512], x[:, k * 512:(k + 1) * 512],
                  g[:, k:k + 1], r, ALU.mult, ALU.mult)
        P.dma('act', dv[:, :, it * 512:(it + 1) * 512], h.re("p (k t) -> p k t", t=512))


def phase_even_inproj(X):
    P, A = X.P, X.A
    P.barrier()
    A.reset()
    st = [A.alloc(512, F32) for _ in range(4)]
    c = [0]

    def epi(j, t0, pss):
        s = st[c[0] % 4]
        if c[0] % 2 == 0:
            P.copy('act', s, pss[0])
        else:
            P.copy('dve', s, pss[0])
        c[0] += 1
        rows = min(128, P_ROWS - j * 128)
        P.dma('act', X.pT[j * 128:j * 128 + rows, t0:t0 + 512], s[0:rows, :])

    gemm(X, X.hT, 16, [X.w_ein], J_EIN, 2048, epi)


def phase_resid_gemm(X, src, KC, w, TB, xin, xout):
    P, A = X.P, X.A
    P.barrier()
    A.reset()
    st = [A.alloc(512, F32) for _ in range(4)]
    c = [0]

    def epi(j, t0, pss):
        s = st[c[0] % 4]
        c[0] += 1
        P.dma('sp', s, xin[j * 128:(j + 1) * 128, t0:t0 + 512])
        P.tt('dve', s, s, pss[0], ALU.add)
        P.dma('act', xout[j * 128:(j + 1) * 128, t0:t0 + 512], s)

    gemm(X, src, KC, [w], 16, TB, epi)


def phase_ffn_up(X, l):
    P, A = X.P, X.A
    P.barrier()
    A.reset()
    cw = X.C('ffn_conv_w%d' % l)
    cb = X.C('ffn_conv_b%d' % l)
    gb = [A.alloc(514, F32) for _ in range(3)]
    acc = [A.alloc(512, F32) for _ in range(2)]
    tmp = [A.alloc(512, F32) for _ in range(2)]
    ub = [A.alloc(512, BF16) for _ in range(3)]
    halo = A.alloc(KFF * 2, F32)
    c = [0]
    TB = 2048

    def epi(j, t0, pss):
        i = c[0]
        c[0] += 1
        g = gb[i % 3]
        gprev = gb[(i - 1) % 3]
        a = acc[i % 2]
        u = ub[i % 3]
        P.copy('act', g[:, 2:514], pss[0])
        if t0 == 0:
            P.memset('pool', g[:, 0:2], 0.0)
        elif t0 % TB == 0:
            P.copy('pool', g[:, 0:2], halo[:, 2 * j:2 * j + 2])
        else:
            P.copy('pool', g[:, 0:2], gprev[:, 512:514])
        if (t0 + 512) % TB == 0:
            P.copy('pool', halo[:, 2 * j:2 * j + 2], g[:, 512:514])
        P.ts('dve', a, g[:, 0:512], cw[:, 3 * j:3 * j + 1], cb[:, j:j + 1], ALU.mult, ALU.add)
        t2 = tmp[i % 2]
        P.ts('pool', t2, g[:, 1:513], cw[:, 3 * j + 1:3 * j + 2], None, ALU.mult)
        P.stt('dve', a, g[:, 2:514], cw[:, 3 * j + 2:3 * j + 3], a, ALU.mult, ALU.add)
        P.tt('pool', a, a, t2, ALU.add)
        P.act(a, a, AF.Silu)
        P.tt('dve', u, a, pss[1], ALU.mult)
        P.dma('act', X.uT[j * 128:(j + 1) * 128, t0:t0 + 512], u)

    gemm(X, X.hT, 16, [X.w_gate[l], X.w_up[l]], KFF, TB, epi)


def phase_rglru(X):
    P, A = X.P, X.A
    P.barrier()
    A.reset()
    wr = A.alloc(8 * 128, BF16)
    wi = A.alloc(8 * 128, BF16)
    load_cast(X, wr, X.a_w_r)
    load_cast(X, wi, X.a_w_i)
    cl = A.alloc(8, F32)
    cl2 = A.alloc(8, F32)
    P.act(cl, X.C('a_lambda'), AF.Exp, scale=-1.0)
    P.ts('dve', cl, cl, 1.0, None, ALU.add)
    P.act(cl, cl, AF.Ln)
    P.ts('dve', cl2, cl, -16.0, None, ALU.mult)
    P.ts('dve', cl, cl, -8.0, None, ALU.mult)
    xa = A.alloc(3 + T, F32)
    ga = A.alloc(T, F32)
    xc = A.alloc(T, F32)
    xcb = A.alloc(T, BF16)
    r = A.alloc(T, F32)
    ig = A.alloc(T, F32)
    a = A.alloc(T, F32)
    s = A.alloc(T, F32)
    yb = A.alloc(T, BF16)
    P.memset('pool', xa[:, 0:3], 0.0)
    cwt = X.C('a_conv_w')
    for c in range(8):
        P.dma('sp', xa[:, 3:3 + T], X.pT[c * 128:(c + 1) * 128, :])
        P.dma('sp', ga, X.pT[(8 + c) * 128:(9 + c) * 128, :])
        P.ts('dve', xc, xa[:, 0:T], cwt[:, 4 * c:4 * c + 1], X.C('a_conv_b')[:, c:c + 1], ALU.mult, ALU.add)
        for j in range(1, 4):
            P.stt('dve', xc, xa[:, j:j + T], cwt[:, 4 * c + j:4 * c + j + 1], xc, ALU.mult, ALU.add)
        P.copy('pool', xcb, xc)
        for it in range(8):
            sl = slice(it * 512, (it + 1) * 512)
            p1 = X.ps[(2 * it) % 6]
            p2 = X.ps[(2 * it + 1) % 6]
            P.mm(p1, wr[:, c * 128:(c + 1) * 128], xcb[:, sl])
            P.mm(p2, wi[:, c * 128:(c + 1) * 128], xcb[:, sl])
            P.act(r[:, sl], p1, AF.Sigmoid, bias=X.C('a_b_r')[:, c:c + 1])
            P.act(ig[:, sl], p2, AF.Sigmoid, bias=X.C('a_b_i')[:, c:c + 1])
        P.act(a, r, AF.Exp, scale=cl[:, c:c + 1])
        P.act(s, r, AF.Exp, scale=cl2[:, c:c + 1])
        P.ts('dve', s, s, -1.0, 1.0, ALU.mult, ALU.add)
        P.act(s, s, AF.Sqrt)
        P.tt('pool', ig, ig, xc, ALU.mult)
        P.tt('dve', s, s, ig, ALU.mult)
        P.scan(r, a, s, 0.0, ALU.mult, ALU.add)
        P.tt('pool', a, ga, ga, ALU.mult)
        P.ts('dve', a, a, 0.044715, 1.0, ALU.mult, ALU.add)
        P.tt('pool', a, a, ga, ALU.mult)
        P.act(a, a, AF.Tanh, scale=0.7978845608028654)
        P.ts('dve', a, a, 1.0, 0.5, ALU.add, ALU.mult)
        P.tt('pool', a, a, ga, ALU.mult)
        P.tt('dve', yb, a, r, ALU.mult)
        P.dma('act', X.yT[c * 128:(c + 1) * 128, :], yb)


def build(stages=('all',), dbg=(), rparts=('prep', 'main', 'post'), rchunks=T // 64):
    nc = bass.Bass("TRN2", target_bir_lowering=False)
    P = Prog(nc)
    X = Ctx()
    X.P = P
    X.rparts = rparts
    import os as _os
    X.rcut = int(_os.environ.get('RCUT', '9'))
    X.rchunks = rchunks
    ncst, offs = const_offsets()

    def dr(name, shape, dtype=F32, kind="Internal"):
        if name in dbg:
            kind = "ExternalOutput"
        return P.dram(name, shape, dtype, kind=kind)

    ein = "ExternalInput"
    X.x0 = dr("xT", [D, T], F32, ein)
    cst_d = dr("cst", [128, ncst], F32, ein)
    X.w_ein = dr("w_ein", [J_EIN, 128, 2048], F32, ein)
    X.w_eout = dr("w_eout", [16, 128, 2048], F32, ein)
    X.w_gate = [dr("w_gate%d" % l, [KFF, 128, 2048], F32, ein) for l in range(2)]
    X.w_up = [dr("w_up%d" % l, [KFF, 128, 2048], F32, ein) for l in range(2)]
    X.w_down = [dr("w_down%d" % l, [16, 128, DFF], F32, ein) for l in range(2)]
    X.w_oin = dr("w_oin", [64, 128, 2048], F32, ein)
    X.w_oout = dr("w_oout", [16, 128, CW], F32, ein)
    X.a_w_r = dr("a_w_r", [128, 1024], F32, ein)
    X.a_w_i = dr("a_w_i", [128, 1024], F32, ein)
    X.wlr = dr("wlr", [128, 1024], F32, ein)
    X.gup1 = dr("gup1", [128, 1024], F32, ein)
    X.gup2 = dr("gup2", [32, 1024], F32, ein)
    X.c_w_if = dr("c_w_if", [128, 3 * 32 * 16], F32, ein)
    X.masks = dr("masks", [128, 128 + 4 * 512], F32, ein)
    X.out = dr("out", [D, T], F32, "ExternalOutput")
    X.hT = dr("hT", [D, T], BF16)
    X.pT = dr("pT", [P_ROWS, T], F32)
    X.yT = dr("yT", [D, T], BF16)
    X.uT = dr("uT", [DFF, T], BF16)
    X.xA = dr("xA", [D, T], F32)
    X.RW = dr("RW", [8, 128, 64 * 320], F32)
    X.gS = dr("gS", [1024, T], F32)
    X.WLS = dr("WLS", [1024, 64], F32)
    X.xmz = dr("xmz", [8192, T], F32)
    X.qT = dr("qT", [CW, T], BF16)
    X.kT = dr("kT", [CW, T], BF16)
    X.q2T = dr("q2T", [CW, T], BF16)
    X.k2T = dr("k2T", [CW, T], BF16)
    X.xcT = dr("xcT", [CW, T], BF16)
    X.vtok = dr("vtok", [T, CW], BF16)
    X.hS = dr("hS", [CW, T], F32)
    X.hsT = dr("hsT", [CW, T], BF16)
    X.mconst = dr("mconst", [128, 1408], F32, ein)
    X.bif_d = dr("bif_d", [16, 1], F32, ein)
    X.rkS = dr("rkS", [1024, T], F32)
    X.vS = dr("vS", [1024, T], F32)
    X.yS = dr("yS", [1024, T], F32)
    X.xB = dr("xB", [D, T], F32)

    A = Arena(P, 50432)
    X.A = A
    X.ps = [P.psum("ps%d" % i) for i in range(8)]
    cst = A.alloc(ncst, F32, "cst")
    P.dma('sp', cst, cst_d)
    X.C = lambda name: cst[:, offs[name][0]:offs[name][0] + offs[name][1]]
    X.ones_bf = A.alloc(128, BF16, "ones")
    P.memset('dve', X.ones_bf, 1.0 / D)
    A.persist()
    X.base0 = A.base

    def on(s):
        return 'all' in stages or s in stages

    if on('e_norm'):
        phase_norm(X, X.x0, 'even_norm', X.hT)
    if on('e_in'):
        phase_even_inproj(X)
    if on('rglru'):
        phase_rglru(X)
    if on('rwkv'):
        phase_rwkv(X)
        A.base = X.base0
    if on('e_out'):
        phase_resid_gemm(X, X.yT, 16, X.w_eout, 2048, X.x0, X.xA)
    if on('ffn0'):
        phase_norm(X, X.xA, 'ffn_norm0', X.hT)
        phase_ffn_up(X, 0)
        phase_resid_gemm(X, X.uT, KFF, X.w_down[0], 1024, X.xA, X.xB)
    if on('mlstm'):
        phase_mlstm(X)
    if on('ffn1'):
        phase_norm(X, X.xA, 'ffn_norm1', X.hT)
        phase_ffn_up(X, 1)
        phase_resid_gemm(X, X.uT, KFF, X.w_down[1], 1024, X.xA, X.xB)
    if on('final'):
        phase_norm(X, X.xB, 'final_norm', X.out, out_f32=True)
    P.finish()
    X.stats = P.stats
    return nc, X


def host_masks():
    i = np.arange(128)[:, None]
    t = np.arange(64)[None, :]
    ident = (np.arange(128)[:, None] == np.arange(128)[None, :]).astype(np.float32)
    mS = np.tile((t > i).astype(np.float32), (1, 8))
    mI = np.tile((t >= i).astype(np.float32), (1, 8))
    mL = np.tile((i > t).astype(np.float32), (1, 8))
    idb = np.tile((t == i).astype(np.float32), (1, 8))
    return np.ascontiguousarray(np.concatenate([ident, mS, mI, mL, idb], axis=1))


def host_mconst():
    p = np.arange(128)[:, None]
    c = np.arange(128)[None, :]
    mbd = ((p // 4) == (c // 4)).astype(np.float32)
    maskC = (p <= c).astype(np.float32)
    ident = (p == c).astype(np.float32)
    sel = np.zeros((128, 1024), np.float32)
    for h in range(8):
        sel[h, h * 128:(h + 1) * 128] = 1.0
    return np.ascontiguousarray(np.concatenate([mbd, maskC, ident, sel], axis=1))


def host_inputs(inp):
    cst, _ = pack_consts(inp)
    sh = {
        'cst': cst,
        'masks': host_masks(),
        'mconst': host_mconst(),
        'bif_d': np.ascontiguousarray(inp['c_b_if'][0].reshape(16, 1)),
        'w_ein': _tile_w(inp['even_w_in'][0], J_EIN),
        'w_eout': _tile_w(inp['even_w_out'][0], 16),
        'w_oin': _tile_w(inp['odd_w_in'][0], 64),
        'w_oout': _tile_w(inp['odd_w_out'][0], 16),
        'a_w_r': np.ascontiguousarray(inp['a_w_r'][0].transpose(1, 0, 2).reshape(128, 1024)),
        'a_w_i': np.ascontiguousarray(inp['a_w_i'][0].transpose(1, 0, 2).reshape(128, 1024)),
        'wlr': np.ascontiguousarray(np.concatenate([inp['b_w_up'][0], inp['b_a_up'][0]], axis=0)),
        'gup1': np.ascontiguousarray(inp['b_g_up'][0][:128]),
        'gup2': np.ascontiguousarray(inp['b_g_up'][0][128:160]),
        'c_w_if': np.ascontiguousarray(inp['c_w_if'][0].reshape(3, 32, 128, 16).transpose(2, 0, 1, 3).reshape(128, 1536)),
    }
    for l in range(2):
        sh['w_gate%d' % l] = _tile_w(inp['ffn_w_gate'][l], KFF)
        sh['w_up%d' % l] = _tile_w(inp['ffn_w_up'][l], KFF)
        sh['w_down%d' % l] = _tile_w(inp['ffn_w_down'][l], 16)
    return sh


def kernel(**inputs):
    inp = {k: np.asarray(v) for k, v in inputs.items()}
    x = inp['x']
    B = x.shape[0]
    nc, X = build()
    sh = host_inputs(inp)
    in_maps = []
    for b in range(B):
        m = dict(sh)
        m['xT'] = np.ascontiguousarray(x[b].T)
        in_maps.append(m)
    res = run_bass_kernel_spmd(nc, in_maps, core_ids=list(range(B)))
    out = np.stack([np.ascontiguousarray(r['out'].T) for r in res.results], axis=0)
    return out.astype(np.float32)


def _c3(v, l=64):
    return v.re("p (c l) -> p c l", l=l)


def phase_rwkv(X):
    P, A = X.P, X.A
    P.barrier()
    A.reset()
    C = X.C
    TT = 1024
    NCH = TT // 64
    wlr = A.alloc(1024, BF16)
    gu1 = A.alloc(1024, BF16)
    gu2 = A.alloc(1024, BF16)
    BO = A.alloc(128, F32)
    BO64 = A.alloc(128, F32)
    for (t_, val) in ((BO, 1.0), (BO64, 1.0 / 64)):
        P.memset('dve', t_, 0.0)
        P.memset('dve', t_[0:64, 0:64], val)
        P.memset('dve', t_[64:128, 64:128], val)
    WL = A.alloc(8 * 64, F32)
    omk = A.alloc(8, F32)
    P.ts('dve', omk, C('b_k_a'), -1.0, 1.0, ALU.mult, ALU.add)
    A.persist()
    load_cast(X, wlr, X.wlr)
    load_cast(X, gu1, X.gup1)
    load_cast(X, gu2, X.gup2, rows=32)
    mask = A.alloc(TT, F32)
    P.memset('dve', mask, 1.0)
    P.memset('dve', _c3(mask)[:, :, 0:1], 0.0)
    xbs = [A.alloc(TT + 1, F32) for _ in range(4)]
    x4 = A.alloc(TT, F32)
    rr = A.alloc(TT, F32)
    k0 = A.alloc(TT, F32)
    vv = A.alloc(TT, F32)
    xwa = A.alloc(TT, BF16)
    sg1 = A.alloc(TT, BF16)
    sg2 = A.alloc(TT, BF16)
    sig, aic, gst, kk, sq, rn, km, bv, lw, cw, cwx, e1, e2, e3, rk = [A.alloc(TT, F32) for _ in range(15)]
    O = A.alloc(NCH * 320, F32)
    Ov = O.re("p (c q l) -> p c q l", q=5, l=64)
    mu = C('b_mu')
    pc = [0]

    def nps():
        pc[0] += 1
        return X.ps[pc[0] % 8]

    def lerp(dst, xb, chunk, nrows, t0):
        r0 = chunk * 128
        if t0 == 0:
            P.memset('pool', xb[0:nrows, 0:1], 0.0)
            P.dma('sp', xb[0:nrows, 1:1 + TT], X.pT[r0:r0 + nrows, t0:t0 + TT])
        else:
            P.dma('sp', xb[0:nrows, 0:1 + TT], X.pT[r0:r0 + nrows, t0 - 1:t0 + TT])
        P.tt('pool', dst[0:nrows], xb[0:nrows, 0:TT], xb[0:nrows, 1:1 + TT], ALU.subtract)
        P.stt('dve', dst[0:nrows], dst[0:nrows], mu[0:nrows, chunk - 16:chunk - 15], xb[0:nrows, 1:1 + TT],
              ALU.mult, ALU.add)

    for tt in (range(T // TT) if 'prep' in X.rparts else []):
        t0 = tt * TT
        lerp(x4, xbs[0], 40, 128, t0)
        P.act(xwa[0:64], x4[0:64], AF.Tanh)
        P.copy('pool', xwa[64:128], x4[64:128])
        lerp(x4, xbs[0], 41, 128, t0)
        P.act(sg1, x4, AF.Sigmoid)
        lerp(x4, xbs[0], 42, 32, t0)
        P.act(sg2[0:32], x4[0:32], AF.Sigmoid)
        for hp in range(8):
            hc = slice(hp * 128, (hp + 1) * 128)
            h1 = slice(hp, hp + 1)
            lerp(rr, xbs[1], 16 + hp, 128, t0)
            lerp(k0, xbs[2], 24 + hp, 128, t0)
            lerp(vv, xbs[3], 32 + hp, 128, t0)
            for sub in range(TT // 512):
                sl = slice(sub * 512, (sub + 1) * 512)
                pz = nps()
                P.mm(pz, wlr[0:64, hc], xwa[0:64, sl])
                P.act(sig[:, sl], pz, AF.Sigmoid, bias=C('b_w0')[:, h1])
                pa = nps()
                P.mm(pa, wlr[64:128, hc], xwa[64:128, sl])
                P.act(aic[:, sl], pa, AF.Sigmoid, bias=C('b_a0')[:, h1])
                pg = nps()
                P.mm(pg, gu1[:, hc], sg1[:, sl], start=True, stop=False)
                P.mm(pg, gu2[0:32, hc], sg2[0:32, sl], start=False, stop=True)
                P.copy('act', gst[:, sl], pg)
            P.dma('act', X.gS[hc, t0:t0 + TT], gst)
            P.ts('dve', kk, k0, C('b_k_k')[:, h1], None, ALU.mult)
            P.tt('pool', sq, kk, kk, ALU.mult)
            for sub in range(TT // 512):
                sl = slice(sub * 512, (sub + 1) * 512)
                pq = nps()
                P.mm(pq, BO, sq[:, sl])
                P.ts('dve', rn[:, sl], pq, 1e-12, None, ALU.max)
            P.act(rn, rn, AF.Ln)
            P.act(rn, rn, AF.Exp, scale=-0.5)
            P.tt('dve', kk, kk, rn, ALU.mult)
            P.ts('dve', km, aic, C('b_k_a')[:, h1], omk[:, h1], ALU.mult, ALU.add)
            P.tt('pool', km, km, k0, ALU.mult)
            P.tt('pool', bv, kk, aic, ALU.mult)
            P.ts('dve', lw, sig, -0.6065306597126334, None, ALU.mult)
            P.scan(cw, mask, lw, 0.0, ALU.mult, ALU.add)
            P.tt('pool', cwx, cw, lw, ALU.subtract)
            P.act(e1, cw, AF.Exp)
            P.act(e2, cwx, AF.Exp)
            P.act(e3, cw, AF.Exp, scale=-1.0)
            P.stt('dve', Ov[:, :, 0, :], _c3(kk), -1.0, _c3(e2), ALU.mult, ALU.mult)
            P.tt('pool', Ov[:, :, 1, :], _c3(rr), _c3(e1), ALU.mult)
            P.tt('dve', Ov[:, :, 2, :], _c3(bv), _c3(e3), ALU.mult)
            P.tt('pool', Ov[:, :, 3, :], _c3(km), _c3(e3), ALU.mult)
            P.copy('act', Ov[:, :, 4, :], _c3(vv))
            wlt = WL[:, hp * 64 + tt * NCH:hp * 64 + (tt + 1) * NCH]
            P.copy('pool', wlt, e1[:, 63:TT:64])
            P.dma('act', X.WLS[hc, tt * NCH:(tt + 1) * NCH], wlt)
            P.tt('dve', rk, rr, km, ALU.mult)
            P.ts('dve', rk, rk, C('b_r_k')[:, h1], None, ALU.mult)
            P.dma('act', X.rkS[hc, t0:t0 + TT], rk)
            P.dma('act', X.vS[hc, t0:t0 + TT], vv)
            P.dma('act', X.RW[hp, :, tt * NCH * 320:(tt + 1) * NCH * 320], O)

    P.barrier()
    A.reset()
    mk = A.alloc(128 + 4 * 512, F32)
    P.dma('sp', mk, X.masks)
    ident = mk[:, 0:128]
    mS = mk[:, 128:640]
    mI = mk[:, 640:1152]
    mL = mk[:, 1152:1664]
    idb = mk[:, 1664:2176]
    NB = 2
    idn = ident[0:64, 0:64]
    WLh = A.alloc(16 * 64, F32)
    P.dma('sp', WLh[0:64].re("p (h c) -> p h c", c=64), X.WLS.re("(h p) c -> p h c", p=64))
    blk = [[A.alloc(NB * 320, F32) for _ in range(16)] for _ in range(2)]
    G = []
    for g in range(2):
        d = Ctx()
        d.vtk, d.btk, d.ktk, d.AabT, d.ArbT, d.AakT, d.ArkT, d.Aab, d.Z, d.U = \
            [A.alloc(512, F32) for _ in range(10)]
        d.Xs = [A.alloc(512, F32) for _ in range(2)]
        d.XTs = [A.alloc(512, F32) for _ in range(2)]
        d.PTs = [A.alloc(512, F32) for _ in range(2)]
        d.S = A.alloc(512, F32)
        P.memset('pool', d.S, 0.0)
        d.yb = [A.alloc(8 * NB * 64, F32) for _ in range(2)]
        d.pc = 0
        G.append(d)
    ySv = X.yS.re("(h p) t -> p h t", p=64)

    def hsl(h):
        return slice(h * 64, (h + 1) * 64)

    for c in (range(X.rchunks) if 'main' in X.rparts else []):
        tb, cc = divmod(c, NB)
        if cc == 0:
            for hh in range(16):
                hp, m = divmod(hh, 2)
                P.dma('sp', blk[tb % 2][hh][0:64], X.RW[hp, m * 64:(m + 1) * 64, tb * NB * 320:(tb + 1) * NB * 320])
        for g in range(2):
            d = G[g]

            def nb():
                d.pc += 1
                return X.ps[g * 4 + d.pc % 4]

            def opnd(h, q):
                return blk[tb % 2][g * 8 + h][0:64].re("p (c q l) -> p c q l", q=5, l=64)[:, cc, q, :]

            for (q, dst, eng) in ((4, d.vtk, 'act'), (2, d.btk, 'dve'), (3, d.ktk, 'act')):
                pt = nb()
                for h in range(8):
                    P.mm(pt[0:64, hsl(h)], opnd(h, q), idn)
                P.copy(eng, dst[0:64], pt[0:64])
            if X.rcut < 1:
                continue
            specs = ((2, 0, d.AabT, mS, 'dve'), (2, 1, d.ArbT, mI, 'pool'), (3, 0, d.AakT, mS, 'dve'),
                     (3, 1, d.ArkT, mI, 'pool'), (0, 2, d.Aab, mL, 'dve'))
            for (ql, qr, dst, mk_, eng) in specs:
                pa = nb()
                for h in range(8):
                    P.mm(pa[0:64, hsl(h)], opnd(h, ql), opnd(h, qr))
                if eng == 'dve':
                    P.tt('dve', dst[0:64], pa[0:64], mk_[0:64], ALU.mult)
                else:
                    P.copy('act', dst[0:64], pa[0:64])
                    P.tt('pool', dst[0:64], dst[0:64], mk_[0:64], ALU.mult)
            if X.rcut < 2:
                continue
            pz = nb()
            for h in range(8):
                P.mm(pz[0:64, hsl(h)], opnd(h, 0), d.S[0:64, hsl(h)], start=True, stop=False)
                P.mm(pz[0:64, hsl(h)], d.AakT[0:64, hsl(h)], d.vtk[0:64, hsl(h)], start=False, stop=True)
            P.copy('act', d.Z[0:64], pz[0:64])
            if X.rcut < 3:
                continue
            Xc, XTc = d.Aab, d.AabT
            PTc = d.PTs[0]
            P.tt('pool', PTc[0:64], d.AabT[0:64], idb[0:64], ALU.add)
            for lev in range(5):
                Xn, XTn, PTn = d.Xs[lev % 2], d.XTs[lev % 2], d.PTs[(lev + 1) % 2]
                px = nb()
                for h in range(8):
                    P.mm(px[0:64, hsl(h)], XTc[0:64, hsl(h)], Xc[0:64, hsl(h)])
                if lev < 4:
                    pxt = nb()
                    for h in range(8):
                        P.mm(pxt[0:64, hsl(h)], Xc[0:64, hsl(h)], XTc[0:64, hsl(h)])
                P.copy('act', Xn[0:64], px[0:64])
                if lev < 4:
                    P.copy('dve', XTn[0:64], pxt[0:64])
                pp_ = nb()
                for h in range(8):
                    P.mm(pp_[0:64, hsl(h)], Xn[0:64, hsl(h)], PTc[0:64, hsl(h)])
                P.tt('dve', PTn[0:64], PTc[0:64], pp_[0:64], ALU.add)
                Xc, XTc, PTc = Xn, XTn, PTn
            if X.rcut < 4:
                continue
            pu = nb()
            for h in range(8):
                P.mm(pu[0:64, hsl(h)], PTc[0:64, hsl(h)], d.Z[0:64, hsl(h)])
            P.copy('act', d.U[0:64], pu[0:64])
            if X.rcut < 5:
                continue
            py = nb()
            for h in range(8):
                o = py[0:64, hsl(h)]
                P.mm(o, d.S[0:64, hsl(h)], opnd(h, 1), start=True, stop=False)
                P.mm(o, d.U[0:64, hsl(h)], d.ArbT[0:64, hsl(h)], start=False, stop=False)
                P.mm(o, d.vtk[0:64, hsl(h)], d.ArkT[0:64, hsl(h)], start=False, stop=True)
            yb = d.yb[tb % 2]
            P.copy('act', yb[0:64].re("p (a t) -> p a t", t=NB * 64)[:, :, cc * 64:(cc + 1) * 64],
                   py[0:64].re("p (a t) -> p a t", t=64))
            if X.rcut < 6:
                continue
            pS = nb()
            for h in range(8):
                o = pS[0:64, hsl(h)]
                P.mm(o, d.btk[0:64, hsl(h)], d.U[0:64, hsl(h)], start=True, stop=False)
                P.mm(o, d.ktk[0:64, hsl(h)], d.vtk[0:64, hsl(h)], start=False, stop=True)
            P.tt('dve', d.S[0:64], d.S[0:64], pS[0:64], ALU.add)
            wl = WLh[0:64].re("p (h c) -> p h c", c=64)[:, g * 8:(g + 1) * 8, c:c + 1]
            P.tt('pool', _c3(d.S[0:64]), _c3(d.S[0:64]), V(wl.ap.broadcast_to([64, 8, 64]), wl.buf), ALU.mult)
            if cc == NB - 1:
                P.dma('act', ySv[:, g * 8:(g + 1) * 8, tb * NB * 64:(tb + 1) * NB * 64],
                      yb[0:64].re("p (a t) -> p a t", t=NB * 64))

    P.barrier()
    A.reset()
    ys, rks, vs_, gs, dd, s2, t1 = [[A.alloc(512, F32) for _ in range(2)] for _ in range(7)]
    ob = [A.alloc(512, BF16) for _ in range(2)]
    it = 0
    for hp in (range(8) if 'post' in X.rparts else []):
        hc = slice(hp * 128, (hp + 1) * 128)
        h1 = slice(hp, hp + 1)
        for ti in range(T // 512):
            tsl = slice(ti * 512, (ti + 1) * 512)
            i2 = it % 2
            it += 1
            y, rk_, v_, g_, d_, q_, t_ = ys[i2], rks[i2], vs_[i2], gs[i2], dd[i2], s2[i2], t1[i2]
            P.dma('sp', y, X.yS[hc, tsl])
            P.dma('sp', rk_, X.rkS[hc, tsl])
            P.dma('sp', v_, X.vS[hc, tsl])
            P.dma('sp', g_, X.gS[hc, tsl])
            p1 = nps()
            P.mm(p1, BO64, y)
            P.tt('dve', d_, y, p1, ALU.subtract)
            P.tt('pool', q_, d_, d_, ALU.mult)
            p2 = nps()
            P.mm(p2, BO64, q_)
            P.ts('dve', q_, p2, B_LN_EPS, None, ALU.add)
            P.act(q_, q_, AF.Ln)
            P.act(q_, q_, AF.Exp, scale=-0.5)
            P.tt('dve', d_, d_, q_, ALU.mult)
            P.ts('dve', d_, d_, C('b_ln_w')[:, h1], C('b_ln_b')[:, h1], ALU.mult, ALU.add)
            p3 = nps()
            P.mm(p3, BO, rk_)
            P.tt('dve', t_, v_, p3, ALU.mult)
            P.tt('pool', d_, d_, t_, ALU.add)
            P.tt('pool', ob[i2], d_, g_, ALU.mult)
            P.dma('act', X.yT[1024 + hp * 128:1024 + (hp + 1) * 128, tsl], ob[i2])


def phase_mlstm(X):
    P, A = X.P, X.A
    C = X.C
    xin, xout = X.xB, X.xA
    phase_norm(X, xin, 'odd_norm', X.hT)
    P.barrier()
    A.reset()
    st = [A.alloc(512, F32) for _ in range(4)]
    cn = [0]

    def epi(j, t0, pss):
        s = st[cn[0] % 4]
        if cn[0] % 2 == 0:
            P.copy('act', s, pss[0])
        else:
            P.copy('dve', s, pss[0])
        cn[0] += 1
        P.dma('act', X.xmz[j * 128:(j + 1) * 128, t0:t0 + 512], s)

    gemm(X, X.hT, 16, [X.w_oin], 64, 2048, epi)
    P.barrier()
    A.reset()
    mc = A.alloc(128 + 128 + 128 + 1024, F32)
    P.dma('sp', mc, X.mconst)
    mbd, maskC, identf, sel = mc[:, 0:128], mc[:, 128:256], mc[:, 256:384], mc[:, 384:1408]
    wif = A.alloc(1536, BF16)
    wifv = wif.re("p (j c g) -> p j c g", j=3, c=32)
    gI = A.alloc(T, F32)
    gF = A.alloc(T, F32)
    P.memset('pool', gI, 0.0)
    P.memset('pool', gF, 0.0)
    A.persist()
    load_cast(X, wif, X.c_w_if)
    xm = A.alloc(3 + T, F32)
    xc = A.alloc(T, F32)
    xmb = A.alloc(T, BF16)
    xcb = A.alloc(T, BF16)
    qb = A.alloc(T, BF16)
    kb = A.alloc(T, BF16)
    vb = A.alloc(T, BF16)
    vt = [A.alloc(512, BF16) for _ in range(2)]
    bd = [A.alloc(128, BF16) for _ in range(3)]
    P.memset('pool', xm[:, 0:3], 0.0)
    cwt = C('c_conv_w')
    pc = [0]

    def nps():
        pc[0] += 1
        return X.ps[pc[0] % 8]

    vtv = X.vtok.re("(b p) f -> p b f", p=128)
    for c in range(32):
        P.dma('sp', xm[:, 3:3 + T], X.xmz[c * 128:(c + 1) * 128, :])
        P.ts('dve', xc, xm[:, 0:T], cwt[:, 4 * c:4 * c + 1], C('c_conv_b')[:, c:c + 1], ALU.mult, ALU.add)
        for j in range(1, 4):
            P.stt('dve', xc, xm[:, j:j + T], cwt[:, 4 * c + j:4 * c + j + 1], xc, ALU.mult, ALU.add)
        P.act(xc, xc, AF.Silu)
        P.copy('pool', xcb, xc)
        P.copy('pool', xmb, xm[:, 3:3 + T])
        for i, nm in enumerate(('c_w_q', 'c_w_k', 'c_w_v')):
            w4 = C(nm)[:, 4 * c:4 * c + 4]
            wb_ = V(w4.ap.unsqueeze(1).broadcast_to([128, 32, 4]), w4.buf)
            P.tt('dve', bd[i].re("p (g j) -> p g j", j=4), mbd.re("p (g j) -> p g j", j=4), wb_, ALU.mult)
        for it in range(8):
            sl = slice(it * 512, (it + 1) * 512)
            p1 = nps()
            P.mm(p1, bd[0], xcb[:, sl])
            P.copy('act', qb[:, sl], p1)
            p2 = nps()
            P.mm(p2, bd[1], xcb[:, sl])
            P.copy('act', kb[:, sl], p2)
            p3 = nps()
            P.mm(p3, bd[2], xmb[:, sl])
            P.copy('dve', vb[:, sl], p3)
            p4 = nps()
            for b4 in range(4):
                tb2 = it * 4 + b4
                P.mm(p4[:, b4 * 128:(b4 + 1) * 128], xmb[:, tb2 * 128:(tb2 + 1) * 128], bd[2])
            v_ = vt[it % 2]
            P.copy('act', v_, p4)
            P.dma('act', vtv[:, it * 4:(it + 1) * 4, c * 128:(c + 1) * 128], v_.re("p (b f) -> p b f", f=128))
            for (gt, lo) in ((gI, 0), (gF, 8)):
                pg = nps()
                P.mm(pg[0:8, :], wifv[:, 0, c, lo:lo + 8], qb[:, sl], start=True, stop=False)
                P.mm(pg[0:8, :], wifv[:, 1, c, lo:lo + 8], kb[:, sl], start=False, stop=False)
                P.mm(pg[0:8, :], wifv[:, 2, c, lo:lo + 8], vb[:, sl], start=False, stop=True)
                P.tt('dve', gt[0:8, sl], gt[0:8, sl], pg[0:8, :], ALU.add)
        P.dma('act', X.qT[c * 128:(c + 1) * 128, :], qb)
        P.dma('act', X.kT[c * 128:(c + 1) * 128, :], kb)
        P.dma('act', X.xcT[c * 128:(c + 1) * 128, :], xcb)
    P.barrier()
    A.reset()
    CH = 128
    NCk = T // CH
    m8 = A.alloc(T, F32)
    P.memset('dve', m8, 1.0)
    P.memset('dve', _c3(m8, CH)[:, :, 0:1], 0.0)
    bif = C('c_b_if')
    bF = A.alloc(1, F32)
    P.dma('sp', bF[0:8, :], X.bif_d[8:16, :])
    lf = A.alloc(T, F32)
    bb = A.alloc(T, F32)
    aa = A.alloc(T, F32)
    eb = A.alloc(T, F32)
    ea = A.alloc(T, F32)
    dec = A.alloc(NCk, F32)
    P.act(lf[0:8], gF[0:8], AF.Sigmoid, bias=bF[0:8, 0:1])
    P.act(lf[0:8], lf[0:8], AF.Ln)
    P.scan(bb[0:8], m8[0:8], lf[0:8], 0.0, ALU.mult, ALU.add)
    P.ts('dve', aa[0:8], gI[0:8], bif[0:8, 0:1], None, ALU.add)
    P.tt('dve', aa[0:8], aa[0:8], bb[0:8], ALU.subtract)
    P.act(eb[0:8], bb[0:8], AF.Exp)
    P.act(ea[0:8], aa[0:8], AF.Exp)
    P.copy('dve', dec[0:8], eb[0:8, CH - 1:T:CH])
    decb = A.alloc(8 * NCk, F32)
    for h in range(8):
        pd = nps()
        P.mm(pd[:, 0:NCk], sel[0:8, h * 128:(h + 1) * 128], dec[0:8, :])
        P.copy('act', decb[:, h * NCk:(h + 1) * NCk], pd[:, 0:NCk])
    identb = A.alloc(128, BF16)
    P.copy('dve', identb, identf)
    onesb = A.alloc(128, BF16)
    P.memset('dve', onesb, 1.0)
    A.persist()
    ebb = [A.alloc(512, F32) for _ in range(2)]
    eab = [A.alloc(512, F32) for _ in range(2)]
    qk = [A.alloc(512, BF16) for _ in range(4)]
    n = 0
    for h in range(8):
        for it in range(8):
            sl = slice(it * 512, (it + 1) * 512)
            e1_, e2_ = ebb[it % 2], eab[it % 2]
            p1 = nps()
            P.mm(p1, sel[0:8, h * 128:(h + 1) * 128], eb[0:8, sl])
            P.copy('act', e1_, p1)
            p2 = nps()
            P.mm(p2, sel[0:8, h * 128:(h + 1) * 128], ea[0:8, sl])
            P.copy('act', e2_, p2)
            for kc in range(4):
                rows = slice(h * 512 + kc * 128, h * 512 + (kc + 1) * 128)
                t1_, t2_ = qk[n % 4], qk[(n + 1) % 4]
                n += 2
                P.dma('sp', t1_, X.qT[rows, sl])
                P.dma('sp', t2_, X.kT[rows, sl])
                P.tt('dve', t1_, t1_, e1_, ALU.mult)
                P.stt('dve', t2_, t2_, 512.0 ** -0.5, e2_, ALU.mult, ALU.mult)
                P.dma('act', X.q2T[rows, sl], t1_)
                P.dma('act', X.k2T[rows, sl], t2_)
    P.barrier()
    A.reset()
    CT = A.alloc(2048, F32)
    CTb = A.alloc(2048, BF16)
    nb_ = A.alloc(512, F32)
    nbb = A.alloc(512, BF16)
    qt = [A.alloc(512, BF16) for _ in range(2)]
    kt = [A.alloc(512, BF16) for _ in range(2)]
    vk = [A.alloc(512, BF16) for _ in range(2)]
    ktok = [A.alloc(512, BF16) for _ in range(2)]
    STb = [A.alloc(128, BF16) for _ in range(2)]
    rec = [A.alloc(128, F32) for _ in range(2)]
    ho = [A.alloc(512, F32) for _ in range(2)]
    tmpc = A.alloc(512, F32)
    vtr = X.vtok
    for h in range(8):
        P.memset('pool', CT, 0.0)
        P.memset('pool', CTb, 0.0)
        P.memset('pool', nb_, 0.0)
        P.memset('pool', nbb, 0.0)
        for c in range(NCk):
            i2 = c % 2
            tsl = slice(c * CH, (c + 1) * CH)
            q_, k_, v_, kk_, S_, r_, o_ = qt[i2], kt[i2], vk[i2], ktok[i2], STb[i2], rec[i2], ho[i2]
            P.dma('sp', q_.re("p (k t) -> p k t", t=CH), X.q2T.re("(k p) t -> p k t", p=128)[:, h * 4:(h + 1) * 4, tsl])
            P.dma('sp', k_.re("p (k t) -> p k t", t=CH), X.k2T.re("(k p) t -> p k t", p=128)[:, h * 4:(h + 1) * 4, tsl])
            P.dma('sp', v_, vtr[tsl, h * 512:(h + 1) * 512])
            pk = nps()
            for kc in range(4):
                P.mm(pk[:, kc * 128:(kc + 1) * 128], k_[:, kc * 128:(kc + 1) * 128], identb)
            P.copy('act', kk_, pk)
            ps_ = nps()
            for kc in range(4):
                P.mm(ps_[:, 0:128], k_[:, kc * 128:(kc + 1) * 128], q_[:, kc * 128:(kc + 1) * 128],
                     start=(kc == 0), stop=(kc == 3))
            P.tt('dve', S_, ps_[:, 0:128], maskC, ALU.mult)
            pn = nps()
            for vc in range(4):
                o = pn[:, vc * 128:(vc + 1) * 128]
                P.mm(o, v_[:, vc * 128:(vc + 1) * 128], S_, start=True, stop=False)
                for kc in range(4):
                    P.mm(o, CTb[:, kc * 512 + vc * 128:kc * 512 + (vc + 1) * 128], q_[:, kc * 128:(kc + 1) * 128],
                         start=False, stop=(kc == 3))
            pdn = nps()
            P.mm(pdn[:, 0:128], onesb, S_, start=True, stop=False)
            for kc in range(4):
                P.mm(pdn[:, 0:128], nbb[:, kc * 128:(kc + 1) * 128], q_[:, kc * 128:(kc + 1) * 128],
                     start=False, stop=(kc == 3))
            P.act(r_, pdn[:, 0:128], AF.Abs)
            P.ts('dve', r_, r_, 1.0, None, ALU.max)
            P.recip(r_, r_)
            P.tt('dve', o_.re("p (a t) -> p a t", t=128), pn.re("p (a t) -> p a t", t=128),
                 V(r_.ap.unsqueeze(1).broadcast_to([128, 4, 128]), r_.buf), ALU.mult)
            P.dma('act', X.hS.re("(k p) t -> p k t", p=128)[:, h * 4:(h + 1) * 4, tsl], o_.re("p (a t) -> p a t", t=128))
            dcol = decb[:, h * NCk + c:h * NCk + c + 1]
            for kc in range(4):
                pc_ = nps()
                P.mm(pc_, kk_[:, kc * 128:(kc + 1) * 128], v_)
                csl = slice(kc * 512, (kc + 1) * 512)
                P.tt('dve', tmpc, CT[:, csl], pc_, ALU.add)
                P.ts('pool', CT[:, csl], tmpc, dcol, None, ALU.mult)
                P.copy('act', CTb[:, csl], CT[:, csl])
            pnn = nps()
            for kc in range(4):
                P.mm(pnn[:, kc * 128:(kc + 1) * 128], kk_[:, kc * 128:(kc + 1) * 128], onesb)
            P.tt('dve', tmpc, nb_, pnn, ALU.add)
            P.ts('pool', nb_, tmpc, dcol, None, ALU.mult)
            P.copy('act', nbb, nb_)
    P.barrier()
    A.base = X.base0
    A.reset()
    o512 = A.alloc(128, F32)
    P.memset('dve', o512, 1.0 / 512)
    hb = [A.alloc(2048, F32) for _ in range(2)]
    dq = [A.alloc(2048, F32) for _ in range(2)]
    rs_ = [A.alloc(512, F32) for _ in range(2)]
    zb = [A.alloc(512, F32) for _ in range(2)]
    xcl = [A.alloc(512, BF16) for _ in range(2)]
    xcf = [A.alloc(512, F32) for _ in range(2)]
    ob = [A.alloc(512, BF16) for _ in range(2)]
    n = 0
    for h in range(8):
        for it in range(8):
            sl = slice(it * 512, (it + 1) * 512)
            i2 = n % 2
            n += 1
            hb_, d_, r_ = hb[i2], dq[i2], rs_[i2]
            P.dma('sp', hb_.re("p (k t) -> p k t", t=512), X.hS.re("(k p) t -> p k t", p=128)[:, h * 4:(h + 1) * 4, sl])
            pm = nps()
            for vc in range(4):
                P.mm(pm, o512, hb_[:, vc * 512:(vc + 1) * 512], start=(vc == 0), stop=(vc == 3))
            for vc in range(4):
                P.tt('dve', d_[:, vc * 512:(vc + 1) * 512], hb_[:, vc * 512:(vc + 1) * 512], pm, ALU.subtract)
            P.tt('pool', hb_, d_, d_, ALU.mult)
            pv = nps()
            for vc in range(4):
                P.mm(pv, o512, hb_[:, vc * 512:(vc + 1) * 512], start=(vc == 0), stop=(vc == 3))
            P.ts('dve', r_, pv, EPS, None, ALU.add)
            P.act(r_, r_, AF.Ln)
            P.act(r_, r_, AF.Exp, scale=-0.5)
            for vc in range(4):
                cidx = h * 4 + vc
                rows = slice(cidx * 128, (cidx + 1) * 128)
                j2 = (n * 4 + vc) % 2
                P.stt('dve', d_[:, vc * 512:(vc + 1) * 512], d_[:, vc * 512:(vc + 1) * 512], C('c_ln_w')[:, cidx:cidx + 1],
                      r_, ALU.mult, ALU.mult)
                P.dma('sp', xcl[j2], X.xcT[rows, sl])
                P.dma('sp', zb[j2], X.xmz[4096 + cidx * 128:4096 + (cidx + 1) * 128, sl])
                P.copy('pool', xcf[j2], xcl[j2])
                P.stt('dve', xcf[j2], xcf[j2], C('c_skip')[:, cidx:cidx + 1], d_[:, vc * 512:(vc + 1) * 512], ALU.mult, ALU.add)
                P.act(zb[j2], zb[j2], AF.Silu)
                P.tt('pool', ob[j2], xcf[j2], zb[j2], ALU.mult)
                P.dma('act', X.hsT[rows, sl], ob[j2])
    A.base = X.base0
    phase_resid_gemm(X, X.hsT, 32, X.w_oout, 1024, xin, xout)
```

```python
from contextlib import ExitStack
import numpy as np
import concourse.bass as bass
import concourse.mybir as mybir
from concourse.bass_utils import run_bass_kernel_spmd

F32 = mybir.dt.float32
BF16 = mybir.dt.bfloat16
AF = mybir.ActivationFunctionType
ALU = mybir.AluOpType
AX = mybir.AxisListType

ENGS = ('pe', 'act', 'dve', 'pool', 'sp')


class Buf:
    __slots__ = ('name', 'kind', 'lw', 'rd', 'semw', 'semr', 'cw', 'cr')

    def __init__(self, name, kind):
        self.name = name
        self.kind = kind
        self.lw = None
        self.rd = {}
        self.semw = None
        self.semr = None
        self.cw = 0
        self.cr = 0


class V:
    __slots__ = ('ap', 'buf')

    def __init__(self, ap, buf):
        self.ap = ap
        self.buf = buf

    def __getitem__(self, idx):
        return V(self.ap[idx], self.buf)

    def re(self, pattern, **kw):
        return V(self.ap.rearrange(pattern, **kw), self.buf)

    def sub(self, name_unused, idx):
        return V(self.ap[idx], self.buf)


def _ap(x):
    return x.ap if isinstance(x, V) else x


class Prog:
    def __init__(self, nc):
        self.nc = nc
        self.stack = ExitStack()
        self.ins = []
        self.nsem = 0
        self.npsum = 0
        self.last_eng = {}
        self.last_dma = {}
        self.bar = set()
        self.sem_pool = []
        self.sem_active = []

    def dram(self, name, shape, dtype, kind="Internal"):
        t = self.nc.dram_tensor(name, list(shape), dtype, kind=kind)
        return V(t.ap(), Buf(name, 'dram'))

    def sbuf(self, name, shape, dtype, nbuf=None):
        t = self.stack.enter_context(self.nc.sbuf_tensor(name, list(shape), dtype))
        return V(t[:], Buf(name, 'sbuf'))

    def psum(self, name, shape=(128, 512), dtype=F32):
        t = self.stack.enter_context(self.nc.psum_tensor(name, list(shape), dtype))
        return V(t[:], Buf(name, 'psum'))

    def view(self, v, name):
        return V(v.ap, Buf(name, v.buf.kind))

    def _sem(self, name):
        self.nsem += 1
        return self.stack.enter_context(self.nc.semaphore(name))

    def emit(self, eng, fn, reads, writes, dma=None):
        iid = len(self.ins)
        deps = set(self.bar)
        wb = []
        for v in writes:
            b = v.buf
            if b in wb:
                continue
            wb.append(b)
            if b.lw is not None:
                deps.add(b.lw)
            deps.update(b.rd.values())
        rb = []
        for v in reads:
            if not isinstance(v, V):
                continue
            b = v.buf
            if b in wb or b in rb:
                continue
            rb.append(b)
            if b.lw is not None:
                deps.add(b.lw)
        key = eng
        dsem = None
        if dma is not None:
            kind, sb = dma
            if kind == 'w':
                if sb.semw is None:
                    sb.semw, sb.cw = self._take_sem("dw_" + sb.name)
                    self.sem_active.append((sb, 'w'))
                sb.cw += 16
                dsem = (sb.semw, sb.cw)
            else:
                if sb.semr is None:
                    sb.semr, sb.cr = self._take_sem("dr_" + sb.name)
                    self.sem_active.append((sb, 'r'))
                sb.cr += 16
                dsem = (sb.semr, sb.cr)
            key = ('dma', id(dsem[0]))
        for b in wb:
            b.lw = iid
            b.rd = {}
        for b in rb:
            b.rd[key] = iid
        if dsem is None:
            self.last_eng[eng] = iid
        else:
            self.last_dma[id(dsem[0])] = iid
        self.ins.append((eng, fn, deps, dsem))
        return iid

    def _take_sem(self, name):
        if self.sem_pool:
            return self.sem_pool.pop()
        return self._sem(name), 0

    def barrier(self):
        self.bar = set(self.last_eng.values()) | set(self.last_dma.values())
        for (b, kind) in self.sem_active:
            if kind == 'w':
                self.sem_pool.append((b.semw, b.cw))
                b.semw = None
            else:
                self.sem_pool.append((b.semr, b.cr))
                b.semr = None
        self.sem_active = []

    def dma(self, q, out, in_):
        ob, ib = out.buf, in_.buf
        if ob.kind == 'sbuf':
            d = ('w', ob)
        else:
            assert ib.kind == 'sbuf', "dram->dram dma not supported"
            d = ('r', ib)
        o, i = out.ap, in_.ap
        return self.emit(q, lambda e: e.dma_start(out=o, in_=i), [in_], [out], dma=d)

    def mm(self, out, lhsT, rhs, start=True, stop=True, **kw):
        o, l, r = out.ap, lhsT.ap, rhs.ap
        return self.emit('pe', lambda e: e.matmul(o, l, r, start=start, stop=stop, **kw),
                         [lhsT, rhs], [out])

    def transpose(self, out, in_, ident):
        o, i, d = out.ap, in_.ap, ident.ap
        return self.emit('pe', lambda e: e.transpose(o, i, d), [in_, ident], [out])

    def act(self, out, in_, func, bias=None, scale=1.0, accum=None, eng='act'):
        o, i = out.ap, in_.ap
        b, s, a = _ap(bias), _ap(scale), _ap(accum)
        kw = {}
        if b is not None:
            kw['bias'] = b
        if a is not None:
            kw['accum_out'] = a
        w = [out] + ([accum] if accum is not None else [])
        return self.emit(eng, lambda e: e.activation(out=o, in_=i, func=func, scale=s, **kw),
                         [in_, bias, scale], w)

    def tt(self, eng, out, in0, in1, op):
        o, a, b = out.ap, in0.ap, in1.ap
        return self.emit(eng, lambda e: e.tensor_tensor(out=o, in0=a, in1=b, op=op), [in0, in1], [out])

    def ts(self, eng, out, in0, s1, s2, op0, op1=None, accum=None):
        o, a = out.ap, in0.ap
        x1, x2, ac = _ap(s1), _ap(s2), _ap(accum)
        kw = {}
        if op1 is not None:
            kw['op1'] = op1
        if ac is not None:
            kw['accum_out'] = ac
        w = [out] + ([accum] if accum is not None else [])
        return self.emit(eng, lambda e: e.tensor_scalar(out=o, in0=a, scalar1=x1, scalar2=x2, op0=op0, **kw),
                         [in0, s1, s2], w)

    def stt(self, eng, out, in0, scalar, in1, op0, op1):
        o, a, b, s = out.ap, in0.ap, in1.ap, _ap(scalar)
        return self.emit(eng, lambda e: e.scalar_tensor_tensor(out=o, in0=a, scalar=s, in1=b, op0=op0, op1=op1),
                         [in0, in1, scalar], [out])

    def scan(self, out, d0, d1, init, op0, op1):
        o, a, b, i = out.ap, d0.ap, d1.ap, _ap(init)
        return self.emit('dve', lambda e: e.tensor_tensor_scan(out=o, data0=a, data1=b, initial=i, op0=op0, op1=op1),
                         [d0, d1, init], [out])

    def copy(self, eng, out, in_):
        o, i = out.ap, in_.ap
        if eng == 'act':
            return self.emit(eng, lambda e: e.copy(out=o, in_=i), [in_], [out])
        return self.emit(eng, lambda e: e.tensor_copy(out=o, in_=i), [in_], [out])

    def memset(self, eng, out, val):
        o = out.ap
        return self.emit(eng, lambda e: e.memset(o, val), [], [out])

    def reduce(self, out, in_, op, axis=AX.X, eng='dve'):
        o, i = out.ap, in_.ap
        return self.emit(eng, lambda e: e.tensor_reduce(out=o, in_=i, axis=axis, op=op), [in_], [out])

    def recip(self, out, in_):
        o, i = out.ap, in_.ap
        return self.emit('dve', lambda e: e.reciprocal(out=o, in_=i), [in_], [out])

    def affine_select(self, out, in_, pattern, cmp, fill, base, cm):
        o, i = out.ap, in_.ap
        return self.emit('pool', lambda e: e.affine_select(out=o, in_=i, pattern=pattern, compare_op=cmp,
                                                           fill=fill, base=base, channel_multiplier=cm),
                         [in_], [out])

    def finish(self, final_wait=True):
        nc = self.nc
        ins = self.ins
        n = len(ins)
        needed = [False] * n
        for (eng, fn, deps, dsem) in ins:
            for d in deps:
                if ins[d][0] == 'pe' and eng == 'pe' and ins[d][3] is None:
                    continue
                needed[d] = True
        esem = {e: self._sem("c_" + e) for e in ('pe', 'act', 'dve', 'pool')}
        ecnt = {e: 0 for e in esem}
        token = [None] * n
        known = {e: {} for e in ENGS}
        snap = [None] * n
        stream = {e: [] for e in ENGS}
        for iid, (eng, fn, deps, dsem) in enumerate(ins):
            kn = known[eng]
            waits = {}
            for d in deps:
                if ins[d][0] == 'pe' and eng == 'pe' and ins[d][3] is None:
                    continue
                sem, val = token[d]
                sid = id(sem)
                if kn.get(sid, 0) >= val:
                    continue
                if sid not in waits or waits[sid][1] < val:
                    waits[sid] = (sem, val)
            for d in deps:
                s = snap[d]
                if s is not None and token[d] is not None and id(token[d][0]) in waits:
                    for k2, v2 in s.items():
                        if kn.get(k2, 0) < v2:
                            kn[k2] = v2
            wl = []
            for sid, (sem, val) in waits.items():
                if kn.get(sid, 0) >= val:
                    continue
                kn[sid] = val
                wl.append((sem, val))
            inc = None
            if dsem is not None:
                token[iid] = dsem
                inc = (dsem[0], 16)
            elif needed[iid]:
                ecnt[eng] += 1
                token[iid] = (esem[eng], ecnt[eng])
                inc = (esem[eng], 1)
                kn[id(esem[eng])] = max(kn.get(id(esem[eng]), 0), 0)
            if needed[iid] or dsem is not None:
                snap[iid] = dict(kn)
            stream[eng].append((wl, fn, inc))
        finals = []
        seen = set()
        for (eng, fn, deps, dsem) in ins:
            if dsem is not None:
                seen.add(id(dsem[0]))
        allbufs = {}
        for (eng, fn, deps, dsem) in ins:
            if dsem is not None:
                sid = id(dsem[0])
                if sid not in allbufs or allbufs[sid][1] < dsem[1]:
                    allbufs[sid] = dsem
        finals = list(allbufs.values())
        self.stats = {e: len(stream[e]) for e in ENGS}
        self.stats['waits'] = sum(len(w) for e in ENGS for (w, _, _) in stream[e])
        self.stats['sems'] = self.nsem

        with nc.Block() as block:
            def run(e, name):
                for (wl, fn, inc) in stream[name]:
                    for (sem, val) in wl:
                        e.wait_ge(sem, val)
                    r = fn(e)
                    if inc is not None:
                        r.then_inc(inc[0], inc[1])
                if name == 'sp' and final_wait:
                    for (sem, val) in finals:
                        e.wait_ge(sem, val)
                    for en in esem:
                        if ecnt[en] > 0:
                            e.wait_ge(esem[en], ecnt[en])

            @block.sync
            def _(e):
                run(e, 'sp')

            @block.scalar
            def _(e):
                run(e, 'act')

            @block.vector
            def _(e):
                run(e, 'dve')

            @block.gpsimd
            def _(e):
                run(e, 'pool')

            @block.tensor
            def _(e):
                run(e, 'pe')
        self.stack.close()
        return nc


T = 4096
D = 2048
EPS = 1e-6
J_EIN = 43
P_ROWS = 5408
DFF = 5632
KFF = 44
CW = 4096
B_LN_EPS = 64e-5


def _cc(v, pad_to=None):
    v = np.asarray(v, np.float32).reshape(-1)
    if pad_to is not None and v.size < pad_to:
        v = np.concatenate([v, np.zeros(pad_to - v.size, np.float32)])
    return np.ascontiguousarray(v.reshape(-1, 128).T)


def _tile_w(w, J):
    K, M = w.shape
    kc = K // 128
    wp = np.zeros((K, J * 128), np.float32)
    wp[:, :M] = w
    return np.ascontiguousarray(wp.reshape(kc, 128, J, 128).transpose(2, 1, 0, 3).reshape(J, 128, kc * 128))


def pack_consts(inp):
    ent = []

    def add(name, arr):
        ent.append((name, np.asarray(arr, np.float32)))

    add('even_norm', _cc(inp['even_norm'][0]))
    add('ffn_norm0', _cc(inp['ffn_norm'][0]))
    add('ffn_norm1', _cc(inp['ffn_norm'][1]))
    add('odd_norm', _cc(inp['odd_norm'][0]))
    add('final_norm', _cc(inp['final_norm']))
    add('a_conv_w', inp['a_conv_w'][0].reshape(4, 8, 128).transpose(2, 1, 0).reshape(128, 32))
    for nm in ('a_conv_b', 'a_b_r', 'a_b_i', 'a_lambda'):
        add(nm, _cc(inp[nm][0]))
    add('b_mu', _cc(inp['b_mu'][0], 27 * 128))
    for nm in ('b_w0', 'b_a0', 'b_k_k', 'b_k_a', 'b_r_k', 'b_ln_w', 'b_ln_b'):
        add(nm, _cc(inp[nm][0]))
    for l in range(2):
        add('ffn_conv_w%d' % l, inp['ffn_conv_w'][l].reshape(3, KFF, 128).transpose(2, 1, 0).reshape(128, KFF * 3))
        add('ffn_conv_b%d' % l, _cc(inp['ffn_conv_b'][l]))
    add('c_conv_w', inp['c_conv_w'][0].reshape(4, 32, 128).transpose(2, 1, 0).reshape(128, 128))
    for nm in ('c_conv_b', 'c_ln_w', 'c_skip'):
        add(nm, _cc(inp[nm][0]))
    for nm in ('c_w_q', 'c_w_k', 'c_w_v'):
        add(nm, inp[nm][0].reshape(32, 128, 4).transpose(1, 0, 2).reshape(128, 128))
    bif = np.zeros((128, 1), np.float32)
    bif[:16, 0] = inp['c_b_if'][0]
    add('c_b_if', bif)
    offs = {}
    o = 0
    for name, a in ent:
        offs[name] = (o, a.shape[1])
        o += a.shape[1]
    return np.ascontiguousarray(np.concatenate([a for _, a in ent], axis=1)), offs


def const_offsets():
    dummy = {
        'even_norm': np.zeros((1, D)), 'ffn_norm': np.zeros((2, D)), 'odd_norm': np.zeros((1, D)),
        'final_norm': np.zeros(D), 'a_conv_w': np.zeros((1, 4, 1024)),
        'b_mu': np.zeros((1, 3360)), 'ffn_conv_w': np.zeros((2, 3, DFF)), 'ffn_conv_b': np.zeros((2, DFF)),
        'c_conv_w': np.zeros((1, 4, CW)), 'c_b_if': np.zeros((1, 16)),
    }
    for nm in ('a_conv_b', 'a_b_r', 'a_b_i', 'a_lambda', 'b_w0', 'b_a0', 'b_k_k', 'b_k_a', 'b_r_k', 'b_ln_w', 'b_ln_b'):
        dummy[nm] = np.zeros((1, 1024))
    for nm in ('c_conv_b', 'c_ln_w', 'c_skip'):
        dummy[nm] = np.zeros((1, CW))
    for nm in ('c_w_q', 'c_w_k', 'c_w_v'):
        dummy[nm] = np.zeros((1, 1024, 4, 4))
    c, offs = pack_consts(dummy)
    return c.shape[1], offs


class Arena:
    def __init__(self, P, cols):
        self.P = P
        self.t = P.sbuf("arena", [128, cols], F32)
        self.cols = cols
        self.base = 0
        self.off = 0
        self.n = 0

    def reset(self):
        self.off = self.base

    def persist(self):
        self.base = self.off

    def alloc(self, cols, dtype=F32, name=None):
        n32 = cols if dtype == F32 else (cols + 1) // 2
        a = self.off
        self.off += n32
        assert self.off <= self.cols, ("arena overflow", self.off, self.cols)
        ap = self.t.ap[:, a:a + n32]
        if dtype != F32:
            ap = ap.bitcast(dtype)[:, 0:cols]
        self.n += 1
        return V(ap, Buf(name or ("ar%d" % self.n), 'sbuf'))


class Ctx:
    pass


def load_cast(X, dst, src, rows=128):
    cols = dst.ap.shape[1]
    stg = X.A.alloc(cols, F32)
    X.P.dma('sp', stg[0:rows], src)
    X.P.copy('act', dst[0:rows], stg[0:rows])


def split_groups(n, g):
    g = min(g, n)
    base, rem = divmod(n, g)
    out = []
    a = 0
    for i in range(g):
        b = a + base + (1 if i < rem else 0)
        out.append((a, b))
        a = b
    return out


def gemm(X, src, KC, wsets, J, TB, epi, jlist=None):
    P, A = X.P, X.A
    ns = len(wsets)
    groups = split_groups(KC, 4)
    act = [A.alloc((b - a) * TB, BF16) for (a, b) in groups]
    wb = [[A.alloc(KC * 128, BF16) for _ in range(2)] for _ in range(ns)]
    wst = [[A.alloc(KC * 128, F32) for _ in range(2)] for _ in range(ns)]
    srcv = src.re("(k p) t -> p k t", p=128)
    nsub = TB // 512
    cnt = 0
    jl = list(range(J)) if jlist is None else jlist
    its = [(tb, ji) for tb in range(T // TB) for ji in range(len(jl))]

    def wload(n):
        tb_, ji_ = its[n]
        for s in range(ns):
            P.dma('sp', wst[s][n % 2], wsets[s][jl[ji_]])
            P.copy('dve' if (n + s) % 2 == 0 else 'act', wb[s][n % 2], wst[s][n % 2])

    wload(0)
    for n, (tb, ji) in enumerate(its):
        j = jl[ji]
        if ji == 0:
            for gi, (a, b) in enumerate(groups):
                P.dma('sp', act[gi].re("p (k t) -> p k t", t=TB), srcv[:, a:b, tb * TB:(tb + 1) * TB])
        if n + 1 < len(its):
            wload(n + 1)
        for sub in range(nsub):
            pss = []
            for s in range(ns):
                ps = X.ps[(cnt % (6 // ns)) * ns + s]
                pss.append(ps)
                for gi, (a, b) in enumerate(groups):
                    for k in range(a, b):
                        P.mm(ps, wb[s][n % 2][:, k * 128:(k + 1) * 128],
                             act[gi][:, (k - a) * TB + sub * 512:(k - a) * TB + (sub + 1) * 512],
                             start=(k == 0), stop=(k == KC - 1))
            cnt += 1
            epi(j, tb * TB + sub * 512, pss)


def phase_norm(X, src, gname, dst, out_f32=False):
    P, A = X.P, X.A
    P.barrier()
    A.reset()
    g = X.C(gname)
    odt = F32 if out_f32 else BF16
    xs = [A.alloc(16 * 512, F32) for _ in range(2)]
    sq = [A.alloc(16 * 512, BF16) for _ in range(2)]
    hs = [A.alloc(16 * 512, odt) for _ in range(2)]
    rs = [A.alloc(512, F32) for _ in range(2)]
    sv = src.re("(k p) t -> p k t", p=128)
    dv = dst.re("(k p) t -> p k t", p=128)
    for it in range(T // 512):
        x, q, h, r = xs[it % 2], sq[it % 2], hs[it % 2], rs[it % 2]
        P.dma('sp', x.re("p (k t) -> p k t", t=512), sv[:, :, it * 512:(it + 1) * 512])
        P.act(q, x, AF.Square)
        ps = X.ps[6 + it % 2]
        for k in range(16):
            P.mm(ps, X.ones_bf, q[:, k * 512:(k + 1) * 512], start=(k == 0), stop=(k == 15))
        P.ts('dve', r, ps, EPS, None, ALU.add)
        P.act(r, r, AF.Ln)
        P.act(r, r, AF.Exp, scale=-0.5)
        for k in range(16):
            P.stt('dve', h[:, k * 512:(k + 1) * 512], x[:, k * 512:(k + 1) * 512],
                  g[:, k:k + 1], r, ALU.mult, ALU.mult)
        P.dma('act', dv[:, :, it * 512:(it + 1) * 512], h.re("p (k t) -> p k t", t=512))


def phase_even_inproj(X):
    P, A = X.P, X.A
    P.barrier()
    A.reset()
    st = [A.alloc(512, F32) for _ in range(4)]
    c = [0]

    def epi(j, t0, pss):
        s = st[c[0] % 4]
        if c[0] % 2 == 0:
            P.copy('act', s, pss[0])
        else:
            P.copy('dve', s, pss[0])
        c[0] += 1
        rows = min(128, P_ROWS - j * 128)
        P.dma('sp', X.pT[j * 128:j * 128 + rows, t0:t0 + 512], s[0:rows, :])

    gemm(X, X.hT, 16, [X.w_ein], J_EIN, 2048, epi)


def phase_resid_gemm(X, src, KC, w, TB, xin, xout):
    P, A = X.P, X.A
    P.barrier()
    A.reset()
    st = [A.alloc(512, F32) for _ in range(4)]
    c = [0]

    def epi(j, t0, pss):
        s = st[c[0] % 4]
        c[0] += 1
        P.dma('sp', s, xin[j * 128:(j + 1) * 128, t0:t0 + 512])
        P.tt('dve', s, s, pss[0], ALU.add)
        P.dma('sp', xout[j * 128:(j + 1) * 128, t0:t0 + 512], s)

    gemm(X, src, KC, [w], 16, TB, epi)


def phase_ffn_up(X, l):
    P, A = X.P, X.A
    P.barrier()
    A.reset()
    cw = X.C('ffn_conv_w%d' % l)
    cb = X.C('ffn_conv_b%d' % l)
    gb = [A.alloc(514, F32) for _ in range(3)]
    acc = [A.alloc(512, F32) for _ in range(2)]
    tmp = [A.alloc(512, F32) for _ in range(2)]
    ub = [A.alloc(512, BF16) for _ in range(3)]
    halo = A.alloc(KFF * 2, F32)
    c = [0]
    TB = 2048

    def epi(j, t0, pss):
        i = c[0]
        c[0] += 1
        g = gb[i % 3]
        gprev = gb[(i - 1) % 3]
        a = acc[i % 2]
        u = ub[i % 3]
        P.copy('act', g[:, 2:514], pss[0])
        if t0 == 0:
            P.memset('dve', g[:, 0:2], 0.0)
        elif t0 % TB == 0:
            P.copy('dve', g[:, 0:2], halo[:, 2 * j:2 * j + 2])
        else:
            P.copy('dve', g[:, 0:2], gprev[:, 512:514])
        if (t0 + 512) % TB == 0:
            P.copy('dve', halo[:, 2 * j:2 * j + 2], g[:, 512:514])
        P.act(a, g[:, 0:512], AF.Identity, bias=cb[:, j:j + 1], scale=cw[:, 3 * j:3 * j + 1])
        P.stt('dve', a, g[:, 1:513], cw[:, 3 * j + 1:3 * j + 2], a, ALU.mult, ALU.add)
        P.stt('dve', a, g[:, 2:514], cw[:, 3 * j + 2:3 * j + 3], a, ALU.mult, ALU.add)
        P.act(a, a, AF.Silu)
        P.tt('dve', u, a, pss[1], ALU.mult)
        P.dma('sp', X.uT[j * 128:(j + 1) * 128, t0:t0 + 512], u)

    gemm(X, X.hT, 16, [X.w_gate[l], X.w_up[l]], KFF, TB, epi)


def phase_rglru(X):
    P, A = X.P, X.A
    P.barrier()
    A.reset()
    wr = A.alloc(8 * 128, BF16)
    wi = A.alloc(8 * 128, BF16)
    load_cast(X, wr, X.a_w_r)
    load_cast(X, wi, X.a_w_i)
    cl = A.alloc(8, F32)
    cl2 = A.alloc(8, F32)
    P.act(cl, X.C('a_lambda'), AF.Exp, scale=-1.0)
    P.ts('dve', cl, cl, 1.0, None, ALU.add)
    P.act(cl, cl, AF.Ln)
    P.ts('dve', cl2, cl, -16.0, None, ALU.mult)
    P.ts('dve', cl, cl, -8.0, None, ALU.mult)
    xa = A.alloc(3 + T, F32)
    ga = A.alloc(T, F32)
    xc = A.alloc(T, F32)
    xcb = A.alloc(T, BF16)
    r = A.alloc(T, F32)
    ig = A.alloc(T, F32)
    a = A.alloc(T, F32)
    s = A.alloc(T, F32)
    yb = A.alloc(T, BF16)
    P.memset('pool', xa[:, 0:3], 0.0)
    cwt = X.C('a_conv_w')
    for c in range(8):
        P.dma('sp', xa[:, 3:3 + T], X.pT[c * 128:(c + 1) * 128, :])
        P.dma('sp', ga, X.pT[(8 + c) * 128:(9 + c) * 128, :])
        P.ts('dve', xc, xa[:, 0:T], cwt[:, 4 * c:4 * c + 1], X.C('a_conv_b')[:, c:c + 1], ALU.mult, ALU.add)
        for j in range(1, 4):
            P.stt('dve', xc, xa[:, j:j + T], cwt[:, 4 * c + j:4 * c + j + 1], xc, ALU.mult, ALU.add)
        P.copy('pool', xcb, xc)
        for it in range(8):
            sl = slice(it * 512, (it + 1) * 512)
            p1 = X.ps[(2 * it) % 6]
            p2 = X.ps[(2 * it + 1) % 6]
            P.mm(p1, wr[:, c * 128:(c + 1) * 128], xcb[:, sl])
            P.mm(p2, wi[:, c * 128:(c + 1) * 128], xcb[:, sl])
            P.act(r[:, sl], p1, AF.Sigmoid, bias=X.C('a_b_r')[:, c:c + 1])
            P.act(ig[:, sl], p2, AF.Sigmoid, bias=X.C('a_b_i')[:, c:c + 1])
        P.act(a, r, AF.Exp, scale=cl[:, c:c + 1])
        P.act(s, r, AF.Exp, scale=cl2[:, c:c + 1])
        P.ts('dve', s, s, -1.0, 1.0, ALU.mult, ALU.add)
        P.act(s, s, AF.Sqrt)
        P.tt('pool', ig, ig, xc, ALU.mult)
        P.tt('dve', s, s, ig, ALU.mult)
        P.scan(r, a, s, 0.0, ALU.mult, ALU.add)
        P.tt('pool', a, ga, ga, ALU.mult)
        P.ts('dve', a, a, 0.044715, 1.0, ALU.mult, ALU.add)
        P.tt('pool', a, a, ga, ALU.mult)
        P.act(a, a, AF.Tanh, scale=0.7978845608028654)
        P.ts('dve', a, a, 1.0, 0.5, ALU.add, ALU.mult)
        P.tt('pool', a, a, ga, ALU.mult)
        P.tt('dve', yb, a, r, ALU.mult)
        P.dma('act', X.yT[c * 128:(c + 1) * 128, :], yb)


def build(stages=('all',), dbg=(), rparts=('prep', 'main', 'post'), rchunks=T // 64):
    nc = bass.Bass("TRN2", target_bir_lowering=False)
    P = Prog(nc)
    X = Ctx()
    X.P = P
    X.rparts = rparts
    import os as _os
    X.rcut = int(_os.environ.get('RCUT', '9'))
    X.rchunks = rchunks
    ncst, offs = const_offsets()

    def dr(name, shape, dtype=F32, kind="Internal"):
        if name in dbg:
            kind = "ExternalOutput"
        return P.dram(name, shape, dtype, kind=kind)

    ein = "ExternalInput"
    X.x0 = dr("xT", [D, T], F32, ein)
    cst_d = dr("cst", [128, ncst], F32, ein)
    X.w_ein = dr("w_ein", [J_EIN, 128, 2048], F32, ein)
    X.w_eout = dr("w_eout", [16, 128, 2048], F32, ein)
    X.w_gate = [dr("w_gate%d" % l, [KFF, 128, 2048], F32, ein) for l in range(2)]
    X.w_up = [dr("w_up%d" % l, [KFF, 128, 2048], F32, ein) for l in range(2)]
    X.w_down = [dr("w_down%d" % l, [16, 128, DFF], F32, ein) for l in range(2)]
    X.w_oin = dr("w_oin", [64, 128, 2048], F32, ein)
    X.w_oout = dr("w_oout", [16, 128, CW], F32, ein)
    X.a_w_r = dr("a_w_r", [128, 1024], F32, ein)
    X.a_w_i = dr("a_w_i", [128, 1024], F32, ein)
    X.wlr = dr("wlr", [128, 1024], F32, ein)
    X.gup1 = dr("gup1", [128, 1024], F32, ein)
    X.gup2 = dr("gup2", [32, 1024], F32, ein)
    X.c_w_if = dr("c_w_if", [128, 3 * 32 * 16], F32, ein)
    X.masks = dr("masks", [128, 128 + 4 * 512], F32, ein)
    X.out = dr("out", [D, T], F32, "ExternalOutput")
    X.hT = dr("hT", [D, T], BF16)
    X.pT = dr("pT", [P_ROWS, T], F32)
    X.yT = dr("yT", [D, T], BF16)
    X.uT = dr("uT", [DFF, T], BF16)
    X.xA = dr("xA", [D, T], F32)
    X.RW = dr("RW", [8, 128, 64 * 320], F32)
    X.gS = dr("gS", [1024, T], F32)
    X.WLS = dr("WLS", [1024, 64], F32)
    X.xmz = dr("xmz", [8192, T], F32)
    X.qT = dr("qT", [CW, T], BF16)
    X.kT = dr("kT", [CW, T], BF16)
    X.q2T = dr("q2T", [CW, T], BF16)
    X.k2T = dr("k2T", [CW, T], BF16)
    X.xcT = dr("xcT", [CW, T], BF16)
    X.vtok = dr("vtok", [T, CW], BF16)
    X.hS = dr("hS", [CW, T], F32)
    X.hsT = dr("hsT", [CW, T], BF16)
    X.mconst = dr("mconst", [128, 1408], F32, ein)
    X.bif_d = dr("bif_d", [16, 1], F32, ein)
    X.rkS = dr("rkS", [1024, T], F32)
    X.vS = dr("vS", [1024, T], F32)
    X.yS = dr("yS", [1024, T], F32)
    X.xB = dr("xB", [D, T], F32)

    A = Arena(P, 50432)
    X.A = A
    X.ps = [P.psum("ps%d" % i) for i in range(8)]
    cst = A.alloc(ncst, F32, "cst")
    P.dma('sp', cst, cst_d)
    X.C = lambda name: cst[:, offs[name][0]:offs[name][0] + offs[name][1]]
    X.ones_bf = A.alloc(128, BF16, "ones")
    P.memset('dve', X.ones_bf, 1.0 / D)
    A.persist()
    X.base0 = A.base

    def on(s):
        return 'all' in stages or s in stages

    if on('e_norm'):
        phase_norm(X, X.x0, 'even_norm', X.hT)
    if on('e_in'):
        phase_even_inproj(X)
    if on('rglru'):
        phase_rglru(X)
    if on('rwkv'):
        phase_rwkv(X)
        A.base = X.base0
    if on('e_out'):
        phase_resid_gemm(X, X.yT, 16, X.w_eout, 2048, X.x0, X.xA)
    if on('ffn0'):
        phase_norm(X, X.xA, 'ffn_norm0', X.hT)
        phase_ffn_up(X, 0)
        phase_resid_gemm(X, X.uT, KFF, X.w_down[0], 1024, X.xA, X.xB)
    if on('mlstm'):
        phase_mlstm(X)
    if on('ffn1'):
        phase_norm(X, X.xA, 'ffn_norm1', X.hT)
        phase_ffn_up(X, 1)
        phase_resid_gemm(X, X.uT, KFF, X.w_down[1], 1024, X.xA, X.xB)
    if on('final'):
        phase_norm(X, X.xB, 'final_norm', X.out, out_f32=True)
    P.finish()
    X.stats = P.stats
    return nc, X


def host_masks():
    i = np.arange(128)[:, None]
    t = np.arange(64)[None, :]
    ident = (np.arange(128)[:, None] == np.arange(128)[None, :]).astype(np.float32)
    mS = np.tile((t > i).astype(np.float32), (1, 8))
    mI = np.tile((t >= i).astype(np.float32), (1, 8))
    mL = np.tile((i > t).astype(np.float32), (1, 8))
    idb = np.tile((t == i).astype(np.float32), (1, 8))
    return np.ascontiguousarray(np.concatenate([ident, mS, mI, mL, idb], axis=1))


def host_mconst():
    p = np.arange(128)[:, None]
    c = np.arange(128)[None, :]
    mbd = ((p // 4) == (c // 4)).astype(np.float32)
    maskC = (p <= c).astype(np.float32)
    ident = (p == c).astype(np.float32)
    sel = np.zeros((128, 1024), np.float32)
    for h in range(8):
        sel[h, h * 128:(h + 1) * 128] = 1.0
    return np.ascontiguousarray(np.concatenate([mbd, maskC, ident, sel], axis=1))


def host_inputs(inp):
    cst, _ = pack_consts(inp)
    sh = {
        'cst': cst,
        'masks': host_masks(),
        'mconst': host_mconst(),
        'bif_d': np.ascontiguousarray(inp['c_b_if'][0].reshape(16, 1)),
        'w_ein': _tile_w(inp['even_w_in'][0], J_EIN),
        'w_eout': _tile_w(inp['even_w_out'][0], 16),
        'w_oin': _tile_w(inp['odd_w_in'][0], 64),
        'w_oout': _tile_w(inp['odd_w_out'][0], 16),
        'a_w_r': np.ascontiguousarray(inp['a_w_r'][0].transpose(1, 0, 2).reshape(128, 1024)),
        'a_w_i': np.ascontiguousarray(inp['a_w_i'][0].transpose(1, 0, 2).reshape(128, 1024)),
        'wlr': np.ascontiguousarray(np.concatenate([inp['b_w_up'][0], inp['b_a_up'][0]], axis=0)),
        'gup1': np.ascontiguousarray(inp['b_g_up'][0][:128]),
        'gup2': np.ascontiguousarray(inp['b_g_up'][0][128:160]),
        'c_w_if': np.ascontiguousarray(inp['c_w_if'][0].reshape(3, 32, 128, 16).transpose(2, 0, 1, 3).reshape(128, 1536)),
    }
    for l in range(2):
        sh['w_gate%d' % l] = _tile_w(inp['ffn_w_gate'][l], KFF)
        sh['w_up%d' % l] = _tile_w(inp['ffn_w_up'][l], KFF)
        sh['w_down%d' % l] = _tile_w(inp['ffn_w_down'][l], 16)
    return sh


def kernel(**inputs):
    inp = {k: np.asarray(v) for k, v in inputs.items()}
    x = inp['x']
    B = x.shape[0]
    nc, X = build()
    sh = host_inputs(inp)
    in_maps = []
    for b in range(B):
        m = dict(sh)
        m['xT'] = np.ascontiguousarray(x[b].T)
        in_maps.append(m)
    res = run_bass_kernel_spmd(nc, in_maps, core_ids=list(range(B)))
    out = np.stack([np.ascontiguousarray(r['out'].T) for r in res.results], axis=0)
    return out.astype(np.float32)


def _c3(v, l=64):
    return v.re("p (c l) -> p c l", l=l)


def phase_rwkv(X):
    P, A = X.P, X.A
    P.barrier()
    A.reset()
    C = X.C
    TT = 1024
    NCH = TT // 64
    wlr = A.alloc(1024, BF16)
    gu1 = A.alloc(1024, BF16)
    gu2 = A.alloc(1024, BF16)
    BO = A.alloc(128, F32)
    BO64 = A.alloc(128, F32)
    for (t_, val) in ((BO, 1.0), (BO64, 1.0 / 64)):
        P.memset('dve', t_, 0.0)
        P.memset('dve', t_[0:64, 0:64], val)
        P.memset('dve', t_[64:128, 64:128], val)
    WL = A.alloc(8 * 64, F32)
    omk = A.alloc(8, F32)
    P.ts('dve', omk, C('b_k_a'), -1.0, 1.0, ALU.mult, ALU.add)
    A.persist()
    load_cast(X, wlr, X.wlr)
    load_cast(X, gu1, X.gup1)
    load_cast(X, gu2, X.gup2, rows=32)
    mask = A.alloc(TT, F32)
    P.memset('dve', mask, 1.0)
    P.memset('dve', _c3(mask)[:, :, 0:1], 0.0)
    xbs = [A.alloc(TT + 1, F32) for _ in range(4)]
    x4 = A.alloc(TT, F32)
    rr = A.alloc(TT, F32)
    k0 = A.alloc(TT, F32)
    vv = A.alloc(TT, F32)
    xwa = A.alloc(TT, BF16)
    sg1 = A.alloc(TT, BF16)
    sg2 = A.alloc(TT, BF16)
    sig, aic, gst, kk, sq, rn, km, bv, lw, cw, cwx, e1, e2, e3, rk = [A.alloc(TT, F32) for _ in range(15)]
    O = A.alloc(NCH * 320, F32)
    Ov = O.re("p (c q l) -> p c q l", q=5, l=64)
    mu = C('b_mu')
    pc = [0]

    def nps():
        pc[0] += 1
        return X.ps[pc[0] % 8]

    def lerp(dst, xb, chunk, nrows, t0):
        r0 = chunk * 128
        if t0 == 0:
            P.memset('pool', xb[0:nrows, 0:1], 0.0)
            P.dma('sp', xb[0:nrows, 1:1 + TT], X.pT[r0:r0 + nrows, t0:t0 + TT])
        else:
            P.dma('sp', xb[0:nrows, 0:1 + TT], X.pT[r0:r0 + nrows, t0 - 1:t0 + TT])
        P.tt('dve', dst[0:nrows], xb[0:nrows, 0:TT], xb[0:nrows, 1:1 + TT], ALU.subtract)
        P.stt('dve', dst[0:nrows], dst[0:nrows], mu[0:nrows, chunk - 16:chunk - 15], xb[0:nrows, 1:1 + TT],
              ALU.mult, ALU.add)

    for tt in (range(T // TT) if 'prep' in X.rparts else []):
        t0 = tt * TT
        lerp(x4, xbs[0], 40, 128, t0)
        P.act(xwa[0:64], x4[0:64], AF.Tanh)
        P.copy('pool', xwa[64:128], x4[64:128])
        lerp(x4, xbs[0], 41, 128, t0)
        P.act(sg1, x4, AF.Sigmoid)
        lerp(x4, xbs[0], 42, 32, t0)
        P.act(sg2[0:32], x4[0:32], AF.Sigmoid)
        for hp in range(8):
            hc = slice(hp * 128, (hp + 1) * 128)
            h1 = slice(hp, hp + 1)
            lerp(rr, xbs[1], 16 + hp, 128, t0)
            lerp(k0, xbs[2], 24 + hp, 128, t0)
            lerp(vv, xbs[3], 32 + hp, 128, t0)
            for sub in range(TT // 512):
                sl = slice(sub * 512, (sub + 1) * 512)
                pz = nps()
                P.mm(pz, wlr[0:64, hc], xwa[0:64, sl])
                P.act(sig[:, sl], pz, AF.Sigmoid, bias=C('b_w0')[:, h1])
                pa = nps()
                P.mm(pa, wlr[64:128, hc], xwa[64:128, sl])
                P.act(aic[:, sl], pa, AF.Sigmoid, bias=C('b_a0')[:, h1])
                pg = nps()
                P.mm(pg, gu1[:, hc], sg1[:, sl], start=True, stop=False)
                P.mm(pg, gu2[0:32, hc], sg2[0:32, sl], start=False, stop=True)
                P.copy('act', gst[:, sl], pg)
            P.dma('act', X.gS[hc, t0:t0 + TT], gst)
            P.ts('dve', kk, k0, C('b_k_k')[:, h1], None, ALU.mult)
            P.act(sq, kk, AF.Square)
            for sub in range(TT // 512):
                sl = slice(sub * 512, (sub + 1) * 512)
                pq = nps()
                P.mm(pq, BO, sq[:, sl])
                P.ts('dve', rn[:, sl], pq, 1e-12, None, ALU.max)
            P.act(rn, rn, AF.Ln)
            P.act(rn, rn, AF.Exp, scale=-0.5)
            P.tt('dve', kk, kk, rn, ALU.mult)
            P.ts('dve', km, aic, C('b_k_a')[:, h1], omk[:, h1], ALU.mult, ALU.add)
            P.tt('pool', km, km, k0, ALU.mult)
            P.tt('pool', bv, kk, aic, ALU.mult)
            P.ts('dve', lw, sig, -0.6065306597126334, None, ALU.mult)
            P.scan(cw, mask, lw, 0.0, ALU.mult, ALU.add)
            P.tt('pool', cwx, cw, lw, ALU.subtract)
            P.act(e1, cw, AF.Exp)
            P.act(e2, cwx, AF.Exp)
            P.act(e3, cw, AF.Exp, scale=-1.0)
            P.stt('dve', Ov[:, :, 0, :], _c3(kk), -1.0, _c3(e2), ALU.mult, ALU.mult)
            P.tt('pool', Ov[:, :, 1, :], _c3(rr), _c3(e1), ALU.mult)
            P.tt('dve', Ov[:, :, 2, :], _c3(bv), _c3(e3), ALU.mult)
            P.tt('pool', Ov[:, :, 3, :], _c3(km), _c3(e3), ALU.mult)
            P.copy('act', Ov[:, :, 4, :], _c3(vv))
            wlt = WL[:, hp * 64 + tt * NCH:hp * 64 + (tt + 1) * NCH]
            P.copy('pool', wlt, e1[:, 63:TT:64])
            P.dma('act', X.WLS[hc, tt * NCH:(tt + 1) * NCH], wlt)
            P.tt('dve', rk, rr, km, ALU.mult)
            P.ts('dve', rk, rk, C('b_r_k')[:, h1], None, ALU.mult)
            P.dma('act', X.rkS[hc, t0:t0 + TT], rk)
            P.dma('act', X.vS[hc, t0:t0 + TT], vv)
            P.dma('act', X.RW[hp, :, tt * NCH * 320:(tt + 1) * NCH * 320], O)

    P.barrier()
    A.reset()
    mk = A.alloc(128 + 4 * 512, F32)
    P.dma('sp', mk, X.masks)
    ident = mk[:, 0:128]
    mS = mk[:, 128:640]
    mI = mk[:, 640:1152]
    mL = mk[:, 1152:1664]
    idb = mk[:, 1664:2176]
    NB = 2
    idn = ident[0:64, 0:64]
    WLh = A.alloc(16 * 64, F32)
    P.dma('sp', WLh[0:64].re("p (h c) -> p h c", c=64), X.WLS.re("(h p) c -> p h c", p=64))
    blk = [[A.alloc(NB * 320, F32) for _ in range(16)] for _ in range(2)]
    G = []
    for g in range(2):
        d = Ctx()
        d.vtk, d.btk, d.ktk, d.AabT, d.ArbT, d.AakT, d.ArkT, d.Aab, d.Z, d.U = \
            [A.alloc(512, F32) for _ in range(10)]
        d.Xs = [A.alloc(512, F32) for _ in range(2)]
        d.XTs = [A.alloc(512, F32) for _ in range(2)]
        d.PTs = [A.alloc(512, F32) for _ in range(2)]
        d.S = A.alloc(512, F32)
        P.memset('pool', d.S, 0.0)
        d.yb = [A.alloc(8 * NB * 64, F32) for _ in range(2)]
        d.pc = 0
        G.append(d)
    ySv = X.yS.re("(h p) t -> p h t", p=64)

    def hsl(h):
        return slice(h * 64, (h + 1) * 64)

    for c in (range(X.rchunks) if 'main' in X.rparts else []):
        tb, cc = divmod(c, NB)
        if cc == 0:
            for hh in range(16):
                hp, m = divmod(hh, 2)
                P.dma('sp', blk[tb % 2][hh][0:64], X.RW[hp, m * 64:(m + 1) * 64, tb * NB * 320:(tb + 1) * NB * 320])
        for g in range(2):
            d = G[g]

            def nb():
                d.pc += 1
                return X.ps[g * 4 + d.pc % 4]

            def opnd(h, q):
                return blk[tb % 2][g * 8 + h][0:64].re("p (c q l) -> p c q l", q=5, l=64)[:, cc, q, :]

            for (q, dst, eng) in ((4, d.vtk, 'act'), (2, d.btk, 'act'), (3, d.ktk, 'act')):
                pt = nb()
                for h in range(8):
                    P.mm(pt[0:64, hsl(h)], opnd(h, q), idn)
                P.copy(eng, dst[0:64], pt[0:64])
            if X.rcut < 1:
                continue
            specs = ((2, 0, d.AabT, mS, 'dve'), (2, 1, d.ArbT, mI, 'dve'), (3, 0, d.AakT, mS, 'dve'),
                     (3, 1, d.ArkT, mI, 'dve'), (0, 2, d.Aab, mL, 'dve'))
            for (ql, qr, dst, mk_, eng) in specs:
                pa = nb()
                for h in range(8):
                    P.mm(pa[0:64, hsl(h)], opnd(h, ql), opnd(h, qr))
                if eng == 'dve':
                    P.tt('dve', dst[0:64], pa[0:64], mk_[0:64], ALU.mult)
                else:
                    P.copy('act', dst[0:64], pa[0:64])
                    P.tt('pool', dst[0:64], dst[0:64], mk_[0:64], ALU.mult)
            if X.rcut < 2:
                continue
            pz = nb()
            for h in range(8):
                P.mm(pz[0:64, hsl(h)], opnd(h, 0), d.S[0:64, hsl(h)], start=True, stop=False)
                P.mm(pz[0:64, hsl(h)], d.AakT[0:64, hsl(h)], d.vtk[0:64, hsl(h)], start=False, stop=True)
            P.copy('act', d.Z[0:64], pz[0:64])
            if X.rcut < 3:
                continue
            Xc, XTc = d.Aab, d.AabT
            PTc = d.PTs[0]
            P.tt('dve', PTc[0:64], d.AabT[0:64], idb[0:64], ALU.add)
            for lev in range(5):
                Xn, XTn, PTn = d.Xs[lev % 2], d.XTs[lev % 2], d.PTs[(lev + 1) % 2]
                px = nb()
                for h in range(8):
                    P.mm(px[0:64, hsl(h)], XTc[0:64, hsl(h)], Xc[0:64, hsl(h)])
                if lev < 4:
                    pxt = nb()
                    for h in range(8):
                        P.mm(pxt[0:64, hsl(h)], Xc[0:64, hsl(h)], XTc[0:64, hsl(h)])
                P.copy('act', Xn[0:64], px[0:64])
                if lev < 4:
                    P.copy('act', XTn[0:64], pxt[0:64])
                pp_ = nb()
                for h in range(8):
                    P.mm(pp_[0:64, hsl(h)], Xn[0:64, hsl(h)], PTc[0:64, hsl(h)])
                P.tt('dve', PTn[0:64], PTc[0:64], pp_[0:64], ALU.add)
                Xc, XTc, PTc = Xn, XTn, PTn
            if X.rcut < 4:
                continue
            pu = nb()
            for h in range(8):
                P.mm(pu[0:64, hsl(h)], PTc[0:64, hsl(h)], d.Z[0:64, hsl(h)])
            P.copy('act', d.U[0:64], pu[0:64])
            if X.rcut < 5:
                continue
            py = nb()
            for h in range(8):
                o = py[0:64, hsl(h)]
                P.mm(o, d.S[0:64, hsl(h)], opnd(h, 1), start=True, stop=False)
                P.mm(o, d.U[0:64, hsl(h)], d.ArbT[0:64, hsl(h)], start=False, stop=False)
                P.mm(o, d.vtk[0:64, hsl(h)], d.ArkT[0:64, hsl(h)], start=False, stop=True)
            yb = d.yb[tb % 2]
            P.copy('act', yb[0:64].re("p (a t) -> p a t", t=NB * 64)[:, :, cc * 64:(cc + 1) * 64],
                   py[0:64].re("p (a t) -> p a t", t=64))
            if X.rcut < 6:
                continue
            pS = nb()
            for h in range(8):
                o = pS[0:64, hsl(h)]
                P.mm(o, d.btk[0:64, hsl(h)], d.U[0:64, hsl(h)], start=True, stop=False)
                P.mm(o, d.ktk[0:64, hsl(h)], d.vtk[0:64, hsl(h)], start=False, stop=True)
            P.tt('dve', d.S[0:64], d.S[0:64], pS[0:64], ALU.add)
            wl = WLh[0:64].re("p (h c) -> p h c", c=64)[:, g * 8:(g + 1) * 8, c:c + 1]
            P.tt('dve', _c3(d.S[0:64]), _c3(d.S[0:64]), V(wl.ap.broadcast_to([64, 8, 64]), wl.buf), ALU.mult)
            if cc == NB - 1:
                P.dma('act', ySv[:, g * 8:(g + 1) * 8, tb * NB * 64:(tb + 1) * NB * 64],
                      yb[0:64].re("p (a t) -> p a t", t=NB * 64))

    P.barrier()
    A.reset()
    ys, rks, vs_, gs, dd, s2, t1 = [[A.alloc(512, F32) for _ in range(2)] for _ in range(7)]
    ob = [A.alloc(512, BF16) for _ in range(2)]
    it = 0
    for hp in (range(8) if 'post' in X.rparts else []):
        hc = slice(hp * 128, (hp + 1) * 128)
        h1 = slice(hp, hp + 1)
        for ti in range(T // 512):
            tsl = slice(ti * 512, (ti + 1) * 512)
            i2 = it % 2
            it += 1
            y, rk_, v_, g_, d_, q_, t_ = ys[i2], rks[i2], vs_[i2], gs[i2], dd[i2], s2[i2], t1[i2]
            P.dma('sp', y, X.yS[hc, tsl])
            P.dma('sp', rk_, X.rkS[hc, tsl])
            P.dma('sp', v_, X.vS[hc, tsl])
            P.dma('sp', g_, X.gS[hc, tsl])
            p1 = nps()
            P.mm(p1, BO64, y)
            P.tt('dve', d_, y, p1, ALU.subtract)
            P.tt('pool', q_, d_, d_, ALU.mult)
            p2 = nps()
            P.mm(p2, BO64, q_)
            P.ts('dve', q_, p2, B_LN_EPS, None, ALU.add)
            P.act(q_, q_, AF.Ln)
            P.act(q_, q_, AF.Exp, scale=-0.5)
            P.tt('dve', d_, d_, q_, ALU.mult)
            P.ts('dve', d_, d_, C('b_ln_w')[:, h1], C('b_ln_b')[:, h1], ALU.mult, ALU.add)
            p3 = nps()
            P.mm(p3, BO, rk_)
            P.tt('dve', t_, v_, p3, ALU.mult)
            P.tt('pool', d_, d_, t_, ALU.add)
            P.tt('pool', ob[i2], d_, g_, ALU.mult)
            P.dma('act', X.yT[1024 + hp * 128:1024 + (hp + 1) * 128, tsl], ob[i2])


def phase_mlstm(X):
    P, A = X.P, X.A
    C = X.C
    xin, xout = X.xB, X.xA
    phase_norm(X, xin, 'odd_norm', X.hT)
    P.barrier()
    A.reset()
    st = [A.alloc(512, F32) for _ in range(4)]
    cn = [0]

    def epi(j, t0, pss):
        s = st[cn[0] % 4]
        if cn[0] % 2 == 0:
            P.copy('act', s, pss[0])
        else:
            P.copy('dve', s, pss[0])
        cn[0] += 1
        P.dma('sp', X.xmz[j * 128:(j + 1) * 128, t0:t0 + 512], s)

    gemm(X, X.hT, 16, [X.w_oin], 64, 2048, epi)
    P.barrier()
    A.reset()
    mc = A.alloc(128 + 128 + 128 + 1024, F32)
    P.dma('sp', mc, X.mconst)
    mbd, maskC, identf, sel = mc[:, 0:128], mc[:, 128:256], mc[:, 256:384], mc[:, 384:1408]
    wif = A.alloc(1536, BF16)
    wifv = wif.re("p (j c g) -> p j c g", j=3, c=32)
    gI = A.alloc(T, F32)
    gF = A.alloc(T, F32)
    P.memset('pool', gI, 0.0)
    P.memset('pool', gF, 0.0)
    A.persist()
    load_cast(X, wif, X.c_w_if)
    xm2 = [A.alloc(3 + T, F32) for _ in range(2)]
    xc2 = [A.alloc(T, F32) for _ in range(2)]
    xmb2 = [A.alloc(T, BF16) for _ in range(2)]
    xcb2 = [A.alloc(T, BF16) for _ in range(2)]
    qb = A.alloc(T, BF16)
    kb = A.alloc(T, BF16)
    vb = A.alloc(T, BF16)
    vt = [A.alloc(512, BF16) for _ in range(2)]
    bd = [A.alloc(128, BF16) for _ in range(3)]
    P.memset('pool', xm2[0][:, 0:3], 0.0)
    P.memset('pool', xm2[1][:, 0:3], 0.0)
    cwt = C('c_conv_w')
    pc = [0]

    def nps():
        pc[0] += 1
        return X.ps[pc[0] % 8]

    vtv = X.vtok.re("(b p) f -> p b f", p=128)
    for c in range(32):
        xm, xc, xmb, xcb = xm2[c % 2], xc2[c % 2], xmb2[c % 2], xcb2[c % 2]
        P.dma('sp', xm[:, 3:3 + T], X.xmz[c * 128:(c + 1) * 128, :])
        P.act(xc, xm[:, 0:T], AF.Identity, bias=C('c_conv_b')[:, c:c + 1], scale=cwt[:, 4 * c:4 * c + 1])
        for j in range(1, 4):
            P.stt('dve', xc, xm[:, j:j + T], cwt[:, 4 * c + j:4 * c + j + 1], xc, ALU.mult, ALU.add)
        P.act(xc, xc, AF.Silu)
        P.copy('act', xcb, xc)
        P.copy('dve', xmb, xm[:, 3:3 + T])
        for i, nm in enumerate(('c_w_q', 'c_w_k', 'c_w_v')):
            w4 = C(nm)[:, 4 * c:4 * c + 4]
            wb_ = V(w4.ap.unsqueeze(1).broadcast_to([128, 32, 4]), w4.buf)
            P.tt('dve', bd[i].re("p (g j) -> p g j", j=4), mbd.re("p (g j) -> p g j", j=4), wb_, ALU.mult)
        for it in range(8):
            sl = slice(it * 512, (it + 1) * 512)
            p1 = nps()
            P.mm(p1, bd[0], xcb[:, sl])
            P.copy('act', qb[:, sl], p1)
            p2 = nps()
            P.mm(p2, bd[1], xcb[:, sl])
            P.copy('act', kb[:, sl], p2)
            p3 = nps()
            P.mm(p3, bd[2], xmb[:, sl])
            P.copy('dve', vb[:, sl], p3)
            p4 = nps()
            for b4 in range(4):
                tb2 = it * 4 + b4
                P.mm(p4[:, b4 * 128:(b4 + 1) * 128], xmb[:, tb2 * 128:(tb2 + 1) * 128], bd[2])
            v_ = vt[it % 2]
            P.copy('act', v_, p4)
            P.dma('act', vtv[:, it * 4:(it + 1) * 4, c * 128:(c + 1) * 128], v_.re("p (b f) -> p b f", f=128))
            for (gt, lo) in ((gI, 0), (gF, 8)):
                pg = nps()
                P.mm(pg[0:8, :], wifv[:, 0, c, lo:lo + 8], qb[:, sl], start=True, stop=False)
                P.mm(pg[0:8, :], wifv[:, 1, c, lo:lo + 8], kb[:, sl], start=False, stop=False)
                P.mm(pg[0:8, :], wifv[:, 2, c, lo:lo + 8], vb[:, sl], start=False, stop=True)
                P.tt('dve', gt[0:8, sl], gt[0:8, sl], pg[0:8, :], ALU.add)
        P.dma('act', X.qT[c * 128:(c + 1) * 128, :], qb)
        P.dma('act', X.kT[c * 128:(c + 1) * 128, :], kb)
        P.dma('act', X.xcT[c * 128:(c + 1) * 128, :], xcb)
    P.barrier()
    A.reset()
    CH = 128
    NCk = T // CH
    bF = A.alloc(1, F32)
    P.dma('sp', bF[0:8, :], X.bif_d[8:16, :])
    dec = A.alloc(NCk, F32)
    decb = A.alloc(8 * NCk, F32)
    identb = A.alloc(128, BF16)
    P.copy('dve', identb, identf)
    onesb = A.alloc(128, BF16)
    P.memset('dve', onesb, 1.0)
    A.persist()
    bif = C('c_b_if')
    m8 = A.alloc(T, F32)
    P.memset('dve', m8, 1.0)
    P.memset('dve', _c3(m8, CH)[:, :, 0:1], 0.0)
    lf = A.alloc(T, F32)
    bb = A.alloc(T, F32)
    aa = A.alloc(T, F32)
    eb = A.alloc(T, F32)
    ea = A.alloc(T, F32)
    P.act(lf[0:8], gF[0:8], AF.Sigmoid, bias=bF[0:8, 0:1])
    P.act(lf[0:8], lf[0:8], AF.Ln)
    P.scan(bb[0:8], m8[0:8], lf[0:8], 0.0, ALU.mult, ALU.add)
    P.ts('dve', aa[0:8], gI[0:8], bif[0:8, 0:1], None, ALU.add)
    P.tt('dve', aa[0:8], aa[0:8], bb[0:8], ALU.subtract)
    P.act(eb[0:8], bb[0:8], AF.Exp)
    P.act(ea[0:8], aa[0:8], AF.Exp)
    P.copy('dve', dec[0:8], eb[0:8, CH - 1:T:CH])
    for h in range(8):
        pd = nps()
        P.mm(pd[:, 0:NCk], sel[0:8, h * 128:(h + 1) * 128], dec[0:8, :])
        P.copy('act', decb[:, h * NCk:(h + 1) * NCk], pd[:, 0:NCk])
    ebb = [A.alloc(512, F32) for _ in range(2)]
    eab = [A.alloc(512, F32) for _ in range(2)]
    qk = [A.alloc(512, BF16) for _ in range(4)]
    n = 0
    for h in range(8):
        for it in range(8):
            sl = slice(it * 512, (it + 1) * 512)
            e1_, e2_ = ebb[it % 2], eab[it % 2]
            p1 = nps()
            P.mm(p1, sel[0:8, h * 128:(h + 1) * 128], eb[0:8, sl])
            P.copy('act', e1_, p1)
            p2 = nps()
            P.mm(p2, sel[0:8, h * 128:(h + 1) * 128], ea[0:8, sl])
            P.copy('act', e2_, p2)
            for kc in range(4):
                rows = slice(h * 512 + kc * 128, h * 512 + (kc + 1) * 128)
                t1_, t2_ = qk[n % 4], qk[(n + 1) % 4]
                n += 2
                P.dma('sp', t1_, X.qT[rows, sl])
                P.dma('sp', t2_, X.kT[rows, sl])
                P.tt('dve', t1_, t1_, e1_, ALU.mult)
                P.stt('dve', t2_, t2_, 512.0 ** -0.5, e2_, ALU.mult, ALU.mult)
                P.dma('act', X.q2T[rows, sl], t1_)
                P.dma('act', X.k2T[rows, sl], t2_)
    P.barrier()
    A.reset()
    CTs = [A.alloc(2048, F32) for _ in range(8)]
    CTbs = [A.alloc(2048, BF16) for _ in range(8)]
    nbs = [A.alloc(512, F32) for _ in range(8)]
    nbbs = [A.alloc(512, BF16) for _ in range(8)]
    NR = 3
    qt = [A.alloc(512, BF16) for _ in range(NR)]
    kt = [A.alloc(512, BF16) for _ in range(NR)]
    vk = [A.alloc(512, BF16) for _ in range(NR)]
    ktok = [A.alloc(512, BF16) for _ in range(NR)]
    STb = [A.alloc(128, BF16) for _ in range(NR)]
    rec = [A.alloc(128, F32) for _ in range(NR)]
    ho = [A.alloc(512, F32) for _ in range(NR)]
    tmpc = [A.alloc(512, F32) for _ in range(4)]
    vtr = X.vtok
    for h in range(8):
        P.memset('pool', CTs[h], 0.0)
        P.memset('pool', CTbs[h], 0.0)
        P.memset('pool', nbs[h], 0.0)
        P.memset('pool', nbbs[h], 0.0)
    q2v = X.q2T.re("(k p) t -> p k t", p=128)
    k2v = X.k2T.re("(k p) t -> p k t", p=128)
    hSv = X.hS.re("(k p) t -> p k t", p=128)
    it_ = 0
    tc_ = 0
    for c in range(NCk):
        tsl = slice(c * CH, (c + 1) * CH)
        for h in range(8):
            i2 = it_ % NR
            it_ += 1
            CT, CTb, nb_, nbb = CTs[h], CTbs[h], nbs[h], nbbs[h]
            q_, k_, v_, kk_, S_, r_, o_ = qt[i2], kt[i2], vk[i2], ktok[i2], STb[i2], rec[i2], ho[i2]
            P.dma('sp', q_.re("p (k t) -> p k t", t=CH), q2v[:, h * 4:(h + 1) * 4, tsl])
            P.dma('sp', k_.re("p (k t) -> p k t", t=CH), k2v[:, h * 4:(h + 1) * 4, tsl])
            P.dma('sp', v_, vtr[tsl, h * 512:(h + 1) * 512])
            pk = nps()
            for kc in range(4):
                P.mm(pk[:, kc * 128:(kc + 1) * 128], k_[:, kc * 128:(kc + 1) * 128], identb)
            P.copy('act', kk_, pk)
            ps_ = nps()
            for kc in range(4):
                P.mm(ps_[:, 0:128], k_[:, kc * 128:(kc + 1) * 128], q_[:, kc * 128:(kc + 1) * 128],
                     start=(kc == 0), stop=(kc == 3))
            P.tt('dve', S_, ps_[:, 0:128], maskC, ALU.mult)
            pn = nps()
            for vc in range(4):
                o = pn[:, vc * 128:(vc + 1) * 128]
                P.mm(o, v_[:, vc * 128:(vc + 1) * 128], S_, start=True, stop=False)
                for kc in range(4):
                    P.mm(o, CTb[:, kc * 512 + vc * 128:kc * 512 + (vc + 1) * 128], q_[:, kc * 128:(kc + 1) * 128],
                         start=False, stop=(kc == 3))
            pdn = nps()
            P.mm(pdn[:, 0:128], onesb, S_, start=True, stop=False)
            for kc in range(4):
                P.mm(pdn[:, 0:128], nbb[:, kc * 128:(kc + 1) * 128], q_[:, kc * 128:(kc + 1) * 128],
                     start=False, stop=(kc == 3))
            P.act(r_, pdn[:, 0:128], AF.Abs)
            P.ts('dve', r_, r_, 1.0, None, ALU.max)
            P.recip(r_, r_)
            P.tt('dve', o_.re("p (a t) -> p a t", t=128), pn.re("p (a t) -> p a t", t=128),
                 V(r_.ap.unsqueeze(1).broadcast_to([128, 4, 128]), r_.buf), ALU.mult)
            P.dma('sp', hSv[:, h * 4:(h + 1) * 4, tsl], o_.re("p (a t) -> p a t", t=128))
            dcol = decb[:, h * NCk + c:h * NCk + c + 1]
            for kc in range(4):
                pc_ = nps()
                P.mm(pc_, kk_[:, kc * 128:(kc + 1) * 128], v_)
                csl = slice(kc * 512, (kc + 1) * 512)
                tm = tmpc[tc_ % 4]
                tc_ += 1
                P.act(tm, pc_, AF.Copy, scale=dcol)
                P.stt('dve', CT[:, csl], CT[:, csl], dcol, tm, ALU.mult, ALU.add)
                P.copy('act', CTb[:, csl], CT[:, csl])
            pnn = nps()
            for kc in range(4):
                P.mm(pnn[:, kc * 128:(kc + 1) * 128], kk_[:, kc * 128:(kc + 1) * 128], onesb)
            tm = tmpc[tc_ % 4]
            tc_ += 1
            P.act(tm, pnn, AF.Copy, scale=dcol)
            P.stt('dve', nb_, nb_, dcol, tm, ALU.mult, ALU.add)
            P.copy('act', nbb, nb_)
    P.barrier()
    A.base = X.base0
    A.reset()
    o512 = A.alloc(128, F32)
    P.memset('dve', o512, 1.0 / 512)
    hb = [A.alloc(2048, F32) for _ in range(2)]
    dq = [A.alloc(2048, F32) for _ in range(2)]
    rs_ = [A.alloc(512, F32) for _ in range(2)]
    zb = [A.alloc(512, F32) for _ in range(2)]
    xcl = [A.alloc(512, BF16) for _ in range(2)]
    xcf = [A.alloc(512, F32) for _ in range(2)]
    ob = [A.alloc(512, BF16) for _ in range(2)]
    n = 0
    for h in range(8):
        for it in range(8):
            sl = slice(it * 512, (it + 1) * 512)
            i2 = n % 2
            n += 1
            hb_, d_, r_ = hb[i2], dq[i2], rs_[i2]
            P.dma('sp', hb_.re("p (k t) -> p k t", t=512), X.hS.re("(k p) t -> p k t", p=128)[:, h * 4:(h + 1) * 4, sl])
            pm = nps()
            for vc in range(4):
                P.mm(pm, o512, hb_[:, vc * 512:(vc + 1) * 512], start=(vc == 0), stop=(vc == 3))
            for vc in range(4):
                P.tt('dve', d_[:, vc * 512:(vc + 1) * 512], hb_[:, vc * 512:(vc + 1) * 512], pm, ALU.subtract)
            P.act(hb_, d_, AF.Square)
            pv = nps()
            for vc in range(4):
                P.mm(pv, o512, hb_[:, vc * 512:(vc + 1) * 512], start=(vc == 0), stop=(vc == 3))
            P.ts('dve', r_, pv, EPS, None, ALU.add)
            P.act(r_, r_, AF.Ln)
            P.act(r_, r_, AF.Exp, scale=-0.5)
            for vc in range(4):
                cidx = h * 4 + vc
                rows = slice(cidx * 128, (cidx + 1) * 128)
                j2 = (n * 4 + vc) % 2
                P.stt('dve', d_[:, vc * 512:(vc + 1) * 512], d_[:, vc * 512:(vc + 1) * 512], C('c_ln_w')[:, cidx:cidx + 1],
                      r_, ALU.mult, ALU.mult)
                P.dma('sp', xcl[j2], X.xcT[rows, sl])
                P.dma('sp', zb[j2], X.xmz[4096 + cidx * 128:4096 + (cidx + 1) * 128, sl])
                P.copy('act', xcf[j2], xcl[j2])
                P.stt('dve', xcf[j2], xcf[j2], C('c_skip')[:, cidx:cidx + 1], d_[:, vc * 512:(vc + 1) * 512], ALU.mult, ALU.add)
                P.act(zb[j2], zb[j2], AF.Silu)
                P.tt('dve', ob[j2], xcf[j2], zb[j2], ALU.mult)
                P.dma('act', X.hsT[rows, sl], ob[j2])
    A.base = X.base0
    phase_resid_gemm(X, X.hsT, 32, X.w_oout, 1024, xin, xout)
```

```python
from contextlib import ExitStack
import numpy as np
import concourse.bass as bass
import concourse.mybir as mybir
from concourse.bass_utils import run_bass_kernel_spmd

F32 = mybir.dt.float32
BF16 = mybir.dt.bfloat16
AF = mybir.ActivationFunctionType
ALU = mybir.AluOpType
AX = mybir.AxisListType

ENGS = ('pe', 'act', 'dve', 'pool', 'sp')


class Buf:
    __slots__ = ('name', 'kind', 'lw', 'rd', 'semw', 'semr', 'cw', 'cr')

    def __init__(self, name, kind):
        self.name = name
        self.kind = kind
        self.lw = None
        self.rd = {}
        self.semw = None
        self.semr = None
        self.cw = 0
        self.cr = 0


class V:
    __slots__ = ('ap', 'buf')

    def __init__(self, ap, buf):
        self.ap = ap
        self.buf = buf

    def __getitem__(self, idx):
        return V(self.ap[idx], self.buf)

    def re(self, pattern, **kw):
        return V(self.ap.rearrange(pattern, **kw), self.buf)

    def sub(self, name_unused, idx):
        return V(self.ap[idx], self.buf)


def _ap(x):
    return x.ap if isinstance(x, V) else x


class Prog:
    def __init__(self, nc):
        self.nc = nc
        self.stack = ExitStack()
        self.ins = []
        self.nsem = 0
        self.npsum = 0
        self.last_eng = {}
        self.last_dma = {}
        self.bar = set()
        self.sem_pool = []
        self.sem_active = []

    def dram(self, name, shape, dtype, kind="Internal"):
        t = self.nc.dram_tensor(name, list(shape), dtype, kind=kind)
        return V(t.ap(), Buf(name, 'dram'))

    def sbuf(self, name, shape, dtype, nbuf=None):
        t = self.stack.enter_context(self.nc.sbuf_tensor(name, list(shape), dtype))
        return V(t[:], Buf(name, 'sbuf'))

    def psum(self, name, shape=(128, 512), dtype=F32):
        t = self.stack.enter_context(self.nc.psum_tensor(name, list(shape), dtype))
        return V(t[:], Buf(name, 'psum'))

    def view(self, v, name):
        return V(v.ap, Buf(name, v.buf.kind))

    def _sem(self, name):
        self.nsem += 1
        return self.stack.enter_context(self.nc.semaphore(name))

    def emit(self, eng, fn, reads, writes, dma=None):
        iid = len(self.ins)
        deps = set(self.bar)
        wb = []
        for v in writes:
            b = v.buf
            if b in wb:
                continue
            wb.append(b)
            if b.lw is not None:
                deps.add(b.lw)
            deps.update(b.rd.values())
        rb = []
        for v in reads:
            if not isinstance(v, V):
                continue
            b = v.buf
            if b in wb or b in rb:
                continue
            rb.append(b)
            if b.lw is not None:
                deps.add(b.lw)
        key = eng
        dsem = None
        if dma is not None:
            kind, sb = dma
            if kind == 'w':
                if sb.semw is None:
                    sb.semw, sb.cw = self._take_sem("dw_" + sb.name)
                    self.sem_active.append((sb, 'w'))
                sb.cw += 16
                dsem = (sb.semw, sb.cw)
            else:
                if sb.semr is None:
                    sb.semr, sb.cr = self._take_sem("dr_" + sb.name)
                    self.sem_active.append((sb, 'r'))
                sb.cr += 16
                dsem = (sb.semr, sb.cr)
            key = ('dma', id(dsem[0]))
        for b in wb:
            b.lw = iid
            b.rd = {}
        for b in rb:
            b.rd[key] = iid
        if dsem is None:
            self.last_eng[eng] = iid
        else:
            self.last_dma[id(dsem[0])] = iid
        self.ins.append((eng, fn, deps, dsem))
        return iid

    def _take_sem(self, name):
        if self.sem_pool:
            return self.sem_pool.pop()
        return self._sem(name), 0

    def barrier(self):
        self.bar = set(self.last_eng.values()) | set(self.last_dma.values())
        for (b, kind) in self.sem_active:
            if kind == 'w':
                self.sem_pool.append((b.semw, b.cw))
                b.semw = None
            else:
                self.sem_pool.append((b.semr, b.cr))
                b.semr = None
        self.sem_active = []

    def dma(self, q, out, in_):
        ob, ib = out.buf, in_.buf
        if ob.kind == 'sbuf':
            d = ('w', ob)
        else:
            assert ib.kind == 'sbuf', "dram->dram dma not supported"
            d = ('r', ib)
        o, i = out.ap, in_.ap
        return self.emit(q, lambda e: e.dma_start(out=o, in_=i), [in_], [out], dma=d)

    def mm(self, out, lhsT, rhs, start=True, stop=True, **kw):
        o, l, r = out.ap, lhsT.ap, rhs.ap
        return self.emit('pe', lambda e: e.matmul(o, l, r, start=start, stop=stop, **kw),
                         [lhsT, rhs], [out])

    def transpose(self, out, in_, ident):
        o, i, d = out.ap, in_.ap, ident.ap
        return self.emit('pe', lambda e: e.transpose(o, i, d), [in_, ident], [out])

    def act(self, out, in_, func, bias=None, scale=1.0, accum=None, eng='act'):
        o, i = out.ap, in_.ap
        b, s, a = _ap(bias), _ap(scale), _ap(accum)
        kw = {}
        if b is not None:
            kw['bias'] = b
        if a is not None:
            kw['accum_out'] = a
        w = [out] + ([accum] if accum is not None else [])
        return self.emit(eng, lambda e: e.activation(out=o, in_=i, func=func, scale=s, **kw),
                         [in_, bias, scale], w)

    def tt(self, eng, out, in0, in1, op):
        o, a, b = out.ap, in0.ap, in1.ap
        return self.emit(eng, lambda e: e.tensor_tensor(out=o, in0=a, in1=b, op=op), [in0, in1], [out])

    def ts(self, eng, out, in0, s1, s2, op0, op1=None, accum=None):
        o, a = out.ap, in0.ap
        x1, x2, ac = _ap(s1), _ap(s2), _ap(accum)
        kw = {}
        if op1 is not None:
            kw['op1'] = op1
        if ac is not None:
            kw['accum_out'] = ac
        w = [out] + ([accum] if accum is not None else [])
        return self.emit(eng, lambda e: e.tensor_scalar(out=o, in0=a, scalar1=x1, scalar2=x2, op0=op0, **kw),
                         [in0, s1, s2], w)

    def stt(self, eng, out, in0, scalar, in1, op0, op1):
        o, a, b, s = out.ap, in0.ap, in1.ap, _ap(scalar)
        return self.emit(eng, lambda e: e.scalar_tensor_tensor(out=o, in0=a, scalar=s, in1=b, op0=op0, op1=op1),
                         [in0, in1, scalar], [out])

    def scan(self, out, d0, d1, init, op0, op1):
        o, a, b, i = out.ap, d0.ap, d1.ap, _ap(init)
        return self.emit('dve', lambda e: e.tensor_tensor_scan(out=o, data0=a, data1=b, initial=i, op0=op0, op1=op1),
                         [d0, d1, init], [out])

    def copy(self, eng, out, in_):
        o, i = out.ap, in_.ap
        if eng == 'act':
            return self.emit(eng, lambda e: e.copy(out=o, in_=i), [in_], [out])
        return self.emit(eng, lambda e: e.tensor_copy(out=o, in_=i), [in_], [out])

    def memset(self, eng, out, val):
        o = out.ap
        return self.emit(eng, lambda e: e.memset(o, val), [], [out])

    def reduce(self, out, in_, op, axis=AX.X, eng='dve'):
        o, i = out.ap, in_.ap
        return self.emit(eng, lambda e: e.tensor_reduce(out=o, in_=i, axis=axis, op=op), [in_], [out])

    def recip(self, out, in_):
        o, i = out.ap, in_.ap
        return self.emit('dve', lambda e: e.reciprocal(out=o, in_=i), [in_], [out])

    def affine_select(self, out, in_, pattern, cmp, fill, base, cm):
        o, i = out.ap, in_.ap
        return self.emit('pool', lambda e: e.affine_select(out=o, in_=i, pattern=pattern, compare_op=cmp,
                                                           fill=fill, base=base, channel_multiplier=cm),
                         [in_], [out])

    def finish(self, final_wait=True):
        nc = self.nc
        ins = self.ins
        n = len(ins)
        needed = [False] * n
        for (eng, fn, deps, dsem) in ins:
            for d in deps:
                if ins[d][0] == 'pe' and eng == 'pe' and ins[d][3] is None:
                    continue
                needed[d] = True
        esem = {e: self._sem("c_" + e) for e in ('pe', 'act', 'dve', 'pool')}
        ecnt = {e: 0 for e in esem}
        token = [None] * n
        known = {e: {} for e in ENGS}
        snap = [None] * n
        stream = {e: [] for e in ENGS}
        for iid, (eng, fn, deps, dsem) in enumerate(ins):
            kn = known[eng]
            waits = {}
            for d in deps:
                if ins[d][0] == 'pe' and eng == 'pe' and ins[d][3] is None:
                    continue
                sem, val = token[d]
                sid = id(sem)
                if kn.get(sid, 0) >= val:
                    continue
                if sid not in waits or waits[sid][1] < val:
                    waits[sid] = (sem, val)
            for d in deps:
                s = snap[d]
                if s is not None and token[d] is not None and id(token[d][0]) in waits:
                    for k2, v2 in s.items():
                        if kn.get(k2, 0) < v2:
                            kn[k2] = v2
            wl = []
            for sid, (sem, val) in waits.items():
                if kn.get(sid, 0) >= val:
                    continue
                kn[sid] = val
                wl.append((sem, val))
            inc = None
            if dsem is not None:
                token[iid] = dsem
                inc = (dsem[0], 16)
            elif needed[iid]:
                ecnt[eng] += 1
                token[iid] = (esem[eng], ecnt[eng])
                inc = (esem[eng], 1)
                kn[id(esem[eng])] = max(kn.get(id(esem[eng]), 0), 0)
            if needed[iid] or dsem is not None:
                snap[iid] = dict(kn)
            stream[eng].append((wl, fn, inc))
        finals = []
        seen = set()
        for (eng, fn, deps, dsem) in ins:
            if dsem is not None:
                seen.add(id(dsem[0]))
        allbufs = {}
        for (eng, fn, deps, dsem) in ins:
            if dsem is not None:
                sid = id(dsem[0])
                if sid not in allbufs or allbufs[sid][1] < dsem[1]:
                    allbufs[sid] = dsem
        finals = list(allbufs.values())
        self.stats = {e: len(stream[e]) for e in ENGS}
        self.stats['waits'] = sum(len(w) for e in ENGS for (w, _, _) in stream[e])
        self.stats['sems'] = self.nsem

        with nc.Block() as block:
            def run(e, name):
                for (wl, fn, inc) in stream[name]:
                    for (sem, val) in wl:
                        e.wait_ge(sem, val)
                    r = fn(e)
                    if inc is not None:
                        r.then_inc(inc[0], inc[1])
                if name == 'sp' and final_wait:
                    for (sem, val) in finals:
                        e.wait_ge(sem, val)
                    for en in esem:
                        if ecnt[en] > 0:
                            e.wait_ge(esem[en], ecnt[en])

            @block.sync
            def _(e):
                run(e, 'sp')

            @block.scalar
            def _(e):
                run(e, 'act')

            @block.vector
            def _(e):
                run(e, 'dve')

            @block.gpsimd
            def _(e):
                run(e, 'pool')

            @block.tensor
            def _(e):
                run(e, 'pe')
        self.stack.close()
        return nc


T = 4096
D = 2048
EPS = 1e-6
J_EIN = 43
P_ROWS = 5408
DFF = 5632
KFF = 44
CW = 4096
B_LN_EPS = 64e-5


def _cc(v, pad_to=None):
    v = np.asarray(v, np.float32).reshape(-1)
    if pad_to is not None and v.size < pad_to:
        v = np.concatenate([v, np.zeros(pad_to - v.size, np.float32)])
    return np.ascontiguousarray(v.reshape(-1, 128).T)


def _tile_w(w, J):
    K, M = w.shape
    kc = K // 128
    wp = np.zeros((K, J * 128), np.float32)
    wp[:, :M] = w
    return np.ascontiguousarray(wp.reshape(kc, 128, J, 128).transpose(2, 1, 0, 3).reshape(J, 128, kc * 128))


def pack_consts(inp):
    ent = []

    def add(name, arr):
        ent.append((name, np.asarray(arr, np.float32)))

    add('even_norm', _cc(inp['even_norm'][0]))
    add('ffn_norm0', _cc(inp['ffn_norm'][0]))
    add('ffn_norm1', _cc(inp['ffn_norm'][1]))
    add('odd_norm', _cc(inp['odd_norm'][0]))
    add('final_norm', _cc(inp['final_norm']))
    add('a_conv_w', inp['a_conv_w'][0].reshape(4, 8, 128).transpose(2, 1, 0).reshape(128, 32))
    for nm in ('a_conv_b', 'a_b_r', 'a_b_i', 'a_lambda'):
        add(nm, _cc(inp[nm][0]))
    add('b_mu', _cc(inp['b_mu'][0], 27 * 128))
    for nm in ('b_w0', 'b_a0', 'b_k_k', 'b_k_a', 'b_r_k', 'b_ln_w', 'b_ln_b'):
        add(nm, _cc(inp[nm][0]))
    for l in range(2):
        add('ffn_conv_w%d' % l, inp['ffn_conv_w'][l].reshape(3, KFF, 128).transpose(2, 1, 0).reshape(128, KFF * 3))
        add('ffn_conv_b%d' % l, _cc(inp['ffn_conv_b'][l]))
    add('c_conv_w', inp['c_conv_w'][0].reshape(4, 32, 128).transpose(2, 1, 0).reshape(128, 128))
    for nm in ('c_conv_b', 'c_ln_w', 'c_skip'):
        add(nm, _cc(inp[nm][0]))
    for nm in ('c_w_q', 'c_w_k', 'c_w_v'):
        add(nm, inp[nm][0].reshape(32, 128, 4).transpose(1, 0, 2).reshape(128, 128))
    bif = np.zeros((128, 1), np.float32)
    bif[:16, 0] = inp['c_b_if'][0]
    add('c_b_if', bif)
    offs = {}
    o = 0
    for name, a in ent:
        offs[name] = (o, a.shape[1])
        o += a.shape[1]
    return np.ascontiguousarray(np.concatenate([a for _, a in ent], axis=1)), offs


def const_offsets():
    dummy = {
        'even_norm': np.zeros((1, D)), 'ffn_norm': np.zeros((2, D)), 'odd_norm': np.zeros((1, D)),
        'final_norm': np.zeros(D), 'a_conv_w': np.zeros((1, 4, 1024)),
        'b_mu': np.zeros((1, 3360)), 'ffn_conv_w': np.zeros((2, 3, DFF)), 'ffn_conv_b': np.zeros((2, DFF)),
        'c_conv_w': np.zeros((1, 4, CW)), 'c_b_if': np.zeros((1, 16)),
    }
    for nm in ('a_conv_b', 'a_b_r', 'a_b_i', 'a_lambda', 'b_w0', 'b_a0', 'b_k_k', 'b_k_a', 'b_r_k', 'b_ln_w', 'b_ln_b'):
        dummy[nm] = np.zeros((1, 1024))
    for nm in ('c_conv_b', 'c_ln_w', 'c_skip'):
        dummy[nm] = np.zeros((1, CW))
    for nm in ('c_w_q', 'c_w_k', 'c_w_v'):
        dummy[nm] = np.zeros((1, 1024, 4, 4))
    c, offs = pack_consts(dummy)
    return c.shape[1], offs


class Arena:
    def __init__(self, P, cols):
        self.P = P
        self.t = P.sbuf("arena", [128, cols], F32)
        self.cols = cols
        self.base = 0
        self.off = 0
        self.n = 0

    def reset(self):
        self.off = self.base

    def persist(self):
        self.base = self.off

    def alloc(self, cols, dtype=F32, name=None):
        n32 = cols if dtype == F32 else (cols + 1) // 2
        a = self.off
        self.off += n32
        assert self.off <= self.cols, ("arena overflow", self.off, self.cols)
        ap = self.t.ap[:, a:a + n32]
        if dtype != F32:
            ap = ap.bitcast(dtype)[:, 0:cols]
        self.n += 1
        return V(ap, Buf(name or ("ar%d" % self.n), 'sbuf'))


class Ctx:
    pass


def load_cast(X, dst, src, rows=128):
    cols = dst.ap.shape[1]
    stg = X.A.alloc(cols, F32)
    X.P.dma('sp', stg[0:rows], src)
    X.P.copy('act', dst[0:rows], stg[0:rows])


def split_groups(n, g):
    g = min(g, n)
    base, rem = divmod(n, g)
    out = []
    a = 0
    for i in range(g):
        b = a + base + (1 if i < rem else 0)
        out.append((a, b))
        a = b
    return out


def gemm(X, src, KC, wsets, J, TB, epi, jlist=None):
    P, A = X.P, X.A
    ns = len(wsets)
    groups = split_groups(KC, 4)
    act = [A.alloc((b - a) * TB, BF16) for (a, b) in groups]
    wb = [[A.alloc(KC * 128, BF16) for _ in range(2)] for _ in range(ns)]
    wst = [[A.alloc(KC * 128, F32) for _ in range(2)] for _ in range(ns)]
    srcv = src.re("(k p) t -> p k t", p=128)
    nsub = TB // 512
    cnt = 0
    jl = list(range(J)) if jlist is None else jlist
    its = [(tb, ji) for tb in range(T // TB) for ji in range(len(jl))]

    def wload(n):
        tb_, ji_ = its[n]
        for s in range(ns):
            P.dma('sp', wst[s][n % 2], wsets[s][jl[ji_]])
            P.copy('dve' if (n + s) % 2 == 0 else 'act', wb[s][n % 2], wst[s][n % 2])

    wload(0)
    for n, (tb, ji) in enumerate(its):
        j = jl[ji]
        if ji == 0:
            for gi, (a, b) in enumerate(groups):
                P.dma('sp', act[gi].re("p (k t) -> p k t", t=TB), srcv[:, a:b, tb * TB:(tb + 1) * TB])
        if n + 1 < len(its):
            wload(n + 1)
        for sub in range(nsub):
            pss = []
            for s in range(ns):
                ps = X.ps[(cnt % (6 // ns)) * ns + s]
                pss.append(ps)
                for gi, (a, b) in enumerate(groups):
                    for k in range(a, b):
                        P.mm(ps, wb[s][n % 2][:, k * 128:(k + 1) * 128],
                             act[gi][:, (k - a) * TB + sub * 512:(k - a) * TB + (sub + 1) * 512],
                             start=(k == 0), stop=(k == KC - 1))
            cnt += 1
            epi(j, tb * TB + sub * 512, pss)


def phase_norm(X, src, gname, dst, out_f32=False):
    P, A = X.P, X.A
    P.barrier()
    A.reset()
    g = X.C(gname)
    odt = F32 if out_f32 else BF16
    xs = [A.alloc(16 * 512, F32) for _ in range(2)]
    sq = [A.alloc(16 * 512, BF16) for _ in range(2)]
    hs = [A.alloc(16 * 512, odt) for _ in range(2)]
    rs = [A.alloc(512, F32) for _ in range(2)]
    sv = src.re("(k p) t -> p k t", p=128)
    dv = dst.re("(k p) t -> p k t", p=128)
    for it in range(T // 512):
        x, q, h, r = xs[it % 2], sq[it % 2], hs[it % 2], rs[it % 2]
        P.dma('sp', x.re("p (k t) -> p k t", t=512), sv[:, :, it * 512:(it + 1) * 512])
        P.act(q, x, AF.Square)
        ps = X.ps[6 + it % 2]
        for k in range(16):
            P.mm(ps, X.ones_bf, q[:, k * 512:(k + 1) * 512], start=(k == 0), stop=(k == 15))
        P.ts('dve', r, ps, EPS, None, ALU.add)
        P.act(r, r, AF.Ln)
        P.act(r, r, AF.Exp, scale=-0.5)
        for k in range(16):
            P.stt('dve', h[:, k * 512:(k + 1) * 512], x[:, k * 512:(k + 1) * 512],
                  g[:, k:k + 1], r, ALU.mult, ALU.mult)
        P.dma('act', dv[:, :, it * 512:(it + 1) * 512], h.re("p (k t) -> p k t", t=512))


def phase_even_inproj(X):
    P, A = X.P, X.A
    P.barrier()
    A.reset()
    st = [A.alloc(512, F32) for _ in range(4)]
    c = [0]

    def epi(j, t0, pss):
        s = st[c[0] % 4]
        if c[0] % 2 == 0:
            P.copy('act', s, pss[0])
        else:
            P.copy('dve', s, pss[0])
        c[0] += 1
        rows = min(128, P_ROWS - j * 128)
        P.dma('sp', X.pT[j * 128:j * 128 + rows, t0:t0 + 512], s[0:rows, :])

    gemm(X, X.hT, 16, [X.w_ein], J_EIN, 2048, epi)


def phase_resid_gemm(X, src, KC, w, TB, xin, xout):
    P, A = X.P, X.A
    P.barrier()
    A.reset()
    st = [A.alloc(512, F32) for _ in range(4)]
    c = [0]

    def epi(j, t0, pss):
        s = st[c[0] % 4]
        c[0] += 1
        P.dma('sp', s, xin[j * 128:(j + 1) * 128, t0:t0 + 512])
        P.tt('dve', s, s, pss[0], ALU.add)
        P.dma('sp', xout[j * 128:(j + 1) * 128, t0:t0 + 512], s)

    gemm(X, src, KC, [w], 16, TB, epi)


def phase_ffn_up(X, l):
    P, A = X.P, X.A
    P.barrier()
    A.reset()
    cw = X.C('ffn_conv_w%d' % l)
    cb = X.C('ffn_conv_b%d' % l)
    gb = [A.alloc(514, F32) for _ in range(3)]
    acc = [A.alloc(512, F32) for _ in range(2)]
    tmp = [A.alloc(512, F32) for _ in range(2)]
    ub = [A.alloc(512, BF16) for _ in range(3)]
    halo = A.alloc(KFF * 2, F32)
    c = [0]
    TB = 2048

    def epi(j, t0, pss):
        i = c[0]
        c[0] += 1
        g = gb[i % 3]
        gprev = gb[(i - 1) % 3]
        a = acc[i % 2]
        u = ub[i % 3]
        P.copy('act', g[:, 2:514], pss[0])
        if t0 == 0:
            P.memset('dve', g[:, 0:2], 0.0)
        elif t0 % TB == 0:
            P.copy('dve', g[:, 0:2], halo[:, 2 * j:2 * j + 2])
        else:
            P.copy('dve', g[:, 0:2], gprev[:, 512:514])
        if (t0 + 512) % TB == 0:
            P.copy('dve', halo[:, 2 * j:2 * j + 2], g[:, 512:514])
        P.act(a, g[:, 0:512], AF.Identity, bias=cb[:, j:j + 1], scale=cw[:, 3 * j:3 * j + 1])
        P.stt('dve', a, g[:, 1:513], cw[:, 3 * j + 1:3 * j + 2], a, ALU.mult, ALU.add)
        P.stt('dve', a, g[:, 2:514], cw[:, 3 * j + 2:3 * j + 3], a, ALU.mult, ALU.add)
        P.act(a, a, AF.Silu)
        P.tt('dve', u, a, pss[1], ALU.mult)
        P.dma('sp', X.uT[j * 128:(j + 1) * 128, t0:t0 + 512], u)

    gemm(X, X.hT, 16, [X.w_gate[l], X.w_up[l]], KFF, TB, epi)


def phase_rglru(X):
    P, A = X.P, X.A
    P.barrier()
    A.reset()
    wr = A.alloc(8 * 128, BF16)
    wi = A.alloc(8 * 128, BF16)
    load_cast(X, wr, X.a_w_r)
    load_cast(X, wi, X.a_w_i)
    cl = A.alloc(8, F32)
    cl2 = A.alloc(8, F32)
    P.act(cl, X.C('a_lambda'), AF.Exp, scale=-1.0)
    P.ts('dve', cl, cl, 1.0, None, ALU.add)
    P.act(cl, cl, AF.Ln)
    P.ts('dve', cl2, cl, -16.0, None, ALU.mult)
    P.ts('dve', cl, cl, -8.0, None, ALU.mult)
    xa = A.alloc(3 + T, F32)
    ga = A.alloc(T, F32)
    xc = A.alloc(T, F32)
    xcb = A.alloc(T, BF16)
    r = A.alloc(T, F32)
    ig = A.alloc(T, F32)
    a = A.alloc(T, F32)
    s = A.alloc(T, F32)
    yb = A.alloc(T, BF16)
    P.memset('pool', xa[:, 0:3], 0.0)
    cwt = X.C('a_conv_w')
    for c in range(8):
        P.dma('sp', xa[:, 3:3 + T], X.pT[c * 128:(c + 1) * 128, :])
        P.dma('sp', ga, X.pT[(8 + c) * 128:(9 + c) * 128, :])
        P.ts('dve', xc, xa[:, 0:T], cwt[:, 4 * c:4 * c + 1], X.C('a_conv_b')[:, c:c + 1], ALU.mult, ALU.add)
        for j in range(1, 4):
            P.stt('dve', xc, xa[:, j:j + T], cwt[:, 4 * c + j:4 * c + j + 1], xc, ALU.mult, ALU.add)
        P.copy('pool', xcb, xc)
        for it in range(8):
            sl = slice(it * 512, (it + 1) * 512)
            p1 = X.ps[(2 * it) % 6]
            p2 = X.ps[(2 * it + 1) % 6]
            P.mm(p1, wr[:, c * 128:(c + 1) * 128], xcb[:, sl])
            P.mm(p2, wi[:, c * 128:(c + 1) * 128], xcb[:, sl])
            P.act(r[:, sl], p1, AF.Sigmoid, bias=X.C('a_b_r')[:, c:c + 1])
            P.act(ig[:, sl], p2, AF.Sigmoid, bias=X.C('a_b_i')[:, c:c + 1])
        P.act(a, r, AF.Exp, scale=cl[:, c:c + 1])
        P.act(s, r, AF.Exp, scale=cl2[:, c:c + 1])
        P.ts('dve', s, s, -1.0, 1.0, ALU.mult, ALU.add)
        P.act(s, s, AF.Sqrt)
        P.tt('pool', ig, ig, xc, ALU.mult)
        P.tt('dve', s, s, ig, ALU.mult)
        P.scan(r, a, s, 0.0, ALU.mult, ALU.add)
        P.tt('pool', a, ga, ga, ALU.mult)
        P.ts('dve', a, a, 0.044715, 1.0, ALU.mult, ALU.add)
        P.tt('pool', a, a, ga, ALU.mult)
        P.act(a, a, AF.Tanh, scale=0.7978845608028654)
        P.ts('dve', a, a, 1.0, 0.5, ALU.add, ALU.mult)
        P.tt('pool', a, a, ga, ALU.mult)
        P.tt('dve', yb, a, r, ALU.mult)
        P.dma('act', X.yT[c * 128:(c + 1) * 128, :], yb)


def build(stages=('all',), dbg=(), rparts=('prep', 'main', 'post'), rchunks=T // 64):
    nc = bass.Bass("TRN2", target_bir_lowering=False)
    P = Prog(nc)
    X = Ctx()
    X.P = P
    X.rparts = rparts
    import os as _os
    X.rcut = int(_os.environ.get('RCUT', '9'))
    X.rchunks = rchunks
    ncst, offs = const_offsets()

    def dr(name, shape, dtype=F32, kind="Internal"):
        if name in dbg:
            kind = "ExternalOutput"
        return P.dram(name, shape, dtype, kind=kind)

    ein = "ExternalInput"
    X.x0 = dr("xT", [D, T], F32, ein)
    cst_d = dr("cst", [128, ncst], F32, ein)
    X.w_ein = dr("w_ein", [J_EIN, 128, 2048], F32, ein)
    X.w_eout = dr("w_eout", [16, 128, 2048], F32, ein)
    X.w_gate = [dr("w_gate%d" % l, [KFF, 128, 2048], F32, ein) for l in range(2)]
    X.w_up = [dr("w_up%d" % l, [KFF, 128, 2048], F32, ein) for l in range(2)]
    X.w_down = [dr("w_down%d" % l, [16, 128, DFF], F32, ein) for l in range(2)]
    X.w_oin = dr("w_oin", [64, 128, 2048], F32, ein)
    X.w_oout = dr("w_oout", [16, 128, CW], F32, ein)
    X.a_w_r = dr("a_w_r", [128, 1024], F32, ein)
    X.a_w_i = dr("a_w_i", [128, 1024], F32, ein)
    X.wlr = dr("wlr", [128, 1024], F32, ein)
    X.gup1 = dr("gup1", [128, 1024], F32, ein)
    X.gup2 = dr("gup2", [32, 1024], F32, ein)
    X.c_w_if = dr("c_w_if", [128, 3 * 32 * 16], F32, ein)
    X.masks = dr("masks", [128, 128 + 4 * 512], F32, ein)
    X.out = dr("out", [D, T], F32, "ExternalOutput")
    X.hT = dr("hT", [D, T], BF16)
    X.pT = dr("pT", [P_ROWS, T], F32)
    X.yT = dr("yT", [D, T], BF16)
    X.uT = dr("uT", [DFF, T], BF16)
    X.xA = dr("xA", [D, T], F32)
    X.RW = dr("RW", [8, 128, 64 * 320], BF16)
    X.gS = dr("gS", [1024, T], F32)
    X.WLS = dr("WLS", [1024, 64], F32)
    X.xmz = dr("xmz", [8192, T], F32)
    X.qT = dr("qT", [CW, T], BF16)
    X.kT = dr("kT", [CW, T], BF16)
    X.q2T = dr("q2T", [CW, T], BF16)
    X.k2T = dr("k2T", [CW, T], BF16)
    X.xcT = dr("xcT", [CW, T], BF16)
    X.vtok = dr("vtok", [T, CW], BF16)
    X.hS = dr("hS", [CW, T], F32)
    X.hsT = dr("hsT", [CW, T], BF16)
    X.mconst = dr("mconst", [128, 1408], F32, ein)
    X.bif_d = dr("bif_d", [16, 1], F32, ein)
    X.rkS = dr("rkS", [1024, T], F32)
    X.vS = dr("vS", [1024, T], F32)
    X.yS = dr("yS", [1024, T], F32)
    X.xB = dr("xB", [D, T], F32)

    A = Arena(P, 50432)
    X.A = A
    X.ps = [P.psum("ps%d" % i) for i in range(8)]
    cst = A.alloc(ncst, F32, "cst")
    P.dma('sp', cst, cst_d)
    X.C = lambda name: cst[:, offs[name][0]:offs[name][0] + offs[name][1]]
    X.ones_bf = A.alloc(128, BF16, "ones")
    P.memset('dve', X.ones_bf, 1.0 / D)
    A.persist()
    X.base0 = A.base

    def on(s):
        return 'all' in stages or s in stages

    if on('e_norm'):
        phase_norm(X, X.x0, 'even_norm', X.hT)
    if on('e_in'):
        phase_even_inproj(X)
    if on('rglru'):
        phase_rglru(X)
    if on('rwkv'):
        phase_rwkv(X)
        A.base = X.base0
    if on('e_out'):
        phase_resid_gemm(X, X.yT, 16, X.w_eout, 2048, X.x0, X.xA)
    if on('ffn0'):
        phase_norm(X, X.xA, 'ffn_norm0', X.hT)
        phase_ffn_up(X, 0)
        phase_resid_gemm(X, X.uT, KFF, X.w_down[0], 1024, X.xA, X.xB)
    if on('mlstm'):
        phase_mlstm(X)
    if on('ffn1'):
        phase_norm(X, X.xA, 'ffn_norm1', X.hT)
        phase_ffn_up(X, 1)
        phase_resid_gemm(X, X.uT, KFF, X.w_down[1], 1024, X.xA, X.xB)
    if on('final'):
        phase_norm(X, X.xB, 'final_norm', X.out, out_f32=True)
    P.finish()
    X.stats = P.stats
    return nc, X


def host_masks():
    i = np.arange(128)[:, None]
    t = np.arange(64)[None, :]
    ident = (np.arange(128)[:, None] == np.arange(128)[None, :]).astype(np.float32)
    mS = np.tile((t > i).astype(np.float32), (1, 8))
    mI = np.tile((t >= i).astype(np.float32), (1, 8))
    mL = np.tile((i > t).astype(np.float32), (1, 8))
    idb = np.tile((t == i).astype(np.float32), (1, 8))
    return np.ascontiguousarray(np.concatenate([ident, mS, mI, mL, idb], axis=1))


def host_mconst():
    p = np.arange(128)[:, None]
    c = np.arange(128)[None, :]
    mbd = ((p // 4) == (c // 4)).astype(np.float32)
    maskC = (p <= c).astype(np.float32)
    ident = (p == c).astype(np.float32)
    sel = np.zeros((128, 1024), np.float32)
    for h in range(8):
        sel[h, h * 128:(h + 1) * 128] = 1.0
    return np.ascontiguousarray(np.concatenate([mbd, maskC, ident, sel], axis=1))


def host_inputs(inp):
    cst, _ = pack_consts(inp)
    sh = {
        'cst': cst,
        'masks': host_masks(),
        'mconst': host_mconst(),
        'bif_d': np.ascontiguousarray(inp['c_b_if'][0].reshape(16, 1)),
        'w_ein': _tile_w(inp['even_w_in'][0], J_EIN),
        'w_eout': _tile_w(inp['even_w_out'][0], 16),
        'w_oin': _tile_w(inp['odd_w_in'][0], 64),
        'w_oout': _tile_w(inp['odd_w_out'][0], 16),
        'a_w_r': np.ascontiguousarray(inp['a_w_r'][0].transpose(1, 0, 2).reshape(128, 1024)),
        'a_w_i': np.ascontiguousarray(inp['a_w_i'][0].transpose(1, 0, 2).reshape(128, 1024)),
        'wlr': np.ascontiguousarray(np.concatenate([inp['b_w_up'][0], inp['b_a_up'][0]], axis=0)),
        'gup1': np.ascontiguousarray(inp['b_g_up'][0][:128]),
        'gup2': np.ascontiguousarray(inp['b_g_up'][0][128:160]),
        'c_w_if': np.ascontiguousarray(inp['c_w_if'][0].reshape(3, 32, 128, 16).transpose(2, 0, 1, 3).reshape(128, 1536)),
    }
    for l in range(2):
        sh['w_gate%d' % l] = _tile_w(inp['ffn_w_gate'][l], KFF)
        sh['w_up%d' % l] = _tile_w(inp['ffn_w_up'][l], KFF)
        sh['w_down%d' % l] = _tile_w(inp['ffn_w_down'][l], 16)
    return sh


def kernel(**inputs):
    inp = {k: np.asarray(v) for k, v in inputs.items()}
    x = inp['x']
    B = x.shape[0]
    nc, X = build()
    sh = host_inputs(inp)
    in_maps = []
    for b in range(B):
        m = dict(sh)
        m['xT'] = np.ascontiguousarray(x[b].T)
        in_maps.append(m)
    res = run_bass_kernel_spmd(nc, in_maps, core_ids=list(range(B)))
    out = np.stack([np.ascontiguousarray(r['out'].T) for r in res.results], axis=0)
    return out.astype(np.float32)


def _c3(v, l=64):
    return v.re("p (c l) -> p c l", l=l)


def phase_rwkv(X):
    P, A = X.P, X.A
    P.barrier()
    A.reset()
    C = X.C
    TT = 1024
    NCH = TT // 64
    wlr = A.alloc(1024, BF16)
    gu1 = A.alloc(1024, BF16)
    gu2 = A.alloc(1024, BF16)
    BO = A.alloc(128, F32)
    BO64 = A.alloc(128, F32)
    for (t_, val) in ((BO, 1.0), (BO64, 1.0 / 64)):
        P.memset('dve', t_, 0.0)
        P.memset('dve', t_[0:64, 0:64], val)
        P.memset('dve', t_[64:128, 64:128], val)
    WL = A.alloc(8 * 64, F32)
    omk = A.alloc(8, F32)
    P.ts('dve', omk, C('b_k_a'), -1.0, 1.0, ALU.mult, ALU.add)
    A.persist()
    load_cast(X, wlr, X.wlr)
    load_cast(X, gu1, X.gup1)
    load_cast(X, gu2, X.gup2, rows=32)
    mask = A.alloc(TT, F32)
    P.memset('dve', mask, 1.0)
    P.memset('dve', _c3(mask)[:, :, 0:1], 0.0)
    xbs = [A.alloc(TT + 1, F32) for _ in range(4)]
    x4 = A.alloc(TT, F32)
    rr = A.alloc(TT, F32)
    k0 = A.alloc(TT, F32)
    vv = A.alloc(TT, F32)
    xwa = A.alloc(TT, BF16)
    sg1 = A.alloc(TT, BF16)
    sg2 = A.alloc(TT, BF16)
    sig, aic, gst, kk, sq, rn, km, bv, lw, cw, cwx, e1, e2, e3, rk = [A.alloc(TT, F32) for _ in range(15)]
    O = A.alloc(NCH * 320, BF16)
    Ov = O.re("p (c q l) -> p c q l", q=5, l=64)
    mu = C('b_mu')
    pc = [0]

    def nps():
        pc[0] += 1
        return X.ps[pc[0] % 8]

    def lerp(dst, xb, chunk, nrows, t0):
        r0 = chunk * 128
        if t0 == 0:
            P.memset('pool', xb[0:nrows, 0:1], 0.0)
            P.dma('sp', xb[0:nrows, 1:1 + TT], X.pT[r0:r0 + nrows, t0:t0 + TT])
        else:
            P.dma('sp', xb[0:nrows, 0:1 + TT], X.pT[r0:r0 + nrows, t0 - 1:t0 + TT])
        P.tt('dve', dst[0:nrows], xb[0:nrows, 0:TT], xb[0:nrows, 1:1 + TT], ALU.subtract)
        P.stt('dve', dst[0:nrows], dst[0:nrows], mu[0:nrows, chunk - 16:chunk - 15], xb[0:nrows, 1:1 + TT],
              ALU.mult, ALU.add)

    for tt in (range(T // TT) if 'prep' in X.rparts else []):
        t0 = tt * TT
        lerp(x4, xbs[0], 40, 128, t0)
        P.act(xwa[0:64], x4[0:64], AF.Tanh)
        P.copy('pool', xwa[64:128], x4[64:128])
        lerp(x4, xbs[0], 41, 128, t0)
        P.act(sg1, x4, AF.Sigmoid)
        lerp(x4, xbs[0], 42, 32, t0)
        P.act(sg2[0:32], x4[0:32], AF.Sigmoid)
        for hp in range(8):
            hc = slice(hp * 128, (hp + 1) * 128)
            h1 = slice(hp, hp + 1)
            lerp(rr, xbs[1], 16 + hp, 128, t0)
            lerp(k0, xbs[2], 24 + hp, 128, t0)
            lerp(vv, xbs[3], 32 + hp, 128, t0)
            for sub in range(TT // 512):
                sl = slice(sub * 512, (sub + 1) * 512)
                pz = nps()
                P.mm(pz, wlr[0:64, hc], xwa[0:64, sl])
                P.act(sig[:, sl], pz, AF.Sigmoid, bias=C('b_w0')[:, h1])
                pa = nps()
                P.mm(pa, wlr[64:128, hc], xwa[64:128, sl])
                P.act(aic[:, sl], pa, AF.Sigmoid, bias=C('b_a0')[:, h1])
                pg = nps()
                P.mm(pg, gu1[:, hc], sg1[:, sl], start=True, stop=False)
                P.mm(pg, gu2[0:32, hc], sg2[0:32, sl], start=False, stop=True)
                P.copy('act', gst[:, sl], pg)
            P.dma('act', X.gS[hc, t0:t0 + TT], gst)
            P.ts('dve', kk, k0, C('b_k_k')[:, h1], None, ALU.mult)
            P.act(sq, kk, AF.Square)
            for sub in range(TT // 512):
                sl = slice(sub * 512, (sub + 1) * 512)
                pq = nps()
                P.mm(pq, BO, sq[:, sl])
                P.ts('dve', rn[:, sl], pq, 1e-12, None, ALU.max)
            P.act(rn, rn, AF.Ln)
            P.act(rn, rn, AF.Exp, scale=-0.5)
            P.tt('dve', kk, kk, rn, ALU.mult)
            P.ts('dve', km, aic, C('b_k_a')[:, h1], omk[:, h1], ALU.mult, ALU.add)
            P.tt('pool', km, km, k0, ALU.mult)
            P.tt('pool', bv, kk, aic, ALU.mult)
            P.ts('dve', lw, sig, -0.6065306597126334, None, ALU.mult)
            P.scan(cw, mask, lw, 0.0, ALU.mult, ALU.add)
            P.tt('pool', cwx, cw, lw, ALU.subtract)
            P.act(e1, cw, AF.Exp)
            P.act(e2, cwx, AF.Exp)
            P.act(e3, cw, AF.Exp, scale=-1.0)
            P.stt('dve', Ov[:, :, 0, :], _c3(kk), -1.0, _c3(e2), ALU.mult, ALU.mult)
            P.tt('pool', Ov[:, :, 1, :], _c3(rr), _c3(e1), ALU.mult)
            P.tt('dve', Ov[:, :, 2, :], _c3(bv), _c3(e3), ALU.mult)
            P.tt('pool', Ov[:, :, 3, :], _c3(km), _c3(e3), ALU.mult)
            P.copy('act', Ov[:, :, 4, :], _c3(vv))
            wlt = WL[:, hp * 64 + tt * NCH:hp * 64 + (tt + 1) * NCH]
            P.copy('pool', wlt, e1[:, 63:TT:64])
            P.dma('act', X.WLS[hc, tt * NCH:(tt + 1) * NCH], wlt)
            P.tt('dve', rk, rr, km, ALU.mult)
            P.ts('dve', rk, rk, C('b_r_k')[:, h1], None, ALU.mult)
            P.dma('act', X.rkS[hc, t0:t0 + TT], rk)
            P.dma('act', X.vS[hc, t0:t0 + TT], vv)
            P.dma('act', X.RW[hp, :, tt * NCH * 320:(tt + 1) * NCH * 320], O)

    P.barrier()
    A.reset()
    mk = A.alloc(128 + 4 * 512, F32)
    P.dma('sp', mk, X.masks)
    ident = mk[:, 0:128]
    mS = mk[:, 128:640]
    mI = mk[:, 640:1152]
    mL = mk[:, 1152:1664]
    idb = mk[:, 1664:2176]
    NB = 2
    idn = ident[0:64, 0:64]
    WLh = A.alloc(16 * 64, F32)
    P.dma('sp', WLh[0:64].re("p (h c) -> p h c", c=64), X.WLS.re("(h p) c -> p h c", p=64))
    blk = [[A.alloc(NB * 320, BF16) for _ in range(16)] for _ in range(2)]
    idnb = A.alloc(64, BF16)
    P.copy('dve', idnb[0:64], idn)
    G = []
    for g in range(2):
        d = Ctx()
        d.vtk, d.btk, d.ktk, d.ArbT, d.AakT, d.ArkT, d.U, d.Sb = [A.alloc(512, BF16) for _ in range(8)]
        d.AabT, d.Aab, d.Z = [A.alloc(512, F32) for _ in range(3)]
        P.memset('pool', d.Sb, 0.0)
        d.Xs = [A.alloc(512, F32) for _ in range(2)]
        d.XTs = [A.alloc(512, F32) for _ in range(2)]
        d.PTs = [A.alloc(512, F32) for _ in range(2)]
        d.S = A.alloc(512, F32)
        P.memset('pool', d.S, 0.0)
        d.yb = [A.alloc(8 * NB * 64, F32) for _ in range(2)]
        d.pc = 0
        G.append(d)
    ySv = X.yS.re("(h p) t -> p h t", p=64)

    def hsl(h):
        return slice(h * 64, (h + 1) * 64)

    for c in (range(X.rchunks) if 'main' in X.rparts else []):
        tb, cc = divmod(c, NB)
        if cc == 0:
            for hh in range(16):
                hp, m = divmod(hh, 2)
                P.dma('sp', blk[tb % 2][hh][0:64], X.RW[hp, m * 64:(m + 1) * 64, tb * NB * 320:(tb + 1) * NB * 320])
        for g in range(2):
            d = G[g]

            def nb():
                d.pc += 1
                return X.ps[g * 4 + d.pc % 4]

            def opnd(h, q):
                return blk[tb % 2][g * 8 + h][0:64].re("p (c q l) -> p c q l", q=5, l=64)[:, cc, q, :]

            for (q, dst, eng) in ((4, d.vtk, 'act'), (2, d.btk, 'act'), (3, d.ktk, 'act')):
                pt = nb()
                for h in range(8):
                    P.mm(pt[0:64, hsl(h)], opnd(h, q), idnb[0:64])
                P.copy(eng, dst[0:64], pt[0:64])
            if X.rcut < 1:
                continue
            specs = ((2, 0, d.AabT, mS, 'dve'), (2, 1, d.ArbT, mI, 'dve'), (3, 0, d.AakT, mS, 'dve'),
                     (3, 1, d.ArkT, mI, 'dve'), (0, 2, d.Aab, mL, 'dve'))
            for (ql, qr, dst, mk_, eng) in specs:
                pa = nb()
                for h in range(8):
                    P.mm(pa[0:64, hsl(h)], opnd(h, ql), opnd(h, qr))
                if eng == 'dve':
                    P.tt('dve', dst[0:64], pa[0:64], mk_[0:64], ALU.mult)
                else:
                    P.copy('act', dst[0:64], pa[0:64])
                    P.tt('pool', dst[0:64], dst[0:64], mk_[0:64], ALU.mult)
            if X.rcut < 2:
                continue
            pz = nb()
            for h in range(8):
                P.mm(pz[0:64, hsl(h)], opnd(h, 0), d.Sb[0:64, hsl(h)], start=True, stop=False)
                P.mm(pz[0:64, hsl(h)], d.AakT[0:64, hsl(h)], d.vtk[0:64, hsl(h)], start=False, stop=True)
            P.copy('act', d.Z[0:64], pz[0:64])
            if X.rcut < 3:
                continue
            Xc, XTc = d.Aab, d.AabT
            PTc = d.PTs[0]
            P.tt('dve', PTc[0:64], d.AabT[0:64], idb[0:64], ALU.add)
            for lev in range(5):
                Xn, XTn, PTn = d.Xs[lev % 2], d.XTs[lev % 2], d.PTs[(lev + 1) % 2]
                px = nb()
                for h in range(8):
                    P.mm(px[0:64, hsl(h)], XTc[0:64, hsl(h)], Xc[0:64, hsl(h)])
                if lev < 4:
                    pxt = nb()
                    for h in range(8):
                        P.mm(pxt[0:64, hsl(h)], Xc[0:64, hsl(h)], XTc[0:64, hsl(h)])
                P.copy('act', Xn[0:64], px[0:64])
                if lev < 4:
                    P.copy('act', XTn[0:64], pxt[0:64])
                pp_ = nb()
                for h in range(8):
                    P.mm(pp_[0:64, hsl(h)], Xn[0:64, hsl(h)], PTc[0:64, hsl(h)])
                P.tt('dve', PTn[0:64], PTc[0:64], pp_[0:64], ALU.add)
                Xc, XTc, PTc = Xn, XTn, PTn
            if X.rcut < 4:
                continue
            pu = nb()
            for h in range(8):
                P.mm(pu[0:64, hsl(h)], PTc[0:64, hsl(h)], d.Z[0:64, hsl(h)])
            P.copy('act', d.U[0:64], pu[0:64])
            if X.rcut < 5:
                continue
            py = nb()
            for h in range(8):
                o = py[0:64, hsl(h)]
                P.mm(o, d.Sb[0:64, hsl(h)], opnd(h, 1), start=True, stop=False)
                P.mm(o, d.U[0:64, hsl(h)], d.ArbT[0:64, hsl(h)], start=False, stop=False)
                P.mm(o, d.vtk[0:64, hsl(h)], d.ArkT[0:64, hsl(h)], start=False, stop=True)
            yb = d.yb[tb % 2]
            P.copy('act', yb[0:64].re("p (a t) -> p a t", t=NB * 64)[:, :, cc * 64:(cc + 1) * 64],
                   py[0:64].re("p (a t) -> p a t", t=64))
            if X.rcut < 6:
                continue
            pS = nb()
            for h in range(8):
                o = pS[0:64, hsl(h)]
                P.mm(o, d.btk[0:64, hsl(h)], d.U[0:64, hsl(h)], start=True, stop=False)
                P.mm(o, d.ktk[0:64, hsl(h)], d.vtk[0:64, hsl(h)], start=False, stop=True)
            P.tt('dve', d.S[0:64], d.S[0:64], pS[0:64], ALU.add)
            wl = WLh[0:64].re("p (h c) -> p h c", c=64)[:, g * 8:(g + 1) * 8, c:c + 1]
            P.tt('dve', _c3(d.S[0:64]), _c3(d.S[0:64]), V(wl.ap.broadcast_to([64, 8, 64]), wl.buf), ALU.mult)
            P.copy('act', d.Sb[0:64], d.S[0:64])
            if cc == NB - 1:
                P.dma('act', ySv[:, g * 8:(g + 1) * 8, tb * NB * 64:(tb + 1) * NB * 64],
                      yb[0:64].re("p (a t) -> p a t", t=NB * 64))

    P.barrier()
    A.reset()
    ys, rks, vs_, gs, dd, s2, t1 = [[A.alloc(512, F32) for _ in range(2)] for _ in range(7)]
    ob = [A.alloc(512, BF16) for _ in range(2)]
    it = 0
    for hp in (range(8) if 'post' in X.rparts else []):
        hc = slice(hp * 128, (hp + 1) * 128)
        h1 = slice(hp, hp + 1)
        for ti in range(T // 512):
            tsl = slice(ti * 512, (ti + 1) * 512)
            i2 = it % 2
            it += 1
            y, rk_, v_, g_, d_, q_, t_ = ys[i2], rks[i2], vs_[i2], gs[i2], dd[i2], s2[i2], t1[i2]
            P.dma('sp', y, X.yS[hc, tsl])
            P.dma('sp', rk_, X.rkS[hc, tsl])
            P.dma('sp', v_, X.vS[hc, tsl])
            P.dma('sp', g_, X.gS[hc, tsl])
            p1 = nps()
            P.mm(p1, BO64, y)
            P.tt('dve', d_, y, p1, ALU.subtract)
            P.tt('pool', q_, d_, d_, ALU.mult)
            p2 = nps()
            P.mm(p2, BO64, q_)
            P.ts('dve', q_, p2, B_LN_EPS, None, ALU.add)
            P.act(q_, q_, AF.Ln)
            P.act(q_, q_, AF.Exp, scale=-0.5)
            P.tt('dve', d_, d_, q_, ALU.mult)
            P.ts('dve', d_, d_, C('b_ln_w')[:, h1], C('b_ln_b')[:, h1], ALU.mult, ALU.add)
            p3 = nps()
            P.mm(p3, BO, rk_)
            P.tt('dve', t_, v_, p3, ALU.mult)
            P.tt('pool', d_, d_, t_, ALU.add)
            P.tt('pool', ob[i2], d_, g_, ALU.mult)
            P.dma('act', X.yT[1024 + hp * 128:1024 + (hp + 1) * 128, tsl], ob[i2])


def phase_mlstm(X):
    P, A = X.P, X.A
    C = X.C
    xin, xout = X.xB, X.xA
    phase_norm(X, xin, 'odd_norm', X.hT)
    P.barrier()
    A.reset()
    st = [A.alloc(512, F32) for _ in range(4)]
    cn = [0]

    def epi(j, t0, pss):
        s = st[cn[0] % 4]
        if cn[0] % 2 == 0:
            P.copy('act', s, pss[0])
        else:
            P.copy('dve', s, pss[0])
        cn[0] += 1
        P.dma('sp', X.xmz[j * 128:(j + 1) * 128, t0:t0 + 512], s)

    gemm(X, X.hT, 16, [X.w_oin], 64, 2048, epi)
    P.barrier()
    A.reset()
    mc = A.alloc(128 + 128 + 128 + 1024, F32)
    P.dma('sp', mc, X.mconst)
    mbd, maskC, identf, sel = mc[:, 0:128], mc[:, 128:256], mc[:, 256:384], mc[:, 384:1408]
    wif = A.alloc(1536, BF16)
    wifv = wif.re("p (j c g) -> p j c g", j=3, c=32)
    gI = A.alloc(T, F32)
    gF = A.alloc(T, F32)
    P.memset('pool', gI, 0.0)
    P.memset('pool', gF, 0.0)
    A.persist()
    load_cast(X, wif, X.c_w_if)
    xm2 = [A.alloc(3 + T, F32) for _ in range(2)]
    xc2 = [A.alloc(T, F32) for _ in range(2)]
    xmb2 = [A.alloc(T, BF16) for _ in range(2)]
    xcb2 = [A.alloc(T, BF16) for _ in range(2)]
    qb = A.alloc(T, BF16)
    kb = A.alloc(T, BF16)
    vb = A.alloc(T, BF16)
    vt = [A.alloc(512, BF16) for _ in range(2)]
    bd = [A.alloc(128, BF16) for _ in range(3)]
    P.memset('pool', xm2[0][:, 0:3], 0.0)
    P.memset('pool', xm2[1][:, 0:3], 0.0)
    cwt = C('c_conv_w')
    pc = [0]

    def nps():
        pc[0] += 1
        return X.ps[pc[0] % 8]

    vtv = X.vtok.re("(b p) f -> p b f", p=128)
    for c in range(32):
        xm, xc, xmb, xcb = xm2[c % 2], xc2[c % 2], xmb2[c % 2], xcb2[c % 2]
        P.dma('sp', xm[:, 3:3 + T], X.xmz[c * 128:(c + 1) * 128, :])
        P.act(xc, xm[:, 0:T], AF.Identity, bias=C('c_conv_b')[:, c:c + 1], scale=cwt[:, 4 * c:4 * c + 1])
        for j in range(1, 4):
            P.stt('dve', xc, xm[:, j:j + T], cwt[:, 4 * c + j:4 * c + j + 1], xc, ALU.mult, ALU.add)
        P.act(xc, xc, AF.Silu)
        P.copy('act', xcb, xc)
        P.copy('dve', xmb, xm[:, 3:3 + T])
        for i, nm in enumerate(('c_w_q', 'c_w_k', 'c_w_v')):
            w4 = C(nm)[:, 4 * c:4 * c + 4]
            wb_ = V(w4.ap.unsqueeze(1).broadcast_to([128, 32, 4]), w4.buf)
            P.tt('dve', bd[i].re("p (g j) -> p g j", j=4), mbd.re("p (g j) -> p g j", j=4), wb_, ALU.mult)
        for it in range(8):
            sl = slice(it * 512, (it + 1) * 512)
            p1 = nps()
            P.mm(p1, bd[0], xcb[:, sl])
            P.copy('act', qb[:, sl], p1)
            p2 = nps()
            P.mm(p2, bd[1], xcb[:, sl])
            P.copy('act', kb[:, sl], p2)
            p3 = nps()
            P.mm(p3, bd[2], xmb[:, sl])
            P.copy('dve', vb[:, sl], p3)
            p4 = nps()
            for b4 in range(4):
                tb2 = it * 4 + b4
                P.mm(p4[:, b4 * 128:(b4 + 1) * 128], xmb[:, tb2 * 128:(tb2 + 1) * 128], bd[2])
            v_ = vt[it % 2]
            P.copy('act', v_, p4)
            P.dma('act', vtv[:, it * 4:(it + 1) * 4, c * 128:(c + 1) * 128], v_.re("p (b f) -> p b f", f=128))
            for (gt, lo) in ((gI, 0), (gF, 8)):
                pg = nps()
                P.mm(pg[0:8, :], wifv[:, 0, c, lo:lo + 8], qb[:, sl], start=True, stop=False)
                P.mm(pg[0:8, :], wifv[:, 1, c, lo:lo + 8], kb[:, sl], start=False, stop=False)
                P.mm(pg[0:8, :], wifv[:, 2, c, lo:lo + 8], vb[:, sl], start=False, stop=True)
                P.tt('dve', gt[0:8, sl], gt[0:8, sl], pg[0:8, :], ALU.add)
        P.dma('act', X.qT[c * 128:(c + 1) * 128, :], qb)
        P.dma('act', X.kT[c * 128:(c + 1) * 128, :], kb)
        P.dma('act', X.xcT[c * 128:(c + 1) * 128, :], xcb)
    P.barrier()
    A.reset()
    CH = 128
    NCk = T // CH
    bF = A.alloc(1, F32)
    P.dma('sp', bF[0:8, :], X.bif_d[8:16, :])
    dec = A.alloc(NCk, F32)
    decb = A.alloc(8 * NCk, F32)
    identb = A.alloc(128, BF16)
    P.copy('dve', identb, identf)
    onesb = A.alloc(128, BF16)
    P.memset('dve', onesb, 1.0)
    A.persist()
    bif = C('c_b_if')
    m8 = A.alloc(T, F32)
    P.memset('dve', m8, 1.0)
    P.memset('dve', _c3(m8, CH)[:, :, 0:1], 0.0)
    lf = A.alloc(T, F32)
    bb = A.alloc(T, F32)
    aa = A.alloc(T, F32)
    eb = A.alloc(T, F32)
    ea = A.alloc(T, F32)
    P.act(lf[0:8], gF[0:8], AF.Sigmoid, bias=bF[0:8, 0:1])
    P.act(lf[0:8], lf[0:8], AF.Ln)
    P.scan(bb[0:8], m8[0:8], lf[0:8], 0.0, ALU.mult, ALU.add)
    P.ts('dve', aa[0:8], gI[0:8], bif[0:8, 0:1], None, ALU.add)
    P.tt('dve', aa[0:8], aa[0:8], bb[0:8], ALU.subtract)
    P.act(eb[0:8], bb[0:8], AF.Exp)
    P.act(ea[0:8], aa[0:8], AF.Exp)
    P.copy('dve', dec[0:8], eb[0:8, CH - 1:T:CH])
    for h in range(8):
        pd = nps()
        P.mm(pd[:, 0:NCk], sel[0:8, h * 128:(h + 1) * 128], dec[0:8, :])
        P.copy('act', decb[:, h * NCk:(h + 1) * NCk], pd[:, 0:NCk])
    ebb = [A.alloc(512, F32) for _ in range(2)]
    eab = [A.alloc(512, F32) for _ in range(2)]
    qk = [A.alloc(512, BF16) for _ in range(4)]
    n = 0
    for h in range(8):
        for it in range(8):
            sl = slice(it * 512, (it + 1) * 512)
            e1_, e2_ = ebb[it % 2], eab[it % 2]
            p1 = nps()
            P.mm(p1, sel[0:8, h * 128:(h + 1) * 128], eb[0:8, sl])
            P.copy('act', e1_, p1)
            p2 = nps()
            P.mm(p2, sel[0:8, h * 128:(h + 1) * 128], ea[0:8, sl])
            P.copy('act', e2_, p2)
            for kc in range(4):
                rows = slice(h * 512 + kc * 128, h * 512 + (kc + 1) * 128)
                t1_, t2_ = qk[n % 4], qk[(n + 1) % 4]
                n += 2
                P.dma('sp', t1_, X.qT[rows, sl])
                P.dma('sp', t2_, X.kT[rows, sl])
                P.tt('dve', t1_, t1_, e1_, ALU.mult)
                P.stt('dve', t2_, t2_, 512.0 ** -0.5, e2_, ALU.mult, ALU.mult)
                P.dma('act', X.q2T[rows, sl], t1_)
                P.dma('act', X.k2T[rows, sl], t2_)
    P.barrier()
    A.reset()
    CTs = [A.alloc(2048, F32) for _ in range(8)]
    CTbs = [A.alloc(2048, BF16) for _ in range(8)]
    nbs = [A.alloc(512, F32) for _ in range(8)]
    nbbs = [A.alloc(512, BF16) for _ in range(8)]
    NR = 3
    qt = [A.alloc(512, BF16) for _ in range(NR)]
    kt = [A.alloc(512, BF16) for _ in range(NR)]
    vk = [A.alloc(512, BF16) for _ in range(NR)]
    ktok = [A.alloc(512, BF16) for _ in range(NR)]
    STb = [A.alloc(128, BF16) for _ in range(NR)]
    rec = [A.alloc(128, F32) for _ in range(NR)]
    ho = [A.alloc(512, F32) for _ in range(NR)]
    tmpc = [A.alloc(512, F32) for _ in range(4)]
    vtr = X.vtok
    for h in range(8):
        P.memset('pool', CTs[h], 0.0)
        P.memset('pool', CTbs[h], 0.0)
        P.memset('pool', nbs[h], 0.0)
        P.memset('pool', nbbs[h], 0.0)
    q2v = X.q2T.re("(k p) t -> p k t", p=128)
    k2v = X.k2T.re("(k p) t -> p k t", p=128)
    hSv = X.hS.re("(k p) t -> p k t", p=128)
    it_ = 0
    tc_ = 0
    for c in range(NCk):
        tsl = slice(c * CH, (c + 1) * CH)
        for h in range(8):
            i2 = it_ % NR
            it_ += 1
            CT, CTb, nb_, nbb = CTs[h], CTbs[h], nbs[h], nbbs[h]
            q_, k_, v_, kk_, S_, r_, o_ = qt[i2], kt[i2], vk[i2], ktok[i2], STb[i2], rec[i2], ho[i2]
            P.dma('sp', q_.re("p (k t) -> p k t", t=CH), q2v[:, h * 4:(h + 1) * 4, tsl])
            P.dma('sp', k_.re("p (k t) -> p k t", t=CH), k2v[:, h * 4:(h + 1) * 4, tsl])
            P.dma('sp', v_, vtr[tsl, h * 512:(h + 1) * 512])
            pk = nps()
            for kc in range(4):
                P.mm(pk[:, kc * 128:(kc + 1) * 128], k_[:, kc * 128:(kc + 1) * 128], identb)
            P.copy('act', kk_, pk)
            ps_ = nps()
            for kc in range(4):
                P.mm(ps_[:, 0:128], k_[:, kc * 128:(kc + 1) * 128], q_[:, kc * 128:(kc + 1) * 128],
                     start=(kc == 0), stop=(kc == 3))
            P.tt('dve', S_, ps_[:, 0:128], maskC, ALU.mult)
            pn = nps()
            for vc in range(4):
                o = pn[:, vc * 128:(vc + 1) * 128]
                P.mm(o, v_[:, vc * 128:(vc + 1) * 128], S_, start=True, stop=False)
                for kc in range(4):
                    P.mm(o, CTb[:, kc * 512 + vc * 128:kc * 512 + (vc + 1) * 128], q_[:, kc * 128:(kc + 1) * 128],
                         start=False, stop=(kc == 3))
            pdn = nps()
            P.mm(pdn[:, 0:128], onesb, S_, start=True, stop=False)
            for kc in range(4):
                P.mm(pdn[:, 0:128], nbb[:, kc * 128:(kc + 1) * 128], q_[:, kc * 128:(kc + 1) * 128],
                     start=False, stop=(kc == 3))
            P.act(r_, pdn[:, 0:128], AF.Abs)
            P.ts('dve', r_, r_, 1.0, None, ALU.max)
            P.recip(r_, r_)
            P.tt('dve', o_.re("p (a t) -> p a t", t=128), pn.re("p (a t) -> p a t", t=128),
                 V(r_.ap.unsqueeze(1).broadcast_to([128, 4, 128]), r_.buf), ALU.mult)
            P.dma('sp', hSv[:, h * 4:(h + 1) * 4, tsl], o_.re("p (a t) -> p a t", t=128))
            dcol = decb[:, h * NCk + c:h * NCk + c + 1]
            for kc in range(4):
                pc_ = nps()
                P.mm(pc_, kk_[:, kc * 128:(kc + 1) * 128], v_)
                csl = slice(kc * 512, (kc + 1) * 512)
                tm = tmpc[tc_ % 4]
                tc_ += 1
                P.act(tm, pc_, AF.Copy, scale=dcol)
                P.stt('dve', CT[:, csl], CT[:, csl], dcol, tm, ALU.mult, ALU.add)
                P.copy('act', CTb[:, csl], CT[:, csl])
            pnn = nps()
            for kc in range(4):
                P.mm(pnn[:, kc * 128:(kc + 1) * 128], kk_[:, kc * 128:(kc + 1) * 128], onesb)
            tm = tmpc[tc_ % 4]
            tc_ += 1
            P.act(tm, pnn, AF.Copy, scale=dcol)
            P.stt('dve', nb_, nb_, dcol, tm, ALU.mult, ALU.add)
            P.copy('act', nbb, nb_)
    P.barrier()
    A.base = X.base0
    A.reset()
    o512 = A.alloc(128, F32)
    P.memset('dve', o512, 1.0 / 512)
    hb = [A.alloc(2048, F32) for _ in range(2)]
    dq = [A.alloc(2048, F32) for _ in range(2)]
    rs_ = [A.alloc(512, F32) for _ in range(2)]
    zb = [A.alloc(512, F32) for _ in range(2)]
    xcl = [A.alloc(512, BF16) for _ in range(2)]
    xcf = [A.alloc(512, F32) for _ in range(2)]
    ob = [A.alloc(512, BF16) for _ in range(2)]
    n = 0
    for h in range(8):
        for it in range(8):
            sl = slice(it * 512, (it + 1) * 512)
            i2 = n % 2
            n += 1
            hb_, d_, r_ = hb[i2], dq[i2], rs_[i2]
            P.dma('sp', hb_.re("p (k t) -> p k t", t=512), X.hS.re("(k p) t -> p k t", p=128)[:, h * 4:(h + 1) * 4, sl])
            pm = nps()
            for vc in range(4):
                P.mm(pm, o512, hb_[:, vc * 512:(vc + 1) * 512], start=(vc == 0), stop=(vc == 3))
            for vc in range(4):
                P.tt('dve', d_[:, vc * 512:(vc + 1) * 512], hb_[:, vc * 512:(vc + 1) * 512], pm, ALU.subtract)
            P.act(hb_, d_, AF.Square)
            pv = nps()
            for vc in range(4):
                P.mm(pv, o512, hb_[:, vc * 512:(vc + 1) * 512], start=(vc == 0), stop=(vc == 3))
            P.ts('dve', r_, pv, EPS, None, ALU.add)
            P.act(r_, r_, AF.Ln)
            P.act(r_, r_, AF.Exp, scale=-0.5)
            for vc in range(4):
                cidx = h * 4 + vc
                rows = slice(cidx * 128, (cidx + 1) * 128)
                j2 = (n * 4 + vc) % 2
                P.stt('dve', d_[:, vc * 512:(vc + 1) * 512], d_[:, vc * 512:(vc + 1) * 512], C('c_ln_w')[:, cidx:cidx + 1],
                      r_, ALU.mult, ALU.mult)
                P.dma('sp', xcl[j2], X.xcT[rows, sl])
                P.dma('sp', zb[j2], X.xmz[4096 + cidx * 128:4096 + (cidx + 1) * 128, sl])
                P.copy('act', xcf[j2], xcl[j2])
                P.stt('dve', xcf[j2], xcf[j2], C('c_skip')[:, cidx:cidx + 1], d_[:, vc * 512:(vc + 1) * 512], ALU.mult, ALU.add)
                P.act(zb[j2], zb[j2], AF.Silu)
                P.tt('dve', ob[j2], xcf[j2], zb[j2], ALU.mult)
                P.dma('act', X.hsT[rows, sl], ob[j2])
    A.base = X.base0
    phase_resid_gemm(X, X.hsT, 32, X.w_oout, 1024, xin, xout)
```

```python
from contextlib import ExitStack
import numpy as np
import concourse.bass as bass
import concourse.mybir as mybir
from concourse.bass_utils import run_bass_kernel_spmd

F32 = mybir.dt.float32
BF16 = mybir.dt.bfloat16
AF = mybir.ActivationFunctionType
ALU = mybir.AluOpType
AX = mybir.AxisListType

ENGS = ('pe', 'act', 'dve', 'pool', 'sp')


class Buf:
    __slots__ = ('name', 'kind', 'lw', 'rd', 'semw', 'semr', 'cw', 'cr')

    def __init__(self, name, kind):
        self.name = name
        self.kind = kind
        self.lw = None
        self.rd = {}
        self.semw = None
        self.semr = None
        self.cw = 0
        self.cr = 0


class V:
    __slots__ = ('ap', 'buf')

    def __init__(self, ap, buf):
        self.ap = ap
        self.buf = buf

    def __getitem__(self, idx):
        return V(self.ap[idx], self.buf)

    def re(self, pattern, **kw):
        return V(self.ap.rearrange(pattern, **kw), self.buf)

    def sub(self, name_unused, idx):
        return V(self.ap[idx], self.buf)


def _ap(x):
    return x.ap if isinstance(x, V) else x


class Prog:
    def __init__(self, nc):
        self.nc = nc
        self.stack = ExitStack()
        self.ins = []
        self.nsem = 0
        self.npsum = 0
        self.last_eng = {}
        self.last_dma = {}
        self.bar = set()
        self.sem_pool = []
        self.sem_active = []

    def dram(self, name, shape, dtype, kind="Internal"):
        t = self.nc.dram_tensor(name, list(shape), dtype, kind=kind)
        return V(t.ap(), Buf(name, 'dram'))

    def sbuf(self, name, shape, dtype, nbuf=None):
        t = self.stack.enter_context(self.nc.sbuf_tensor(name, list(shape), dtype))
        return V(t[:], Buf(name, 'sbuf'))

    def psum(self, name, shape=(128, 512), dtype=F32):
        t = self.stack.enter_context(self.nc.psum_tensor(name, list(shape), dtype))
        return V(t[:], Buf(name, 'psum'))

    def view(self, v, name):
        return V(v.ap, Buf(name, v.buf.kind))

    def _sem(self, name):
        self.nsem += 1
        return self.stack.enter_context(self.nc.semaphore(name))

    def emit(self, eng, fn, reads, writes, dma=None):
        iid = len(self.ins)
        deps = set(self.bar)
        wb = []
        for v in writes:
            b = v.buf
            if b in wb:
                continue
            wb.append(b)
            if b.lw is not None:
                deps.add(b.lw)
            deps.update(b.rd.values())
        rb = []
        for v in reads:
            if not isinstance(v, V):
                continue
            b = v.buf
            if b in wb or b in rb:
                continue
            rb.append(b)
            if b.lw is not None:
                deps.add(b.lw)
        key = eng
        dsem = None
        if dma is not None:
            kind, sb = dma
            if kind == 'w':
                if sb.semw is None:
                    sb.semw, sb.cw = self._take_sem("dw_" + sb.name)
                    self.sem_active.append((sb, 'w'))
                sb.cw += 16
                dsem = (sb.semw, sb.cw)
            else:
                if sb.semr is None:
                    sb.semr, sb.cr = self._take_sem("dr_" + sb.name)
                    self.sem_active.append((sb, 'r'))
                sb.cr += 16
                dsem = (sb.semr, sb.cr)
            key = ('dma', id(dsem[0]))
        for b in wb:
            b.lw = iid
            b.rd = {}
        for b in rb:
            b.rd[key] = iid
        if dsem is None:
            self.last_eng[eng] = iid
        else:
            self.last_dma[id(dsem[0])] = iid
        self.ins.append((eng, fn, deps, dsem))
        return iid

    def _take_sem(self, name):
        if self.sem_pool:
            return self.sem_pool.pop()
        return self._sem(name), 0

    def barrier(self):
        self.bar = set(self.last_eng.values()) | set(self.last_dma.values())
        for (b, kind) in self.sem_active:
            if kind == 'w':
                self.sem_pool.append((b.semw, b.cw))
                b.semw = None
            else:
                self.sem_pool.append((b.semr, b.cr))
                b.semr = None
        self.sem_active = []

    def dma(self, q, out, in_):
        ob, ib = out.buf, in_.buf
        if ob.kind == 'sbuf':
            d = ('w', ob)
        else:
            assert ib.kind == 'sbuf', "dram->dram dma not supported"
            d = ('r', ib)
        o, i = out.ap, in_.ap
        return self.emit(q, lambda e: e.dma_start(out=o, in_=i), [in_], [out], dma=d)

    def mm(self, out, lhsT, rhs, start=True, stop=True, **kw):
        o, l, r = out.ap, lhsT.ap, rhs.ap
        return self.emit('pe', lambda e: e.matmul(o, l, r, start=start, stop=stop, **kw),
                         [lhsT, rhs], [out])

    def transpose(self, out, in_, ident):
        o, i, d = out.ap, in_.ap, ident.ap
        return self.emit('pe', lambda e: e.transpose(o, i, d), [in_, ident], [out])

    def act(self, out, in_, func, bias=None, scale=1.0, accum=None, eng='act'):
        o, i = out.ap, in_.ap
        b, s, a = _ap(bias), _ap(scale), _ap(accum)
        kw = {}
        if b is not None:
            kw['bias'] = b
        if a is not None:
            kw['accum_out'] = a
        w = [out] + ([accum] if accum is not None else [])
        return self.emit(eng, lambda e: e.activation(out=o, in_=i, func=func, scale=s, **kw),
                         [in_, bias, scale], w)

    def tt(self, eng, out, in0, in1, op):
        o, a, b = out.ap, in0.ap, in1.ap
        return self.emit(eng, lambda e: e.tensor_tensor(out=o, in0=a, in1=b, op=op), [in0, in1], [out])

    def ts(self, eng, out, in0, s1, s2, op0, op1=None, accum=None):
        o, a = out.ap, in0.ap
        x1, x2, ac = _ap(s1), _ap(s2), _ap(accum)
        kw = {}
        if op1 is not None:
            kw['op1'] = op1
        if ac is not None:
            kw['accum_out'] = ac
        w = [out] + ([accum] if accum is not None else [])
        return self.emit(eng, lambda e: e.tensor_scalar(out=o, in0=a, scalar1=x1, scalar2=x2, op0=op0, **kw),
                         [in0, s1, s2], w)

    def stt(self, eng, out, in0, scalar, in1, op0, op1):
        o, a, b, s = out.ap, in0.ap, in1.ap, _ap(scalar)
        return self.emit(eng, lambda e: e.scalar_tensor_tensor(out=o, in0=a, scalar=s, in1=b, op0=op0, op1=op1),
                         [in0, in1, scalar], [out])

    def scan(self, out, d0, d1, init, op0, op1):
        o, a, b, i = out.ap, d0.ap, d1.ap, _ap(init)
        return self.emit('dve', lambda e: e.tensor_tensor_scan(out=o, data0=a, data1=b, initial=i, op0=op0, op1=op1),
                         [d0, d1, init], [out])

    def copy(self, eng, out, in_):
        o, i = out.ap, in_.ap
        if eng == 'act':
            return self.emit(eng, lambda e: e.copy(out=o, in_=i), [in_], [out])
        return self.emit(eng, lambda e: e.tensor_copy(out=o, in_=i), [in_], [out])

    def memset(self, eng, out, val):
        o = out.ap
        return self.emit(eng, lambda e: e.memset(o, val), [], [out])

    def reduce(self, out, in_, op, axis=AX.X, eng='dve'):
        o, i = out.ap, in_.ap
        return self.emit(eng, lambda e: e.tensor_reduce(out=o, in_=i, axis=axis, op=op), [in_], [out])

    def recip(self, out, in_):
        o, i = out.ap, in_.ap
        return self.emit('dve', lambda e: e.reciprocal(out=o, in_=i), [in_], [out])

    def affine_select(self, out, in_, pattern, cmp, fill, base, cm):
        o, i = out.ap, in_.ap
        return self.emit('pool', lambda e: e.affine_select(out=o, in_=i, pattern=pattern, compare_op=cmp,
                                                           fill=fill, base=base, channel_multiplier=cm),
                         [in_], [out])

    def finish(self, final_wait=True):
        nc = self.nc
        ins = self.ins
        n = len(ins)
        needed = [False] * n
        for (eng, fn, deps, dsem) in ins:
            for d in deps:
                if ins[d][0] == 'pe' and eng == 'pe' and ins[d][3] is None:
                    continue
                needed[d] = True
        esem = {e: self._sem("c_" + e) for e in ('pe', 'act', 'dve', 'pool')}
        ecnt = {e: 0 for e in esem}
        token = [None] * n
        known = {e: {} for e in ENGS}
        snap = [None] * n
        stream = {e: [] for e in ENGS}
        for iid, (eng, fn, deps, dsem) in enumerate(ins):
            kn = known[eng]
            waits = {}
            for d in deps:
                if ins[d][0] == 'pe' and eng == 'pe' and ins[d][3] is None:
                    continue
                sem, val = token[d]
                sid = id(sem)
                if kn.get(sid, 0) >= val:
                    continue
                if sid not in waits or waits[sid][1] < val:
                    waits[sid] = (sem, val)
            for d in deps:
                s = snap[d]
                if s is not None and token[d] is not None and id(token[d][0]) in waits:
                    for k2, v2 in s.items():
                        if kn.get(k2, 0) < v2:
                            kn[k2] = v2
            wl = []
            for sid, (sem, val) in waits.items():
                if kn.get(sid, 0) >= val:
                    continue
                kn[sid] = val
                wl.append((sem, val))
            inc = None
            if dsem is not None:
                token[iid] = dsem
                inc = (dsem[0], 16)
            elif needed[iid]:
                ecnt[eng] += 1
                token[iid] = (esem[eng], ecnt[eng])
                inc = (esem[eng], 1)
                kn[id(esem[eng])] = max(kn.get(id(esem[eng]), 0), 0)
            if needed[iid] or dsem is not None:
                snap[iid] = dict(kn)
            stream[eng].append((wl, fn, inc))
        finals = []
        seen = set()
        for (eng, fn, deps, dsem) in ins:
            if dsem is not None:
                seen.add(id(dsem[0]))
        allbufs = {}
        for (eng, fn, deps, dsem) in ins:
            if dsem is not None:
                sid = id(dsem[0])
                if sid not in allbufs or allbufs[sid][1] < dsem[1]:
                    allbufs[sid] = dsem
        finals = list(allbufs.values())
        self.stats = {e: len(stream[e]) for e in ENGS}
        self.stats['waits'] = sum(len(w) for e in ENGS for (w, _, _) in stream[e])
        self.stats['sems'] = self.nsem

        with nc.Block() as block:
            def run(e, name):
                for (wl, fn, inc) in stream[name]:
                    for (sem, val) in wl:
                        e.wait_ge(sem, val)
                    r = fn(e)
                    if inc is not None:
                        r.then_inc(inc[0], inc[1])
                if name == 'sp' and final_wait:
                    for (sem, val) in finals:
                        e.wait_ge(sem, val)
                    for en in esem:
                        if ecnt[en] > 0:
                            e.wait_ge(esem[en], ecnt[en])

            @block.sync
            def _(e):
                run(e, 'sp')

            @block.scalar
            def _(e):
                run(e, 'act')

            @block.vector
            def _(e):
                run(e, 'dve')

            @block.gpsimd
            def _(e):
                run(e, 'pool')

            @block.tensor
            def _(e):
                run(e, 'pe')
        self.stack.close()
        return nc


T = 4096
D = 2048
EPS = 1e-6
J_EIN = 43
P_ROWS = 5408
DFF = 5632
KFF = 44
CW = 4096
B_LN_EPS = 64e-5


def _cc(v, pad_to=None):
    v = np.asarray(v, np.float32).reshape(-1)
    if pad_to is not None and v.size < pad_to:
        v = np.concatenate([v, np.zeros(pad_to - v.size, np.float32)])
    return np.ascontiguousarray(v.reshape(-1, 128).T)


def _tile_w(w, J):
    K, M = w.shape
    kc = K // 128
    wp = np.zeros((K, J * 128), np.float32)
    wp[:, :M] = w
    return np.ascontiguousarray(wp.reshape(kc, 128, J, 128).transpose(2, 1, 0, 3).reshape(J, 128, kc * 128))


def pack_consts(inp):
    ent = []

    def add(name, arr):
        ent.append((name, np.asarray(arr, np.float32)))

    add('even_norm', _cc(inp['even_norm'][0]))
    add('ffn_norm0', _cc(inp['ffn_norm'][0]))
    add('ffn_norm1', _cc(inp['ffn_norm'][1]))
    add('odd_norm', _cc(inp['odd_norm'][0]))
    add('final_norm', _cc(inp['final_norm']))
    add('a_conv_w', inp['a_conv_w'][0].reshape(4, 8, 128).transpose(2, 1, 0).reshape(128, 32))
    for nm in ('a_conv_b', 'a_b_r', 'a_b_i', 'a_lambda'):
        add(nm, _cc(inp[nm][0]))
    add('b_mu', _cc(inp['b_mu'][0], 27 * 128))
    for nm in ('b_w0', 'b_a0', 'b_k_k', 'b_k_a', 'b_r_k', 'b_ln_w', 'b_ln_b'):
        add(nm, _cc(inp[nm][0]))
    for l in range(2):
        add('ffn_conv_w%d' % l, inp['ffn_conv_w'][l].reshape(3, KFF, 128).transpose(2, 1, 0).reshape(128, KFF * 3))
        add('ffn_conv_b%d' % l, _cc(inp['ffn_conv_b'][l]))
    add('c_conv_w', inp['c_conv_w'][0].reshape(4, 32, 128).transpose(2, 1, 0).reshape(128, 128))
    for nm in ('c_conv_b', 'c_ln_w', 'c_skip'):
        add(nm, _cc(inp[nm][0]))
    for nm in ('c_w_q', 'c_w_k', 'c_w_v'):
        add(nm, inp[nm][0].reshape(32, 128, 4).transpose(1, 0, 2).reshape(128, 128))
    bif = np.zeros((128, 1), np.float32)
    bif[:16, 0] = inp['c_b_if'][0]
    add('c_b_if', bif)
    offs = {}
    o = 0
    for name, a in ent:
        offs[name] = (o, a.shape[1])
        o += a.shape[1]
    return np.ascontiguousarray(np.concatenate([a for _, a in ent], axis=1)), offs


def const_offsets():
    dummy = {
        'even_norm': np.zeros((1, D)), 'ffn_norm': np.zeros((2, D)), 'odd_norm': np.zeros((1, D)),
        'final_norm': np.zeros(D), 'a_conv_w': np.zeros((1, 4, 1024)),
        'b_mu': np.zeros((1, 3360)), 'ffn_conv_w': np.zeros((2, 3, DFF)), 'ffn_conv_b': np.zeros((2, DFF)),
        'c_conv_w': np.zeros((1, 4, CW)), 'c_b_if': np.zeros((1, 16)),
    }
    for nm in ('a_conv_b', 'a_b_r', 'a_b_i', 'a_lambda', 'b_w0', 'b_a0', 'b_k_k', 'b_k_a', 'b_r_k', 'b_ln_w', 'b_ln_b'):
        dummy[nm] = np.zeros((1, 1024))
    for nm in ('c_conv_b', 'c_ln_w', 'c_skip'):
        dummy[nm] = np.zeros((1, CW))
    for nm in ('c_w_q', 'c_w_k', 'c_w_v'):
        dummy[nm] = np.zeros((1, 1024, 4, 4))
    c, offs = pack_consts(dummy)
    return c.shape[1], offs


class Arena:
    def __init__(self, P, cols):
        self.P = P
        self.t = P.sbuf("arena", [128, cols], F32)
        self.cols = cols
        self.base = 0
        self.off = 0
        self.n = 0

    def reset(self):
        self.off = self.base

    def persist(self):
        self.base = self.off

    def alloc(self, cols, dtype=F32, name=None):
        n32 = cols if dtype == F32 else (cols + 1) // 2
        a = self.off
        self.off += n32
        assert self.off <= self.cols, ("arena overflow", self.off, self.cols)
        ap = self.t.ap[:, a:a + n32]
        if dtype != F32:
            ap = ap.bitcast(dtype)[:, 0:cols]
        self.n += 1
        return V(ap, Buf(name or ("ar%d" % self.n), 'sbuf'))


class Ctx:
    pass


def load_cast(X, dst, src, rows=128):
    cols = dst.ap.shape[1]
    stg = X.A.alloc(cols, F32)
    X.P.dma('sp', stg[0:rows], src)
    X.P.copy('act', dst[0:rows], stg[0:rows])


def split_groups(n, g):
    g = min(g, n)
    base, rem = divmod(n, g)
    out = []
    a = 0
    for i in range(g):
        b = a + base + (1 if i < rem else 0)
        out.append((a, b))
        a = b
    return out


def gemm(X, src, KC, wsets, J, TB, epi, jlist=None):
    P, A = X.P, X.A
    ns = len(wsets)
    groups = split_groups(KC, 4)
    act = [A.alloc((b - a) * TB, BF16) for (a, b) in groups]
    wb = [[A.alloc(KC * 128, BF16) for _ in range(2)] for _ in range(ns)]
    wst = [[A.alloc(KC * 128, F32) for _ in range(2)] for _ in range(ns)]
    srcv = src.re("(k p) t -> p k t", p=128)
    nsub = TB // 512
    cnt = 0
    jl = list(range(J)) if jlist is None else jlist
    its = [(tb, ji) for tb in range(T // TB) for ji in range(len(jl))]

    def wload(n):
        tb_, ji_ = its[n]
        for s in range(ns):
            P.dma('sp', wst[s][n % 2], wsets[s][jl[ji_]])
            P.copy('dve' if (n + s) % 2 == 0 else 'act', wb[s][n % 2], wst[s][n % 2])

    wload(0)
    for n, (tb, ji) in enumerate(its):
        j = jl[ji]
        if ji == 0:
            for gi, (a, b) in enumerate(groups):
                P.dma('sp', act[gi].re("p (k t) -> p k t", t=TB), srcv[:, a:b, tb * TB:(tb + 1) * TB])
        if n + 1 < len(its):
            wload(n + 1)
        for sp in range(0, nsub, 2):
            subs = [sp, sp + 1] if sp + 1 < nsub else [sp]
            base = (cnt % 2) * 4
            pgrp = [[X.ps[base + si * ns + s] for s in range(ns)] for si in range(len(subs))]
            for s in range(ns):
                for gi, (a, b) in enumerate(groups):
                    for k in range(a, b):
                        for si, sub in enumerate(subs):
                            P.mm(pgrp[si][s], wb[s][n % 2][:, k * 128:(k + 1) * 128],
                                 act[gi][:, (k - a) * TB + sub * 512:(k - a) * TB + (sub + 1) * 512],
                                 start=(k == 0), stop=(k == KC - 1))
            cnt += 1
            for si, sub in enumerate(subs):
                epi(j, tb * TB + sub * 512, pgrp[si])


def phase_norm(X, src, gname, dst, out_f32=False):
    P, A = X.P, X.A
    P.barrier()
    A.reset()
    g = X.C(gname)
    odt = F32 if out_f32 else BF16
    xs = [A.alloc(16 * 512, F32) for _ in range(2)]
    sq = [A.alloc(16 * 512, BF16) for _ in range(2)]
    hs = [A.alloc(16 * 512, odt) for _ in range(2)]
    rs = [A.alloc(512, F32) for _ in range(2)]
    sv = src.re("(k p) t -> p k t", p=128)
    dv = dst.re("(k p) t -> p k t", p=128)
    for it in range(T // 512):
        x, q, h, r = xs[it % 2], sq[it % 2], hs[it % 2], rs[it % 2]
        P.dma('sp', x.re("p (k t) -> p k t", t=512), sv[:, :, it * 512:(it + 1) * 512])
        P.act(q, x, AF.Square)
        ps = X.ps[6 + it % 2]
        for k in range(16):
            P.mm(ps, X.ones_bf, q[:, k * 512:(k + 1) * 512], start=(k == 0), stop=(k == 15))
        P.ts('dve', r, ps, EPS, None, ALU.add)
        P.act(r, r, AF.Ln)
        P.act(r, r, AF.Exp, scale=-0.5)
        for k in range(16):
            P.stt('dve', h[:, k * 512:(k + 1) * 512], x[:, k * 512:(k + 1) * 512],
                  g[:, k:k + 1], r, ALU.mult, ALU.mult)
        P.dma('act', dv[:, :, it * 512:(it + 1) * 512], h.re("p (k t) -> p k t", t=512))


def phase_even_inproj(X):
    P, A = X.P, X.A
    P.barrier()
    A.reset()
    st = [A.alloc(512, F32) for _ in range(4)]
    c = [0]

    def epi(j, t0, pss):
        s = st[c[0] % 4]
        if c[0] % 2 == 0:
            P.copy('act', s, pss[0])
        else:
            P.copy('dve', s, pss[0])
        c[0] += 1
        rows = min(128, P_ROWS - j * 128)
        P.dma('sp', X.pT[j * 128:j * 128 + rows, t0:t0 + 512], s[0:rows, :])

    gemm(X, X.hT, 16, [X.w_ein], J_EIN, 2048, epi)


def phase_resid_gemm(X, src, KC, w, TB, xin, xout):
    P, A = X.P, X.A
    P.barrier()
    A.reset()
    st = [A.alloc(512, F32) for _ in range(4)]
    c = [0]

    def epi(j, t0, pss):
        s = st[c[0] % 4]
        c[0] += 1
        P.dma('sp', s, xin[j * 128:(j + 1) * 128, t0:t0 + 512])
        P.tt('dve', s, s, pss[0], ALU.add)
        P.dma('sp', xout[j * 128:(j + 1) * 128, t0:t0 + 512], s)

    gemm(X, src, KC, [w], 16, TB, epi)


def phase_ffn_up(X, l):
    P, A = X.P, X.A
    P.barrier()
    A.reset()
    cw = X.C('ffn_conv_w%d' % l)
    cb = X.C('ffn_conv_b%d' % l)
    gb = [A.alloc(514, F32) for _ in range(3)]
    acc = [A.alloc(512, F32) for _ in range(2)]
    tmp = [A.alloc(512, F32) for _ in range(2)]
    ub = [A.alloc(512, BF16) for _ in range(3)]
    halo = A.alloc(KFF * 2, F32)
    c = [0]
    TB = 2048

    def epi(j, t0, pss):
        i = c[0]
        c[0] += 1
        g = gb[i % 3]
        gprev = gb[(i - 1) % 3]
        a = acc[i % 2]
        u = ub[i % 3]
        P.copy('act', g[:, 2:514], pss[0])
        if t0 == 0:
            P.memset('dve', g[:, 0:2], 0.0)
        elif t0 % TB == 0:
            P.copy('dve', g[:, 0:2], halo[:, 2 * j:2 * j + 2])
        else:
            P.copy('dve', g[:, 0:2], gprev[:, 512:514])
        if (t0 + 512) % TB == 0:
            P.copy('dve', halo[:, 2 * j:2 * j + 2], g[:, 512:514])
        P.act(a, g[:, 0:512], AF.Identity, bias=cb[:, j:j + 1], scale=cw[:, 3 * j:3 * j + 1])
        P.stt('dve', a, g[:, 1:513], cw[:, 3 * j + 1:3 * j + 2], a, ALU.mult, ALU.add)
        P.stt('dve', a, g[:, 2:514], cw[:, 3 * j + 2:3 * j + 3], a, ALU.mult, ALU.add)
        P.act(a, a, AF.Silu)
        P.tt('dve', u, a, pss[1], ALU.mult)
        P.dma('sp', X.uT[j * 128:(j + 1) * 128, t0:t0 + 512], u)

    gemm(X, X.hT, 16, [X.w_gate[l], X.w_up[l]], KFF, TB, epi)


def phase_rglru(X):
    P, A = X.P, X.A
    P.barrier()
    A.reset()
    wr = A.alloc(8 * 128, BF16)
    wi = A.alloc(8 * 128, BF16)
    load_cast(X, wr, X.a_w_r)
    load_cast(X, wi, X.a_w_i)
    cl = A.alloc(8, F32)
    cl2 = A.alloc(8, F32)
    P.act(cl, X.C('a_lambda'), AF.Exp, scale=-1.0)
    P.ts('dve', cl, cl, 1.0, None, ALU.add)
    P.act(cl, cl, AF.Ln)
    P.ts('dve', cl2, cl, -16.0, None, ALU.mult)
    P.ts('dve', cl, cl, -8.0, None, ALU.mult)
    xa = A.alloc(3 + T, F32)
    ga = A.alloc(T, F32)
    xc = A.alloc(T, F32)
    xcb = A.alloc(T, BF16)
    r = A.alloc(T, F32)
    ig = A.alloc(T, F32)
    a = A.alloc(T, F32)
    s = A.alloc(T, F32)
    yb = A.alloc(T, BF16)
    P.memset('pool', xa[:, 0:3], 0.0)
    cwt = X.C('a_conv_w')
    for c in range(8):
        P.dma('sp', xa[:, 3:3 + T], X.pT[c * 128:(c + 1) * 128, :])
        P.dma('sp', ga, X.pT[(8 + c) * 128:(9 + c) * 128, :])
        P.ts('dve', xc, xa[:, 0:T], cwt[:, 4 * c:4 * c + 1], X.C('a_conv_b')[:, c:c + 1], ALU.mult, ALU.add)
        for j in range(1, 4):
            P.stt('dve', xc, xa[:, j:j + T], cwt[:, 4 * c + j:4 * c + j + 1], xc, ALU.mult, ALU.add)
        P.copy('pool', xcb, xc)
        for it in range(8):
            sl = slice(it * 512, (it + 1) * 512)
            p1 = X.ps[(2 * it) % 6]
            p2 = X.ps[(2 * it + 1) % 6]
            P.mm(p1, wr[:, c * 128:(c + 1) * 128], xcb[:, sl])
            P.mm(p2, wi[:, c * 128:(c + 1) * 128], xcb[:, sl])
            P.act(r[:, sl], p1, AF.Sigmoid, bias=X.C('a_b_r')[:, c:c + 1])
            P.act(ig[:, sl], p2, AF.Sigmoid, bias=X.C('a_b_i')[:, c:c + 1])
        P.act(a, r, AF.Exp, scale=cl[:, c:c + 1])
        P.act(s, r, AF.Exp, scale=cl2[:, c:c + 1])
        P.ts('dve', s, s, -1.0, 1.0, ALU.mult, ALU.add)
        P.act(s, s, AF.Sqrt)
        P.tt('pool', ig, ig, xc, ALU.mult)
        P.tt('dve', s, s, ig, ALU.mult)
        P.scan(r, a, s, 0.0, ALU.mult, ALU.add)
        P.tt('pool', a, ga, ga, ALU.mult)
        P.ts('dve', a, a, 0.044715, 1.0, ALU.mult, ALU.add)
        P.tt('pool', a, a, ga, ALU.mult)
        P.act(a, a, AF.Tanh, scale=0.7978845608028654)
        P.ts('dve', a, a, 1.0, 0.5, ALU.add, ALU.mult)
        P.tt('pool', a, a, ga, ALU.mult)
        P.tt('dve', yb, a, r, ALU.mult)
        P.dma('act', X.yT[c * 128:(c + 1) * 128, :], yb)


def build(stages=('all',), dbg=(), rparts=('prep', 'main', 'post'), rchunks=T // 64):
    nc = bass.Bass("TRN2", target_bir_lowering=False)
    P = Prog(nc)
    X = Ctx()
    X.P = P
    X.rparts = rparts
    import os as _os
    X.rcut = int(_os.environ.get('RCUT', '9'))
    X.rchunks = rchunks
    ncst, offs = const_offsets()

    def dr(name, shape, dtype=F32, kind="Internal"):
        if name in dbg:
            kind = "ExternalOutput"
        return P.dram(name, shape, dtype, kind=kind)

    ein = "ExternalInput"
    X.x0 = dr("xT", [D, T], F32, ein)
    cst_d = dr("cst", [128, ncst], F32, ein)
    X.w_ein = dr("w_ein", [J_EIN, 128, 2048], F32, ein)
    X.w_eout = dr("w_eout", [16, 128, 2048], F32, ein)
    X.w_gate = [dr("w_gate%d" % l, [KFF, 128, 2048], F32, ein) for l in range(2)]
    X.w_up = [dr("w_up%d" % l, [KFF, 128, 2048], F32, ein) for l in range(2)]
    X.w_down = [dr("w_down%d" % l, [16, 128, DFF], F32, ein) for l in range(2)]
    X.w_oin = dr("w_oin", [64, 128, 2048], F32, ein)
    X.w_oout = dr("w_oout", [16, 128, CW], F32, ein)
    X.a_w_r = dr("a_w_r", [128, 1024], F32, ein)
    X.a_w_i = dr("a_w_i", [128, 1024], F32, ein)
    X.wlr = dr("wlr", [128, 1024], F32, ein)
    X.gup1 = dr("gup1", [128, 1024], F32, ein)
    X.gup2 = dr("gup2", [32, 1024], F32, ein)
    X.c_w_if = dr("c_w_if", [128, 3 * 32 * 16], F32, ein)
    X.masks = dr("masks", [128, 128 + 4 * 512], F32, ein)
    X.out = dr("out", [D, T], F32, "ExternalOutput")
    X.hT = dr("hT", [D, T], BF16)
    X.pT = dr("pT", [P_ROWS, T], F32)
    X.yT = dr("yT", [D, T], BF16)
    X.uT = dr("uT", [DFF, T], BF16)
    X.xA = dr("xA", [D, T], F32)
    X.RW = dr("RW", [8, 128, 64 * 320], BF16)
    X.gS = dr("gS", [1024, T], F32)
    X.WLS = dr("WLS", [1024, 64], F32)
    X.xmz = dr("xmz", [8192, T], F32)
    X.qT = dr("qT", [CW, T], BF16)
    X.kT = dr("kT", [CW, T], BF16)
    X.q2T = dr("q2T", [CW, T], BF16)
    X.k2T = dr("k2T", [CW, T], BF16)
    X.xcT = dr("xcT", [CW, T], BF16)
    X.vtok = dr("vtok", [T, CW], BF16)
    X.hS = dr("hS", [CW, T], F32)
    X.hsT = dr("hsT", [CW, T], BF16)
    X.mconst = dr("mconst", [128, 1408], F32, ein)
    X.bif_d = dr("bif_d", [16, 1], F32, ein)
    X.rkS = dr("rkS", [1024, T], F32)
    X.vS = dr("vS", [1024, T], F32)
    X.yS = dr("yS", [1024, T], F32)
    X.xB = dr("xB", [D, T], F32)

    A = Arena(P, 50432)
    X.A = A
    X.ps = [P.psum("ps%d" % i) for i in range(8)]
    cst = A.alloc(ncst, F32, "cst")
    P.dma('sp', cst, cst_d)
    X.C = lambda name: cst[:, offs[name][0]:offs[name][0] + offs[name][1]]
    X.ones_bf = A.alloc(128, BF16, "ones")
    P.memset('dve', X.ones_bf, 1.0 / D)
    A.persist()
    X.base0 = A.base

    def on(s):
        return 'all' in stages or s in stages

    if on('e_norm'):
        phase_norm(X, X.x0, 'even_norm', X.hT)
    if on('e_in'):
        phase_even_inproj(X)
    if on('rglru'):
        phase_rglru(X)
    if on('rwkv'):
        phase_rwkv(X)
        A.base = X.base0
    if on('e_out'):
        phase_resid_gemm(X, X.yT, 16, X.w_eout, 2048, X.x0, X.xA)
    if on('ffn0'):
        phase_norm(X, X.xA, 'ffn_norm0', X.hT)
        phase_ffn_up(X, 0)
        phase_resid_gemm(X, X.uT, KFF, X.w_down[0], 1024, X.xA, X.xB)
    if on('mlstm'):
        phase_mlstm(X)
    if on('ffn1'):
        phase_norm(X, X.xA, 'ffn_norm1', X.hT)
        phase_ffn_up(X, 1)
        phase_resid_gemm(X, X.uT, KFF, X.w_down[1], 1024, X.xA, X.xB)
    if on('final'):
        phase_norm(X, X.xB, 'final_norm', X.out, out_f32=True)
    P.finish()
    X.stats = P.stats
    return nc, X


def host_masks():
    i = np.arange(128)[:, None]
    t = np.arange(64)[None, :]
    ident = (np.arange(128)[:, None] == np.arange(128)[None, :]).astype(np.float32)
    mS = np.tile((t > i).astype(np.float32), (1, 8))
    mI = np.tile((t >= i).astype(np.float32), (1, 8))
    mL = np.tile((i > t).astype(np.float32), (1, 8))
    idb = np.tile((t == i).astype(np.float32), (1, 8))
    return np.ascontiguousarray(np.concatenate([ident, mS, mI, mL, idb], axis=1))


def host_mconst():
    p = np.arange(128)[:, None]
    c = np.arange(128)[None, :]
    mbd = ((p // 4) == (c // 4)).astype(np.float32)
    maskC = (p <= c).astype(np.float32)
    ident = (p == c).astype(np.float32)
    sel = np.zeros((128, 1024), np.float32)
    for h in range(8):
        sel[h, h * 128:(h + 1) * 128] = 1.0
    return np.ascontiguousarray(np.concatenate([mbd, maskC, ident, sel], axis=1))


def host_inputs(inp):
    cst, _ = pack_consts(inp)
    sh = {
        'cst': cst,
        'masks': host_masks(),
        'mconst': host_mconst(),
        'bif_d': np.ascontiguousarray(inp['c_b_if'][0].reshape(16, 1)),
        'w_ein': _tile_w(inp['even_w_in'][0], J_EIN),
        'w_eout': _tile_w(inp['even_w_out'][0], 16),
        'w_oin': _tile_w(inp['odd_w_in'][0], 64),
        'w_oout': _tile_w(inp['odd_w_out'][0], 16),
        'a_w_r': np.ascontiguousarray(inp['a_w_r'][0].transpose(1, 0, 2).reshape(128, 1024)),
        'a_w_i': np.ascontiguousarray(inp['a_w_i'][0].transpose(1, 0, 2).reshape(128, 1024)),
        'wlr': np.ascontiguousarray(np.concatenate([inp['b_w_up'][0], inp['b_a_up'][0]], axis=0)),
        'gup1': np.ascontiguousarray(inp['b_g_up'][0][:128]),
        'gup2': np.ascontiguousarray(inp['b_g_up'][0][128:160]),
        'c_w_if': np.ascontiguousarray(inp['c_w_if'][0].reshape(3, 32, 128, 16).transpose(2, 0, 1, 3).reshape(128, 1536)),
    }
    for l in range(2):
        sh['w_gate%d' % l] = _tile_w(inp['ffn_w_gate'][l], KFF)
        sh['w_up%d' % l] = _tile_w(inp['ffn_w_up'][l], KFF)
        sh['w_down%d' % l] = _tile_w(inp['ffn_w_down'][l], 16)
    return sh


def kernel(**inputs):
    inp = {k: np.asarray(v) for k, v in inputs.items()}
    x = inp['x']
    B = x.shape[0]
    nc, X = build()
    sh = host_inputs(inp)
    in_maps = []
    for b in range(B):
        m = dict(sh)
        m['xT'] = np.ascontiguousarray(x[b].T)
        in_maps.append(m)
    res = run_bass_kernel_spmd(nc, in_maps, core_ids=list(range(B)))
    out = np.stack([np.ascontiguousarray(r['out'].T) for r in res.results], axis=0)
    return out.astype(np.float32)


def _c3(v, l=64):
    return v.re("p (c l) -> p c l", l=l)


def phase_rwkv(X):
    P, A = X.P, X.A
    P.barrier()
    A.reset()
    C = X.C
    TT = 1024
    NCH = TT // 64
    wlr = A.alloc(1024, BF16)
    gu1 = A.alloc(1024, BF16)
    gu2 = A.alloc(1024, BF16)
    BO = A.alloc(128, F32)
    BO64 = A.alloc(128, F32)
    for (t_, val) in ((BO, 1.0), (BO64, 1.0 / 64)):
        P.memset('dve', t_, 0.0)
        P.memset('dve', t_[0:64, 0:64], val)
        P.memset('dve', t_[64:128, 64:128], val)
    WL = A.alloc(8 * 64, F32)
    omk = A.alloc(8, F32)
    P.ts('dve', omk, C('b_k_a'), -1.0, 1.0, ALU.mult, ALU.add)
    A.persist()
    load_cast(X, wlr, X.wlr)
    load_cast(X, gu1, X.gup1)
    load_cast(X, gu2, X.gup2, rows=32)
    mask = A.alloc(TT, F32)
    P.memset('dve', mask, 1.0)
    P.memset('dve', _c3(mask)[:, :, 0:1], 0.0)
    xbs = [A.alloc(TT + 1, F32) for _ in range(4)]
    x4 = A.alloc(TT, F32)
    rr = A.alloc(TT, F32)
    k0 = A.alloc(TT, F32)
    vv = A.alloc(TT, F32)
    xwa = A.alloc(TT, BF16)
    sg1 = A.alloc(TT, BF16)
    sg2 = A.alloc(TT, BF16)
    sig, aic, gst, kk, sq, rn, km, bv, lw, cw, cwx, e1, e2, e3, rk = [A.alloc(TT, F32) for _ in range(15)]
    O = A.alloc(NCH * 320, BF16)
    Ov = O.re("p (c q l) -> p c q l", q=5, l=64)
    mu = C('b_mu')
    pc = [0]

    def nps():
        pc[0] += 1
        return X.ps[pc[0] % 8]

    def lerp(dst, xb, chunk, nrows, t0):
        r0 = chunk * 128
        if t0 == 0:
            P.memset('pool', xb[0:nrows, 0:1], 0.0)
            P.dma('sp', xb[0:nrows, 1:1 + TT], X.pT[r0:r0 + nrows, t0:t0 + TT])
        else:
            P.dma('sp', xb[0:nrows, 0:1 + TT], X.pT[r0:r0 + nrows, t0 - 1:t0 + TT])
        P.tt('dve', dst[0:nrows], xb[0:nrows, 0:TT], xb[0:nrows, 1:1 + TT], ALU.subtract)
        P.stt('dve', dst[0:nrows], dst[0:nrows], mu[0:nrows, chunk - 16:chunk - 15], xb[0:nrows, 1:1 + TT],
              ALU.mult, ALU.add)

    for tt in (range(T // TT) if 'prep' in X.rparts else []):
        t0 = tt * TT
        lerp(x4, xbs[0], 40, 128, t0)
        P.act(xwa[0:64], x4[0:64], AF.Tanh)
        P.copy('pool', xwa[64:128], x4[64:128])
        lerp(x4, xbs[0], 41, 128, t0)
        P.act(sg1, x4, AF.Sigmoid)
        lerp(x4, xbs[0], 42, 32, t0)
        P.act(sg2[0:32], x4[0:32], AF.Sigmoid)
        for hp in range(8):
            hc = slice(hp * 128, (hp + 1) * 128)
            h1 = slice(hp, hp + 1)
            lerp(rr, xbs[1], 16 + hp, 128, t0)
            lerp(k0, xbs[2], 24 + hp, 128, t0)
            lerp(vv, xbs[3], 32 + hp, 128, t0)
            for sub in range(TT // 512):
                sl = slice(sub * 512, (sub + 1) * 512)
                pz = nps()
                P.mm(pz, wlr[0:64, hc], xwa[0:64, sl])
                P.act(sig[:, sl], pz, AF.Sigmoid, bias=C('b_w0')[:, h1])
                pa = nps()
                P.mm(pa, wlr[64:128, hc], xwa[64:128, sl])
                P.act(aic[:, sl], pa, AF.Sigmoid, bias=C('b_a0')[:, h1])
                pg = nps()
                P.mm(pg, gu1[:, hc], sg1[:, sl], start=True, stop=False)
                P.mm(pg, gu2[0:32, hc], sg2[0:32, sl], start=False, stop=True)
                P.copy('act', gst[:, sl], pg)
            P.dma('act', X.gS[hc, t0:t0 + TT], gst)
            P.ts('dve', kk, k0, C('b_k_k')[:, h1], None, ALU.mult)
            P.act(sq, kk, AF.Square)
            for sub in range(TT // 512):
                sl = slice(sub * 512, (sub + 1) * 512)
                pq = nps()
                P.mm(pq, BO, sq[:, sl])
                P.ts('dve', rn[:, sl], pq, 1e-12, None, ALU.max)
            P.act(rn, rn, AF.Ln)
            P.act(rn, rn, AF.Exp, scale=-0.5)
            P.tt('dve', kk, kk, rn, ALU.mult)
            P.ts('dve', km, aic, C('b_k_a')[:, h1], omk[:, h1], ALU.mult, ALU.add)
            P.tt('pool', km, km, k0, ALU.mult)
            P.tt('pool', bv, kk, aic, ALU.mult)
            P.ts('dve', lw, sig, -0.6065306597126334, None, ALU.mult)
            P.scan(cw, mask, lw, 0.0, ALU.mult, ALU.add)
            P.tt('pool', cwx, cw, lw, ALU.subtract)
            P.act(e1, cw, AF.Exp)
            P.act(e2, cwx, AF.Exp)
            P.act(e3, cw, AF.Exp, scale=-1.0)
            P.stt('dve', Ov[:, :, 0, :], _c3(kk), -1.0, _c3(e2), ALU.mult, ALU.mult)
            P.tt('pool', Ov[:, :, 1, :], _c3(rr), _c3(e1), ALU.mult)
            P.tt('dve', Ov[:, :, 2, :], _c3(bv), _c3(e3), ALU.mult)
            P.tt('pool', Ov[:, :, 3, :], _c3(km), _c3(e3), ALU.mult)
            P.copy('act', Ov[:, :, 4, :], _c3(vv))
            wlt = WL[:, hp * 64 + tt * NCH:hp * 64 + (tt + 1) * NCH]
            P.copy('pool', wlt, e1[:, 63:TT:64])
            P.dma('act', X.WLS[hc, tt * NCH:(tt + 1) * NCH], wlt)
            P.tt('dve', rk, rr, km, ALU.mult)
            P.ts('dve', rk, rk, C('b_r_k')[:, h1], None, ALU.mult)
            P.dma('act', X.rkS[hc, t0:t0 + TT], rk)
            P.dma('act', X.vS[hc, t0:t0 + TT], vv)
            P.dma('act', X.RW[hp, :, tt * NCH * 320:(tt + 1) * NCH * 320], O)

    P.barrier()
    A.reset()
    mk = A.alloc(128 + 4 * 512, F32)
    P.dma('sp', mk, X.masks)
    ident = mk[:, 0:128]
    mS = mk[:, 128:640]
    mI = mk[:, 640:1152]
    mL = mk[:, 1152:1664]
    idb = mk[:, 1664:2176]
    NB = 2
    idn = ident[0:64, 0:64]
    WLh = A.alloc(16 * 64, F32)
    P.dma('sp', WLh[0:64].re("p (h c) -> p h c", c=64), X.WLS.re("(h p) c -> p h c", p=64))
    blk = [[A.alloc(NB * 320, BF16) for _ in range(16)] for _ in range(2)]
    idnb = A.alloc(64, BF16)
    P.copy('dve', idnb[0:64], idn)
    G = []
    for g in range(2):
        d = Ctx()
        d.vtk, d.btk, d.ktk, d.ArbT, d.AakT, d.ArkT, d.U, d.Sb = [A.alloc(512, BF16) for _ in range(8)]
        d.AabT, d.Aab, d.Z = [A.alloc(512, F32) for _ in range(3)]
        P.memset('pool', d.Sb, 0.0)
        d.Xs = [A.alloc(512, F32) for _ in range(2)]
        d.XTs = [A.alloc(512, F32) for _ in range(2)]
        d.PTs = [A.alloc(512, F32) for _ in range(2)]
        d.S = A.alloc(512, F32)
        P.memset('pool', d.S, 0.0)
        d.yb = [A.alloc(8 * NB * 64, F32) for _ in range(2)]
        d.pc = 0
        G.append(d)
    ySv = X.yS.re("(h p) t -> p h t", p=64)

    def hsl(h):
        return slice(h * 64, (h + 1) * 64)

    for c in (range(X.rchunks) if 'main' in X.rparts else []):
        tb, cc = divmod(c, NB)
        if cc == 0:
            for hh in range(16):
                hp, m = divmod(hh, 2)
                P.dma('sp', blk[tb % 2][hh][0:64], X.RW[hp, m * 64:(m + 1) * 64, tb * NB * 320:(tb + 1) * NB * 320])
        for g in range(2):
            d = G[g]

            def nb():
                d.pc += 1
                return X.ps[g * 4 + d.pc % 4]

            def opnd(h, q):
                return blk[tb % 2][g * 8 + h][0:64].re("p (c q l) -> p c q l", q=5, l=64)[:, cc, q, :]

            for (q, dst, eng) in ((4, d.vtk, 'act'), (2, d.btk, 'act'), (3, d.ktk, 'act')):
                pt = nb()
                for h in range(8):
                    P.mm(pt[0:64, hsl(h)], opnd(h, q), idnb[0:64])
                P.copy(eng, dst[0:64], pt[0:64])
            if X.rcut < 1:
                continue
            specs = ((2, 0, d.AabT, mS, 'dve'), (2, 1, d.ArbT, mI, 'dve'), (3, 0, d.AakT, mS, 'dve'),
                     (3, 1, d.ArkT, mI, 'dve'), (0, 2, d.Aab, mL, 'dve'))
            for (ql, qr, dst, mk_, eng) in specs:
                pa = nb()
                for h in range(8):
                    P.mm(pa[0:64, hsl(h)], opnd(h, ql), opnd(h, qr))
                if eng == 'dve':
                    P.tt('dve', dst[0:64], pa[0:64], mk_[0:64], ALU.mult)
                else:
                    P.copy('act', dst[0:64], pa[0:64])
                    P.tt('pool', dst[0:64], dst[0:64], mk_[0:64], ALU.mult)
            if X.rcut < 2:
                continue
            pz = nb()
            for h in range(8):
                P.mm(pz[0:64, hsl(h)], opnd(h, 0), d.Sb[0:64, hsl(h)], start=True, stop=False)
                P.mm(pz[0:64, hsl(h)], d.AakT[0:64, hsl(h)], d.vtk[0:64, hsl(h)], start=False, stop=True)
            P.copy('act', d.Z[0:64], pz[0:64])
            if X.rcut < 3:
                continue
            Xc, XTc = d.Aab, d.AabT
            PTc = d.PTs[0]
            P.tt('dve', PTc[0:64], d.AabT[0:64], idb[0:64], ALU.add)
            for lev in range(5):
                Xn, XTn, PTn = d.Xs[lev % 2], d.XTs[lev % 2], d.PTs[(lev + 1) % 2]
                px = nb()
                for h in range(8):
                    P.mm(px[0:64, hsl(h)], XTc[0:64, hsl(h)], Xc[0:64, hsl(h)])
                if lev < 4:
                    pxt = nb()
                    for h in range(8):
                        P.mm(pxt[0:64, hsl(h)], Xc[0:64, hsl(h)], XTc[0:64, hsl(h)])
                P.copy('act', Xn[0:64], px[0:64])
                if lev < 4:
                    P.copy('act', XTn[0:64], pxt[0:64])
                pp_ = nb()
                for h in range(8):
                    P.mm(pp_[0:64, hsl(h)], Xn[0:64, hsl(h)], PTc[0:64, hsl(h)])
                P.tt('dve', PTn[0:64], PTc[0:64], pp_[0:64], ALU.add)
                Xc, XTc, PTc = Xn, XTn, PTn
            if X.rcut < 4:
                continue
            pu = nb()
            for h in range(8):
                P.mm(pu[0:64, hsl(h)], PTc[0:64, hsl(h)], d.Z[0:64, hsl(h)])
            P.copy('act', d.U[0:64], pu[0:64])
            if X.rcut < 5:
                continue
            py = nb()
            for h in range(8):
                o = py[0:64, hsl(h)]
                P.mm(o, d.Sb[0:64, hsl(h)], opnd(h, 1), start=True, stop=False)
                P.mm(o, d.U[0:64, hsl(h)], d.ArbT[0:64, hsl(h)], start=False, stop=False)
                P.mm(o, d.vtk[0:64, hsl(h)], d.ArkT[0:64, hsl(h)], start=False, stop=True)
            yb = d.yb[tb % 2]
            P.copy('act', yb[0:64].re("p (a t) -> p a t", t=NB * 64)[:, :, cc * 64:(cc + 1) * 64],
                   py[0:64].re("p (a t) -> p a t", t=64))
            if X.rcut < 6:
                continue
            pS = nb()
            for h in range(8):
                o = pS[0:64, hsl(h)]
                P.mm(o, d.btk[0:64, hsl(h)], d.U[0:64, hsl(h)], start=True, stop=False)
                P.mm(o, d.ktk[0:64, hsl(h)], d.vtk[0:64, hsl(h)], start=False, stop=True)
            P.tt('dve', d.S[0:64], d.S[0:64], pS[0:64], ALU.add)
            wl = WLh[0:64].re("p (h c) -> p h c", c=64)[:, g * 8:(g + 1) * 8, c:c + 1]
            P.tt('dve', _c3(d.S[0:64]), _c3(d.S[0:64]), V(wl.ap.broadcast_to([64, 8, 64]), wl.buf), ALU.mult)
            P.copy('act', d.Sb[0:64], d.S[0:64])
            if cc == NB - 1:
                P.dma('act', ySv[:, g * 8:(g + 1) * 8, tb * NB * 64:(tb + 1) * NB * 64],
                      yb[0:64].re("p (a t) -> p a t", t=NB * 64))

    P.barrier()
    A.reset()
    ys, rks, vs_, gs, dd, s2, t1 = [[A.alloc(512, F32) for _ in range(2)] for _ in range(7)]
    ob = [A.alloc(512, BF16) for _ in range(2)]
    it = 0
    for hp in (range(8) if 'post' in X.rparts else []):
        hc = slice(hp * 128, (hp + 1) * 128)
        h1 = slice(hp, hp + 1)
        for ti in range(T // 512):
            tsl = slice(ti * 512, (ti + 1) * 512)
            i2 = it % 2
            it += 1
            y, rk_, v_, g_, d_, q_, t_ = ys[i2], rks[i2], vs_[i2], gs[i2], dd[i2], s2[i2], t1[i2]
            P.dma('sp', y, X.yS[hc, tsl])
            P.dma('sp', rk_, X.rkS[hc, tsl])
            P.dma('sp', v_, X.vS[hc, tsl])
            P.dma('sp', g_, X.gS[hc, tsl])
            p1 = nps()
            P.mm(p1, BO64, y)
            P.tt('dve', d_, y, p1, ALU.subtract)
            P.tt('pool', q_, d_, d_, ALU.mult)
            p2 = nps()
            P.mm(p2, BO64, q_)
            P.ts('dve', q_, p2, B_LN_EPS, None, ALU.add)
            P.act(q_, q_, AF.Ln)
            P.act(q_, q_, AF.Exp, scale=-0.5)
            P.tt('dve', d_, d_, q_, ALU.mult)
            P.ts('dve', d_, d_, C('b_ln_w')[:, h1], C('b_ln_b')[:, h1], ALU.mult, ALU.add)
            p3 = nps()
            P.mm(p3, BO, rk_)
            P.tt('dve', t_, v_, p3, ALU.mult)
            P.tt('pool', d_, d_, t_, ALU.add)
            P.tt('pool', ob[i2], d_, g_, ALU.mult)
            P.dma('act', X.yT[1024 + hp * 128:1024 + (hp + 1) * 128, tsl], ob[i2])


def phase_mlstm(X):
    P, A = X.P, X.A
    C = X.C
    xin, xout = X.xB, X.xA
    phase_norm(X, xin, 'odd_norm', X.hT)
    P.barrier()
    A.reset()
    st = [A.alloc(512, F32) for _ in range(4)]
    cn = [0]

    def epi(j, t0, pss):
        s = st[cn[0] % 4]
        if cn[0] % 2 == 0:
            P.copy('act', s, pss[0])
        else:
            P.copy('dve', s, pss[0])
        cn[0] += 1
        P.dma('sp', X.xmz[j * 128:(j + 1) * 128, t0:t0 + 512], s)

    gemm(X, X.hT, 16, [X.w_oin], 64, 2048, epi)
    P.barrier()
    A.reset()
    mc = A.alloc(128 + 128 + 128 + 1024, F32)
    P.dma('sp', mc, X.mconst)
    mbd, maskC, identf, sel = mc[:, 0:128], mc[:, 128:256], mc[:, 256:384], mc[:, 384:1408]
    wif = A.alloc(1536, BF16)
    wifv = wif.re("p (j c g) -> p j c g", j=3, c=32)
    gI = A.alloc(T, F32)
    gF = A.alloc(T, F32)
    P.memset('pool', gI, 0.0)
    P.memset('pool', gF, 0.0)
    A.persist()
    load_cast(X, wif, X.c_w_if)
    xm2 = [A.alloc(3 + T, F32) for _ in range(2)]
    xc2 = [A.alloc(T, F32) for _ in range(2)]
    xmb2 = [A.alloc(T, BF16) for _ in range(2)]
    xcb2 = [A.alloc(T, BF16) for _ in range(2)]
    qb = A.alloc(T, BF16)
    kb = A.alloc(T, BF16)
    vb = A.alloc(T, BF16)
    vt = [A.alloc(512, BF16) for _ in range(2)]
    bd = [A.alloc(128, BF16) for _ in range(3)]
    P.memset('pool', xm2[0][:, 0:3], 0.0)
    P.memset('pool', xm2[1][:, 0:3], 0.0)
    cwt = C('c_conv_w')
    pc = [0]

    def nps():
        pc[0] += 1
        return X.ps[pc[0] % 8]

    vtv = X.vtok.re("(b p) f -> p b f", p=128)
    for c in range(32):
        xm, xc, xmb, xcb = xm2[c % 2], xc2[c % 2], xmb2[c % 2], xcb2[c % 2]
        P.dma('sp', xm[:, 3:3 + T], X.xmz[c * 128:(c + 1) * 128, :])
        P.act(xc, xm[:, 0:T], AF.Identity, bias=C('c_conv_b')[:, c:c + 1], scale=cwt[:, 4 * c:4 * c + 1])
        for j in range(1, 4):
            P.stt('dve', xc, xm[:, j:j + T], cwt[:, 4 * c + j:4 * c + j + 1], xc, ALU.mult, ALU.add)
        P.act(xc, xc, AF.Silu)
        P.copy('act', xcb, xc)
        P.copy('dve', xmb, xm[:, 3:3 + T])
        for i, nm in enumerate(('c_w_q', 'c_w_k', 'c_w_v')):
            w4 = C(nm)[:, 4 * c:4 * c + 4]
            wb_ = V(w4.ap.unsqueeze(1).broadcast_to([128, 32, 4]), w4.buf)
            P.tt('dve', bd[i].re("p (g j) -> p g j", j=4), mbd.re("p (g j) -> p g j", j=4), wb_, ALU.mult)
        for it in range(8):
            sl = slice(it * 512, (it + 1) * 512)
            p1 = nps()
            P.mm(p1, bd[0], xcb[:, sl])
            P.copy('act', qb[:, sl], p1)
            p2 = nps()
            P.mm(p2, bd[1], xcb[:, sl])
            P.copy('act', kb[:, sl], p2)
            p3 = nps()
            P.mm(p3, bd[2], xmb[:, sl])
            P.copy('dve', vb[:, sl], p3)
            p4 = nps()
            for b4 in range(4):
                tb2 = it * 4 + b4
                P.mm(p4[:, b4 * 128:(b4 + 1) * 128], xmb[:, tb2 * 128:(tb2 + 1) * 128], bd[2])
            v_ = vt[it % 2]
            P.copy('act', v_, p4)
            P.dma('act', vtv[:, it * 4:(it + 1) * 4, c * 128:(c + 1) * 128], v_.re("p (b f) -> p b f", f=128))
            for (gt, lo) in ((gI, 0), (gF, 8)):
                pg = nps()
                P.mm(pg[0:8, :], wifv[:, 0, c, lo:lo + 8], qb[:, sl], start=True, stop=False)
                P.mm(pg[0:8, :], wifv[:, 1, c, lo:lo + 8], kb[:, sl], start=False, stop=False)
                P.mm(pg[0:8, :], wifv[:, 2, c, lo:lo + 8], vb[:, sl], start=False, stop=True)
                P.tt('dve', gt[0:8, sl], gt[0:8, sl], pg[0:8, :], ALU.add)
        P.dma('act', X.qT[c * 128:(c + 1) * 128, :], qb)
        P.dma('act', X.kT[c * 128:(c + 1) * 128, :], kb)
        P.dma('act', X.xcT[c * 128:(c + 1) * 128, :], xcb)
    P.barrier()
    A.reset()
    CH = 128
    NCk = T // CH
    bF = A.alloc(1, F32)
    P.dma('sp', bF[0:8, :], X.bif_d[8:16, :])
    dec = A.alloc(NCk, F32)
    decb = A.alloc(8 * NCk, F32)
    identb = A.alloc(128, BF16)
    P.copy('dve', identb, identf)
    onesb = A.alloc(128, BF16)
    P.memset('dve', onesb, 1.0)
    A.persist()
    bif = C('c_b_if')
    m8 = A.alloc(T, F32)
    P.memset('dve', m8, 1.0)
    P.memset('dve', _c3(m8, CH)[:, :, 0:1], 0.0)
    lf = A.alloc(T, F32)
    bb = A.alloc(T, F32)
    aa = A.alloc(T, F32)
    eb = A.alloc(T, F32)
    ea = A.alloc(T, F32)
    P.act(lf[0:8], gF[0:8], AF.Sigmoid, bias=bF[0:8, 0:1])
    P.act(lf[0:8], lf[0:8], AF.Ln)
    P.scan(bb[0:8], m8[0:8], lf[0:8], 0.0, ALU.mult, ALU.add)
    P.ts('dve', aa[0:8], gI[0:8], bif[0:8, 0:1], None, ALU.add)
    P.tt('dve', aa[0:8], aa[0:8], bb[0:8], ALU.subtract)
    P.act(eb[0:8], bb[0:8], AF.Exp)
    P.act(ea[0:8], aa[0:8], AF.Exp)
    P.copy('dve', dec[0:8], eb[0:8, CH - 1:T:CH])
    for h in range(8):
        pd = nps()
        P.mm(pd[:, 0:NCk], sel[0:8, h * 128:(h + 1) * 128], dec[0:8, :])
        P.copy('act', decb[:, h * NCk:(h + 1) * NCk], pd[:, 0:NCk])
    ebb = [A.alloc(512, F32) for _ in range(2)]
    eab = [A.alloc(512, F32) for _ in range(2)]
    qk = [A.alloc(512, BF16) for _ in range(4)]
    n = 0
    for h in range(8):
        for it in range(8):
            sl = slice(it * 512, (it + 1) * 512)
            e1_, e2_ = ebb[it % 2], eab[it % 2]
            p1 = nps()
            P.mm(p1, sel[0:8, h * 128:(h + 1) * 128], eb[0:8, sl])
            P.copy('act', e1_, p1)
            p2 = nps()
            P.mm(p2, sel[0:8, h * 128:(h + 1) * 128], ea[0:8, sl])
            P.copy('act', e2_, p2)
            for kc in range(4):
                rows = slice(h * 512 + kc * 128, h * 512 + (kc + 1) * 128)
                t1_, t2_ = qk[n % 4], qk[(n + 1) % 4]
                n += 2
                P.dma('sp', t1_, X.qT[rows, sl])
                P.dma('sp', t2_, X.kT[rows, sl])
                P.tt('dve', t1_, t1_, e1_, ALU.mult)
                P.stt('dve', t2_, t2_, 512.0 ** -0.5, e2_, ALU.mult, ALU.mult)
                P.dma('act', X.q2T[rows, sl], t1_)
                P.dma('act', X.k2T[rows, sl], t2_)
    P.barrier()
    A.reset()
    CTs = [A.alloc(2048, F32) for _ in range(8)]
    CTbs = [A.alloc(2048, BF16) for _ in range(8)]
    nbs = [A.alloc(512, F32) for _ in range(8)]
    nbbs = [A.alloc(512, BF16) for _ in range(8)]
    NR = 3
    qt = [A.alloc(512, BF16) for _ in range(NR)]
    kt = [A.alloc(512, BF16) for _ in range(NR)]
    vk = [A.alloc(512, BF16) for _ in range(NR)]
    ktok = [A.alloc(512, BF16) for _ in range(NR)]
    STb = [A.alloc(128, BF16) for _ in range(NR)]
    rec = [A.alloc(128, F32) for _ in range(NR)]
    ho = [A.alloc(512, F32) for _ in range(NR)]
    tmpc = [A.alloc(512, F32) for _ in range(4)]
    vtr = X.vtok
    for h in range(8):
        P.memset('pool', CTs[h], 0.0)
        P.memset('pool', CTbs[h], 0.0)
        P.memset('pool', nbs[h], 0.0)
        P.memset('pool', nbbs[h], 0.0)
    q2v = X.q2T.re("(k p) t -> p k t", p=128)
    k2v = X.k2T.re("(k p) t -> p k t", p=128)
    hSv = X.hS.re("(k p) t -> p k t", p=128)
    it_ = 0
    tc_ = 0
    for c in range(NCk):
        tsl = slice(c * CH, (c + 1) * CH)
        for h in range(8):
            i2 = it_ % NR
            it_ += 1
            CT, CTb, nb_, nbb = CTs[h], CTbs[h], nbs[h], nbbs[h]
            q_, k_, v_, kk_, S_, r_, o_ = qt[i2], kt[i2], vk[i2], ktok[i2], STb[i2], rec[i2], ho[i2]
            P.dma('sp', q_.re("p (k t) -> p k t", t=CH), q2v[:, h * 4:(h + 1) * 4, tsl])
            P.dma('sp', k_.re("p (k t) -> p k t", t=CH), k2v[:, h * 4:(h + 1) * 4, tsl])
            P.dma('sp', v_, vtr[tsl, h * 512:(h + 1) * 512])
            pk = nps()
            for kc in range(4):
                P.mm(pk[:, kc * 128:(kc + 1) * 128], k_[:, kc * 128:(kc + 1) * 128], identb)
            P.copy('act', kk_, pk)
            ps_ = nps()
            for kc in range(4):
                P.mm(ps_[:, 0:128], k_[:, kc * 128:(kc + 1) * 128], q_[:, kc * 128:(kc + 1) * 128],
                     start=(kc == 0), stop=(kc == 3))
            P.tt('dve', S_, ps_[:, 0:128], maskC, ALU.mult)
            pn = nps()
            for vc in range(4):
                o = pn[:, vc * 128:(vc + 1) * 128]
                P.mm(o, v_[:, vc * 128:(vc + 1) * 128], S_, start=True, stop=False)
                for kc in range(4):
                    P.mm(o, CTb[:, kc * 512 + vc * 128:kc * 512 + (vc + 1) * 128], q_[:, kc * 128:(kc + 1) * 128],
                         start=False, stop=(kc == 3))
            pdn = nps()
            P.mm(pdn[:, 0:128], onesb, S_, start=True, stop=False)
            for kc in range(4):
                P.mm(pdn[:, 0:128], nbb[:, kc * 128:(kc + 1) * 128], q_[:, kc * 128:(kc + 1) * 128],
                     start=False, stop=(kc == 3))
            P.act(r_, pdn[:, 0:128], AF.Abs)
            P.ts('dve', r_, r_, 1.0, None, ALU.max)
            P.recip(r_, r_)
            P.tt('dve', o_.re("p (a t) -> p a t", t=128), pn.re("p (a t) -> p a t", t=128),
                 V(r_.ap.unsqueeze(1).broadcast_to([128, 4, 128]), r_.buf), ALU.mult)
            P.dma('sp', hSv[:, h * 4:(h + 1) * 4, tsl], o_.re("p (a t) -> p a t", t=128))
            dcol = decb[:, h * NCk + c:h * NCk + c + 1]
            for kc in range(4):
                pc_ = nps()
                P.mm(pc_, kk_[:, kc * 128:(kc + 1) * 128], v_)
                csl = slice(kc * 512, (kc + 1) * 512)
                tm = tmpc[tc_ % 4]
                tc_ += 1
                P.act(tm, pc_, AF.Copy, scale=dcol)
                P.stt('dve', CT[:, csl], CT[:, csl], dcol, tm, ALU.mult, ALU.add)
                P.copy('act', CTb[:, csl], CT[:, csl])
            pnn = nps()
            for kc in range(4):
                P.mm(pnn[:, kc * 128:(kc + 1) * 128], kk_[:, kc * 128:(kc + 1) * 128], onesb)
            tm = tmpc[tc_ % 4]
            tc_ += 1
            P.act(tm, pnn, AF.Copy, scale=dcol)
            P.stt('dve', nb_, nb_, dcol, tm, ALU.mult, ALU.add)
            P.copy('act', nbb, nb_)
    P.barrier()
    A.base = X.base0
    A.reset()
    o512 = A.alloc(128, F32)
    P.memset('dve', o512, 1.0 / 512)
    hb = [A.alloc(2048, F32) for _ in range(2)]
    dq = [A.alloc(2048, F32) for _ in range(2)]
    rs_ = [A.alloc(512, F32) for _ in range(2)]
    zb = [A.alloc(512, F32) for _ in range(2)]
    xcl = [A.alloc(512, BF16) for _ in range(2)]
    xcf = [A.alloc(512, F32) for _ in range(2)]
    ob = [A.alloc(512, BF16) for _ in range(2)]
    n = 0
    for h in range(8):
        for it in range(8):
            sl = slice(it * 512, (it + 1) * 512)
            i2 = n % 2
            n += 1
            hb_, d_, r_ = hb[i2], dq[i2], rs_[i2]
            P.dma('sp', hb_.re("p (k t) -> p k t", t=512), X.hS.re("(k p) t -> p k t", p=128)[:, h * 4:(h + 1) * 4, sl])
            pm = nps()
            for vc in range(4):
                P.mm(pm, o512, hb_[:, vc * 512:(vc + 1) * 512], start=(vc == 0), stop=(vc == 3))
            for vc in range(4):
                P.tt('dve', d_[:, vc * 512:(vc + 1) * 512], hb_[:, vc * 512:(vc + 1) * 512], pm, ALU.subtract)
            P.act(hb_, d_, AF.Square)
            pv = nps()
            for vc in range(4):
                P.mm(pv, o512, hb_[:, vc * 512:(vc + 1) * 512], start=(vc == 0), stop=(vc == 3))
            P.ts('dve', r_, pv, EPS, None, ALU.add)
            P.act(r_, r_, AF.Ln)
            P.act(r_, r_, AF.Exp, scale=-0.5)
            for vc in range(4):
                cidx = h * 4 + vc
                rows = slice(cidx * 128, (cidx + 1) * 128)
                j2 = (n * 4 + vc) % 2
                P.stt('dve', d_[:, vc * 512:(vc + 1) * 512], d_[:, vc * 512:(vc + 1) * 512], C('c_ln_w')[:, cidx:cidx + 1],
                      r_, ALU.mult, ALU.mult)
                P.dma('sp', xcl[j2], X.xcT[rows, sl])
                P.dma('sp', zb[j2], X.xmz[4096 + cidx * 128:4096 + (cidx + 1) * 128, sl])
                P.copy('act', xcf[j2], xcl[j2])
                P.stt('dve', xcf[j2], xcf[j2], C('c_skip')[:, cidx:cidx + 1], d_[:, vc * 512:(vc + 1) * 512], ALU.mult, ALU.add)
                P.act(zb[j2], zb[j2], AF.Silu)
                P.tt('dve', ob[j2], xcf[j2], zb[j2], ALU.mult)
                P.dma('act', X.hsT[rows, sl], ob[j2])
    A.base = X.base0
    phase_resid_gemm(X, X.hsT, 32, X.w_oout, 1024, xin, xout)
```
